# Optimizing a Trainium2 kernel written in Bass

```python
import math
import jax, jax.numpy as jnp
from jax import lax
import numpy as np

D_MODEL = 1024
BATCH = 8
SEQ = 2048
DEPTH = 2

N_BRANCH = 4
BRANCH_WIDTH = D_MODEL // N_BRANCH
GLA_HEADS = 4
GLA_DK = BRANCH_WIDTH // (2 * GLA_HEADS)
GLA_DV = BRANCH_WIDTH // GLA_HEADS
GLA_GATE_RANK = 16
GLA_GATE_NORMALIZER = 16.0
GLA_CHUNK = 32
DIFF_HEADS = 4
DIFF_DQK = BRANCH_WIDTH // (2 * DIFF_HEADS)
DIFF_DV = 2 * DIFF_DQK
DIFF_QBLOCK = 128
ROPE_THETA = 10000.0
CONV_CH = BRANCH_WIDTH
CONV_WIDTH = 31
RWKV_HEADS = 4
RWKV_HEAD = BRANCH_WIDTH // RWKV_HEADS
RWKV_DECAY_RANK = 64
RWKV_AAA_RANK = 64
RWKV_GATE_RANK = 128
RWKV_SIZES = (BRANCH_WIDTH, BRANCH_WIDTH, BRANCH_WIDTH, RWKV_DECAY_RANK, RWKV_AAA_RANK, RWKV_GATE_RANK)
RWKV_COLS = sum(RWKV_SIZES)
IN_SIZES = (GLA_HEADS * GLA_DK, GLA_HEADS * GLA_DK, GLA_HEADS * GLA_DV, GLA_HEADS * GLA_DV, GLA_GATE_RANK,
            DIFF_HEADS * 2 * DIFF_DQK, DIFF_HEADS * 2 * DIFF_DQK, DIFF_HEADS * DIFF_DV,
            CONV_CH, CONV_CH,
            RWKV_COLS,
            N_BRANCH * D_MODEL)
N_IN = sum(IN_SIZES)
D_FF = ((8 * D_MODEL // 3 + 255) // 256) * 256
N_EXPERTS = 8
TOP_K = 2
D_EXPERT = 7 * D_MODEL // 2
N_DENSE = (DEPTH + 1) // 2
N_MOE = DEPTH // 2
RMS_EPS = 1e-6
LN_EPS = 1e-5
RWKV_GN_EPS = 64e-5

kernel_name = "hybrid_gated_gla_diff_conv_rwkv7_moe_block"

F32 = jnp.float32


def _split(t, sizes):
    idx = [int(i) for i in np.cumsum(sizes)[:-1]]
    return jnp.split(t, idx, axis=-1)


def rms_norm(x, g, eps=RMS_EPS):
    xf = x.astype(F32)
    y = xf * lax.rsqrt(jnp.mean(xf * xf, axis=-1, keepdims=True) + eps)
    return (y * g.astype(F32)).astype(x.dtype)


def layer_norm(x, g, b, eps=LN_EPS):
    xf = x.astype(F32)
    mu = jnp.mean(xf, axis=-1, keepdims=True)
    xc = xf - mu
    var = jnp.mean(xc * xc, axis=-1, keepdims=True)
    return (xc * lax.rsqrt(var + eps) * g.astype(F32) + b.astype(F32)).astype(x.dtype)


def rope_tables(positions):
    inv = 1.0 / (ROPE_THETA ** (jnp.arange(0, DIFF_DQK, 2, dtype=F32) / DIFF_DQK))
    ang = positions.astype(F32)[..., None] * inv
    return jnp.cos(ang), jnp.sin(ang)


def apply_rope(t, cos, sin):
    tf = t.astype(F32)
    t1, t2 = jnp.split(tf, 2, axis=-1)
    c = cos[:, :, None, None, :]
    s = sin[:, :, None, None, :]
    return jnp.concatenate([t1 * c - t2 * s, t1 * s + t2 * c], axis=-1)


def gla_mixer(q, k, v, og, gz, gate_w2, gate_b, norm_g):
    B, T, _ = q.shape
    n = T // GLA_CHUNK

    def heads(t, d):
        return t.astype(F32).reshape(B, n, GLA_CHUNK, GLA_HEADS, d).transpose(0, 3, 1, 2, 4)

    gk = jax.nn.log_sigmoid(gz.astype(F32) @ gate_w2.astype(F32) + gate_b.astype(F32)) / GLA_GATE_NORMALIZER
    qh = heads(q, GLA_DK) * (GLA_DK ** -0.5)
    kh = heads(k, GLA_DK)
    vh = heads(v, GLA_DV)
    G = jnp.cumsum(heads(gk, GLA_DK), axis=3)
    idx = jnp.arange(GLA_CHUNK)
    causal = idx[:, None] >= idx[None, :]
    gdiff = G[..., :, None, :] - G[..., None, :, :]
    decay = jnp.exp(jnp.where(causal[..., None], gdiff, -jnp.inf))
    A = jnp.einsum('bhnid,bhnjd,bhnijd->bhnij', qh, kh, decay)
    o_intra = jnp.einsum('bhnij,bhnjv->bhniv', A, vh)
    G_last = G[..., -1:, :]
    q_dec = qh * jnp.exp(G)
    k_dec = kh * jnp.exp(G_last - G)
    chunk_decay = jnp.exp(G_last[..., 0, :])

    def step(S, inp):
        qd, kd, vc, cd = inp
        o = jnp.einsum('bhid,bhdv->bhiv', qd, S)
        S = cd[..., None] * S + jnp.einsum('bhjd,bhjv->bhdv', kd, vc)
        return S, o

    S0 = jnp.zeros((B, GLA_HEADS, GLA_DK, GLA_DV), F32)
    mv = lambda t: jnp.moveaxis(t, 2, 0)
    _, o_inter = lax.scan(step, S0, (mv(q_dec), mv(k_dec), mv(vh), mv(chunk_decay)))
    o = o_intra + jnp.moveaxis(o_inter, 0, 2)
    o = o.transpose(0, 2, 3, 1, 4).reshape(B, T, GLA_HEADS, GLA_DV)
    o = rms_norm(o, norm_g) * jax.nn.silu(og.astype(F32)).reshape(B, T, GLA_HEADS, GLA_DV)
    return o.reshape(B, T, GLA_HEADS * GLA_DV).astype(q.dtype)


def diff_attention(q, k, v, cos, sin, lam_params, subln_g, layer_idx):
    B, T, _ = q.shape
    H, d = DIFF_HEADS, DIFF_DQK
    qh = apply_rope(q.reshape(B, T, H, 2, d), cos, sin).transpose(0, 2, 3, 1, 4) * (d ** -0.5)
    kh = apply_rope(k.reshape(B, T, H, 2, d), cos, sin).transpose(0, 2, 3, 1, 4)
    vh = v.astype(F32).reshape(B, T, H, DIFF_DV).transpose(0, 2, 1, 3)
    lam_init = 0.8 - 0.6 * math.exp(-0.3 * layer_idx)
    lp = lam_params.astype(F32)
    lam = jnp.exp(jnp.sum(lp[0] * lp[1])) - jnp.exp(jnp.sum(lp[2] * lp[3])) + lam_init
    outs = []
    for blk in range(T // DIFF_QBLOCK):
        start = blk * DIFF_QBLOCK
        end = start + DIFF_QBLOCK
        s = jnp.einsum('bhcqd,bhckd->bhcqk', qh[:, :, :, start:end], kh[:, :, :, :end])
        qpos = start + jnp.arange(DIFF_QBLOCK)
        kpos = jnp.arange(end)
        s = jnp.where(kpos[None, :] <= qpos[:, None], s, -jnp.inf)
        p = jax.nn.softmax(s, axis=-1)
        attn = p[:, :, 0] - lam * p[:, :, 1]
        outs.append(jnp.einsum('bhqk,bhkv->bhqv', attn, vh[:, :, :end]))
    o = jnp.concatenate(outs, axis=2)
    o = rms_norm(o, subln_g) * (1.0 - lam_init)
    return o.transpose(0, 2, 1, 3).reshape(B, T, H * DIFF_DV).astype(v.dtype)


def conformer_conv(a, b, conv_w, conv_b, ln_g, ln_b):
    u = a.astype(F32) * jax.nn.sigmoid(b.astype(F32))
    y = lax.conv_general_dilated(u, conv_w.astype(F32)[:, None, :], window_strides=(1,),
                                 padding=[(CONV_WIDTH - 1, 0)],
                                 dimension_numbers=('NWC', 'WIO', 'NWC'),
                                 feature_group_count=CONV_CH) + conv_b.astype(F32)
    y = layer_norm(y, ln_g, ln_b)
    return jax.nn.silu(y).astype(a.dtype)


def rwkv7_time_mix(cols, mu, w0, w2, a0, a2, g2, k_k, k_a, r_k, ln_g, ln_b):
    B, T, _ = cols.shape
    H, N = RWKV_HEADS, RWKV_HEAD
    xf = cols.astype(F32)
    prev = jnp.pad(xf, ((0, 0), (1, 0), (0, 0)))[:, :-1]
    xm = xf + (prev - xf) * mu.astype(F32)
    r, k, v, zw, za, zg = _split(xm, RWKV_SIZES)
    w = -jax.nn.softplus(-(w0.astype(F32) + jnp.tanh(zw) @ w2.astype(F32))) - 0.5
    decay = jnp.exp(-jnp.exp(w))
    a = jax.nn.sigmoid(a0.astype(F32) + za @ a2.astype(F32))
    g = jax.nn.sigmoid(zg) @ g2.astype(F32)
    hd = lambda t: t.reshape(B, T, H, N)
    kk = hd(k * k_k.astype(F32))
    kk = kk / jnp.maximum(jnp.sqrt(jnp.sum(kk * kk, axis=-1, keepdims=True)), 1e-12)
    k = k * (1.0 + (a - 1.0) * k_a.astype(F32))
    rh, wh, kh, vh, ah = hd(r), hd(decay), hd(k), hd(v), hd(a)

    def step(S, inp):
        rt, wt, kt, vt, kkt, at = inp
        sa = jnp.einsum('bhvk,bhk->bhv', S, -kkt)
        S = S * wt[:, :, None, :] + sa[..., None] * (kkt * at)[:, :, None, :] + vt[..., None] * kt[:, :, None, :]
        return S, jnp.einsum('bhvk,bhk->bhv', S, rt)

    seq_first = tuple(jnp.moveaxis(t, 1, 0) for t in (rh, wh, kh, vh, kk, ah))
    S0 = jnp.zeros((B, H, N, N), F32)
    _, y = lax.scan(step, S0, seq_first)
    y = jnp.moveaxis(y, 0, 1)
    y = layer_norm(y, ln_g.reshape(H, N), ln_b.reshape(H, N), eps=RWKV_GN_EPS)
    bonus = jnp.sum(rh * kh * r_k.astype(F32), axis=-1, keepdims=True) * vh
    y = (y + bonus).reshape(B, T, H * N) * g
    return y.astype(cols.dtype)


def swiglu(h, wg, wu, wd):
    return (jax.nn.silu(h @ wg) * (h @ wu)) @ wd


def moe_swiglu(h, router_w, router_b, wg, wu, wd):
    logits = h.astype(F32) @ router_w.astype(F32) + router_b.astype(F32)
    top_v, top_i = lax.top_k(logits, TOP_K)
    top_p = jax.nn.softmax(top_v, axis=-1)
    combine = jnp.sum(jax.nn.one_hot(top_i, N_EXPERTS, dtype=F32) * top_p[..., None], axis=-2)
    out = jnp.zeros(h.shape, F32)
    for e in range(N_EXPERTS):
        out = out + combine[..., e:e + 1] * swiglu(h, wg[e], wu[e], wd[e]).astype(F32)
    return out.astype(h.dtype)


def setup_inputs(seed: int = 0) -> dict:
    key = jax.random.key(seed)
    keys = jax.random.split(key, 48)
    counter = [0]

    def nk():
        kk = keys[counter[0]]
        counter[0] += 1
        return kk

    def nrm(shape, scale):
        return jax.random.normal(nk(), shape, F32) * scale

    def gain(shape):
        return 1.0 + nrm(shape, 0.02)

    D, L = D_MODEL, DEPTH
    W = BRANCH_WIDTH
    x = nrm((BATCH, SEQ, D), 1.0)
    c = nrm((BATCH, D), 1.0)
    positions = (jax.random.randint(nk(), (BATCH, 1), 0, 1024, dtype=jnp.int32)
                 + jnp.arange(SEQ, dtype=jnp.int32)[None, :])
    return {
        "x": x,
        "c": c,
        "positions": positions,
        "ada_w": nrm((L, D, 6 * D), 0.5 * D ** -0.5),
        "ada_b": nrm((L, 6 * D), 0.02),
        "norm_mix_pre": gain((L, D)),
        "norm_mix_post": gain((L, D)),
        "norm_ffn_pre": gain((L, D)),
        "norm_ffn_post": gain((L, D)),
        "w_in": nrm((L, D, N_IN), D ** -0.5),
        "gla_gate_w2": nrm((L, GLA_GATE_RANK, GLA_HEADS * GLA_DK), GLA_GATE_RANK ** -0.5),
        "gla_gate_b": 1.0 + nrm((L, GLA_HEADS * GLA_DK), 0.5),
        "gla_norm": gain((L, GLA_DV)),
        "diff_lambda": nrm((L, 4, DIFF_DQK), 0.1),
        "diff_subln": gain((L, DIFF_DV)),
        "conv_w": nrm((L, CONV_WIDTH, CONV_CH), CONV_WIDTH ** -0.5),
        "conv_b": nrm((L, CONV_CH), 0.02),
        "conv_ln_g": gain((L, CONV_CH)),
        "conv_ln_b": nrm((L, CONV_CH), 0.02),
        "rwkv_mu": jax.random.uniform(nk(), (L, RWKV_COLS), F32),
        "rwkv_w0": jax.random.uniform(nk(), (L, W), F32, minval=-3.0, maxval=1.0),
        "rwkv_w2": nrm((L, RWKV_DECAY_RANK, W), 0.5 * RWKV_DECAY_RANK ** -0.5),
        "rwkv_a0": nrm((L, W), 0.5),
        "rwkv_a2": nrm((L, RWKV_AAA_RANK, W), 0.5 * RWKV_AAA_RANK ** -0.5),
        "rwkv_g2": nrm((L, RWKV_GATE_RANK, W), RWKV_GATE_RANK ** -0.5),
        "rwkv_k_k": 0.85 + nrm((L, W), 0.05),
        "rwkv_k_a": 1.0 + nrm((L, W), 0.05),
        "rwkv_r_k": nrm((L, RWKV_HEADS, RWKV_HEAD), 0.1),
        "rwkv_ln_g": gain((L, W)),
        "rwkv_ln_b": nrm((L, W), 0.02),
        "w_branch": nrm((L, N_BRANCH, W, D), W ** -0.5),
        "w_out": nrm((L, D, D), D ** -0.5),
        "ffn_w_gate": nrm((N_DENSE, D, D_FF), D ** -0.5),
        "ffn_w_up": nrm((N_DENSE, D, D_FF), D ** -0.5),
        "ffn_w_down": nrm((N_DENSE, D_FF, D), D_FF ** -0.5),
        "router_w": nrm((N_MOE, D, N_EXPERTS), D ** -0.5),
        "router_b": nrm((N_MOE, N_EXPERTS), 0.01),
        "moe_w_gate": nrm((N_MOE, N_EXPERTS, D, D_EXPERT), D ** -0.5),
        "moe_w_up": nrm((N_MOE, N_EXPERTS, D, D_EXPERT), D ** -0.5),
        "moe_w_down": nrm((N_MOE, N_EXPERTS, D_EXPERT, D), D_EXPERT ** -0.5),
    }


def reference(x, c, positions, ada_w, ada_b, norm_mix_pre, norm_mix_post, norm_ffn_pre, norm_ffn_post,
              w_in, gla_gate_w2, gla_gate_b, gla_norm, diff_lambda, diff_subln,
              conv_w, conv_b, conv_ln_g, conv_ln_b,
              rwkv_mu, rwkv_w0, rwkv_w2, rwkv_a0, rwkv_a2, rwkv_g2, rwkv_k_k, rwkv_k_a, rwkv_r_k,
              rwkv_ln_g, rwkv_ln_b, w_branch, w_out,
              ffn_w_gate, ffn_w_up, ffn_w_down,
              router_w, router_b, moe_w_gate, moe_w_up, moe_w_down):
    B, T, D = x.shape
    cos, sin = rope_tables(positions)
    c_act = jax.nn.silu(c)
    for l in range(DEPTH):
        mod = (c_act @ ada_w[l] + ada_b[l])[:, None, :]
        sh_m, sc_m, gt_m, sh_f, sc_f, gt_f = jnp.split(mod, 6, axis=-1)

        h = rms_norm(x, norm_mix_pre[l]) * (1.0 + sc_m) + sh_m
        proj = h @ w_in[l]
        (g_q, g_k, g_v, g_o, g_z, d_q, d_k, d_v, c_a, c_b, r_cols, gate_cols) = _split(proj, IN_SIZES)
        o_gla = gla_mixer(g_q, g_k, g_v, g_o, g_z, gla_gate_w2[l], gla_gate_b[l], gla_norm[l])
        o_diff = diff_attention(d_q, d_k, d_v, cos, sin, diff_lambda[l], diff_subln[l], l)
        o_conv = conformer_conv(c_a, c_b, conv_w[l], conv_b[l], conv_ln_g[l], conv_ln_b[l])
        o_rwkv = rwkv7_time_mix(r_cols, rwkv_mu[l], rwkv_w0[l], rwkv_w2[l], rwkv_a0[l], rwkv_a2[l],
                                rwkv_g2[l], rwkv_k_k[l], rwkv_k_a[l], rwkv_r_k[l],
                                rwkv_ln_g[l], rwkv_ln_b[l])
        branches = jnp.stack([o_gla.astype(h.dtype), o_diff.astype(h.dtype),
                              o_conv.astype(h.dtype), o_rwkv.astype(h.dtype)], axis=2)
        branch_d = jnp.einsum('btgc,gcd->btgd', branches, w_branch[l])
        gates = jax.nn.sigmoid(gate_cols.reshape(B, T, N_BRANCH, D))
        merged = jnp.sum(gates * branch_d, axis=2)
        y = merged @ w_out[l]
        x = (x + gt_m * rms_norm(y, norm_mix_post[l])).astype(x.dtype)

        h = rms_norm(x, norm_ffn_pre[l]) * (1.0 + sc_f) + sh_f
        if l % 2 == 0:
            i = l // 2
            y = swiglu(h, ffn_w_gate[i], ffn_w_up[i], ffn_w_down[i])
        else:
            i = l // 2
            y = moe_swiglu(h, router_w[i], router_b[i], moe_w_gate[i], moe_w_up[i], moe_w_down[i])
        x = (x + gt_f * rms_norm(y, norm_ffn_post[l])).astype(x.dtype)
    return x
```

```python
import math
import numpy as np
from contextlib import ExitStack
import concourse.bass as bass
import concourse.mybir as mybir
from concourse.bass_utils import run_bass_kernel_spmd

F32 = mybir.dt.float32
BF16 = mybir.dt.bfloat16
I32 = mybir.dt.int32
AF = mybir.ActivationFunctionType
ALU = mybir.AluOpType
AX = mybir.AxisListType

T = 2048
D = 1024
NT = 16
NB = 4
DEPTH = 2
N_IN_A = 3088
D_FF = 2816
D_EXP = 3584
N_EXP = 8
RMS_EPS = 1e-6
LN_EPS = 1e-5
RWKV_GN_EPS = 64e-5
PI = math.pi


class Buf:
    __slots__ = ("w", "r", "excl", "name")

    def __init__(self, name="", excl=False):
        self.w = None
        self.r = {}
        self.excl = excl
        self.name = name


class Clock:
    def __init__(self, name, sem, eng=None):
        self.name = name
        self.sem = sem
        self.eng = eng
        self.count = 0
        self.seen = {}


class K:
    def __init__(self, nc, es, n_dma_sems=32):
        self.nc = nc
        self.E = {}
        for n, h in (("pe", nc.tensor), ("dve", nc.vector), ("act", nc.scalar), ("pool", nc.gpsimd), ("sp", nc.sync)):
            self.E[n] = Clock(n, es.enter_context(nc.semaphore("s_" + n)), h)
        self.dma_sems = [Clock("d%d" % i, es.enter_context(nc.semaphore("sd%d" % i))) for i in range(n_dma_sems)]
        self.dma_pool = {"sp": self.dma_sems[: n_dma_sems // 2], "pool": self.dma_sems[n_dma_sems // 2:]}
        self.dma_rr = {"sp": 0, "pool": 0}
        self.nins = 0
        self.nwait = 0

    def _deps(self, reads, writes):
        deps = {}

        def add(c, v):
            if deps.get(c, 0) < v:
                deps[c] = v

        for b in reads:
            if b.w is not None:
                add(*b.w)
            if b.excl:
                for c, v in b.r.items():
                    add(c, v)
        for b in writes:
            if b.w is not None:
                add(*b.w)
            for c, v in b.r.items():
                add(c, v)
        return deps

    def _wait(self, e, deps):
        for c, v in deps.items():
            if c is e and e.name == "pe":
                continue
            if e.seen.get(c, 0) >= v:
                continue
            e.eng.wait_ge(c.sem, v)
            e.seen[c] = v
            self.nwait += 1

    def _mark(self, c, v, reads, writes):
        for b in writes:
            b.w = (c, v)
            b.r = {}
        for b in reads:
            if b.excl:
                b.w = (c, v)
                b.r = {}
            else:
                b.r[c] = v

    def op(self, en, fn, reads=(), writes=()):
        e = self.E[en]
        self._wait(e, self._deps(reads, writes))
        ins = fn(e.eng)
        e.count += 1
        ins.then_inc(e.sem, 1)
        self.nins += 1
        self._mark(e, e.count, reads, writes)
        return ins

    def pe(self, fn, r=(), w=()):
        return self.op("pe", fn, r, w)

    def dve(self, fn, r=(), w=()):
        return self.op("dve", fn, r, w)

    def act(self, fn, r=(), w=()):
        return self.op("act", fn, r, w)

    def pool(self, fn, r=(), w=()):
        return self.op("pool", fn, r, w)

    def dma(self, qn, out, in_, reads=(), writes=(), **kw):
        q = self.E[qn]
        pool_ = self.dma_pool[qn]
        d = pool_[self.dma_rr[qn]]
        self.dma_rr[qn] = (self.dma_rr[qn] + 1) % len(pool_)
        deps = self._deps(reads, writes)
        if d.count > 0 and deps.get(d, 0) < d.count:
            deps[d] = d.count
        self._wait(q, deps)
        ins = q.eng.dma_start(out=out, in_=in_, **kw)
        d.count += 16
        ins.then_inc(d.sem, 16)
        self.nins += 1
        self._mark(d, d.count, reads, writes)
        return ins

    def barrier(self, engines=None):
        clocks = list(self.E.values()) + [d for d in self.dma_sems if d.count > 0]
        targets = [(c, c.count) for c in clocks if c.count > 0]
        for e in self.E.values():
            for c, v in targets:
                if e.seen.get(c, 0) < v:
                    e.eng.wait_ge(c.sem, v)
                    e.seen[c] = v
                    self.nwait += 1

    def finish(self, bufs):
        q = self.E["sp"]
        deps = {}
        for b in bufs:
            if b.w is not None:
                c, v = b.w
                if deps.get(c, 0) < v:
                    deps[c] = v
        self._wait(q, deps)


def _pcol(v, nchunk):
    return np.ascontiguousarray(np.asarray(v).reshape(nchunk, 128).T)


def _consts():
    p = np.arange(128)[:, None]
    f = np.arange(128)[None, :]
    c = {}
    c["ident"] = (p == f).astype(np.float32)
    c["m_le"] = (p <= f).astype(np.float32)
    c["m_lt"] = (p < f).astype(np.float32)
    c["m_gt"] = (p > f).astype(np.float32)
    c["blk64"] = ((p // 64) == (f // 64)).astype(np.float32)
    c["hsel"] = ((p // 64) == np.arange(2)[None, :]).astype(np.float32)
    j = (np.arange(128) % 32) % 16
    c["invf"] = (1.0 / (10000.0 ** (2.0 * j / 32.0))).astype(np.float32)[:, None]
    return c


class Prog:
    pass


def build_program(taps=(), upto=None):
    nc = bass.Bass("TRN2", target_bir_lowering=False)
    P = Prog()
    P.nc = nc
    shapes = {
        "x": ([T, D], F32),
        "c_t": ([128, 8], F32),
        "pos": ([1, T], I32),
        "ada_w": ([DEPTH, D, 6 * D], F32),
        "ada_bt": ([DEPTH, 128, 48], F32),
        "ada_b": ([DEPTH, 6 * D], F32),
        "nmp_t": ([DEPTH, 128, 8], F32),
        "nfp_t": ([DEPTH, 128, 8], F32),
        "norm_mix_post": ([DEPTH, D], F32),
        "norm_ffn_post": ([DEPTH, D], F32),
        "w_in_a": ([DEPTH, D, N_IN_A], F32),
        "w_gate": ([DEPTH, 8, 128, 4, 8, 128], F32),
        "gla_gate_w2": ([DEPTH, 16, 128], F32),
        "gla_gate_b": ([DEPTH, 128], F32),
        "gla_norm": ([DEPTH, 64], F32),
        "diff_lambda": ([DEPTH, 128], F32),
        "diff_subln_t": ([DEPTH, 128, 1], F32),
        "conv_wt": ([DEPTH, 128, 2, 31], F32),
        "conv_bt": ([DEPTH, 128, 2], F32),
        "conv_lg_t": ([DEPTH, 128, 2], F32),
        "conv_lb_t": ([DEPTH, 128, 2], F32),
        "rwkv_mu_t": ([DEPTH, 128, 8], F32),
        "rwkv_w0": ([DEPTH, 256], F32),
        "rwkv_w2": ([DEPTH, 64, 256], F32),
        "rwkv_a0_t": ([DEPTH, 128, 2], F32),
        "rwkv_a2": ([DEPTH, 64, 256], F32),
        "rwkv_g2": ([DEPTH, 128, 256], F32),
        "rwkv_kk_t": ([DEPTH, 128, 2], F32),
        "rwkv_ka_t": ([DEPTH, 128, 2], F32),
        "rwkv_rk_t": ([DEPTH, 128, 2], F32),
        "rwkv_ln_g": ([DEPTH, 256], F32),
        "rwkv_ln_b": ([DEPTH, 256], F32),
        "w_branch": ([DEPTH, 4, 256, D], F32),
        "w_out": ([DEPTH, D, D], F32),
        "ffn_wg": ([1, 22, 128, 8, 128], F32),
        "ffn_wu": ([1, 22, 128, 8, 128], F32),
        "ffn_wd": ([1, D_FF, D], F32),
        "router_wt": ([1, 128, 8, 8], F32),
        "router_b": ([1, 8], F32),
        "moe_wg": ([1, N_EXP, 28, 128, 8, 128], F32),
        "moe_wu": ([1, N_EXP, 28, 128, 8, 128], F32),
        "moe_wd": ([1, N_EXP, D_EXP, D], F32),
    }
    for n_, a_ in _consts().items():
        shapes[n_] = (list(a_.shape), F32)

    class LazyDr(dict):
        def __missing__(self, name):
            shp, dt = shapes[name]
            ap = nc.dram_tensor(name, list(shp), dt, kind="ExternalInput").ap()
            self[name] = ap
            return ap

    dr = LazyDr()
    y_out = nc.dram_tensor("y", [T, D], F32, kind="ExternalOutput").ap()
    xs = [nc.dram_tensor("xs%d" % i, [T, D], F32, kind="Internal").ap() for i in range(2)]
    rope_dram = nc.dram_tensor("rope_tab", [2, 128, T], F32, kind="Internal").ap()
    ropeb = Buf("rope")
    tap_out = {}
    P.dr = dr

    with ExitStack() as es:
        k = K(nc, es)
        P.k = k

        sbn = [0]

        def sb(stack, name, shape, dt=F32):
            sbn[0] += 1
            return stack.enter_context(nc.sbuf_tensor("sb%d_%s" % (sbn[0], name), list(shape), dt))

        PS = [es.enter_context(nc.psum_tensor("ps%d" % i, [128, 512], F32)) for i in range(8)]
        PSb = [Buf("ps%d" % i, excl=True) for i in range(8)]

        def tap(name, ap, buf, shape):
            if name not in taps:
                return
            t = nc.dram_tensor("tap_" + name, list(shape), ap.dtype, kind="ExternalOutput").ap()
            tap_out[name] = t
            b = Buf("tap")
            k.dma("sp", t, ap, reads=[buf], writes=[b])
            P.tapbufs.append(b)

        P.tapbufs = []

        IDF = sb(es, "idf", [128, 128])
        MLE = sb(es, "mle", [128, 128])
        MLT = sb(es, "mlt", [128, 128])
        MGT = sb(es, "mgt", [128, 128])
        BLK = sb(es, "blk", [128, 128])
        HSEL = sb(es, "hsel", [128, 2])
        INVF = sb(es, "invf", [128, 1])
        ONES = sb(es, "ones", [128, 128])
        ONESB = sb(es, "onesb", [128, 128], BF16)
        MLEB = sb(es, "mleb", [128, 128], BF16)
        CT = sb(es, "ct", [128, 8])
        CA = sb(es, "ca", [128, 8], BF16)
        CAR = sb(es, "car", [128, 8, 128], BF16)
        HT = sb(es, "ht", [128, 8, T], BF16)
        cb = Buf("consts")
        HTb = Buf("ht")
        for nm, t_ in (("ident", IDF), ("m_le", MLE), ("m_lt", MLT), ("m_gt", MGT), ("blk64", BLK), ("hsel", HSEL), ("invf", INVF), ("c_t", CT)):
            k.dma("sp", t_[:], dr[nm][:], writes=[cb])
        k.pool(lambda e: e.memset(ONES[:], 1.0), w=[cb])
        k.pool(lambda e: e.memset(ONESB[:], 1.0), w=[cb])
        k.dve(lambda e: e.tensor_copy(out=MLEB[:], in_=MLE[:]), r=[cb], w=[cb])
        k.act(lambda e: e.activation(out=CA[:], in_=CT[:], func=AF.Silu), r=[cb], w=[cb])
        k.dve(lambda e: e.tensor_copy(out=CAR[:], in_=CA[:].unsqueeze(2).to_broadcast([128, 8, 128])), r=[cb], w=[cb])
        k.barrier()

        def win(l, c0, n):
            return dr["w_in_a"][l].rearrange("(kc p) n -> p kc n", p=128)[:, :, c0:c0 + n]

        def mm(ps_ap, lhsT, rhs, start, stop, r, w, tp=None, sw=False):
            if sw:
                pe_ = k.E["pe"]
                if pe_.count > 0 and pe_.seen.get(pe_, 0) < pe_.count:
                    pe_.eng.wait_ge(pe_.sem, pe_.count)
                    pe_.seen[pe_] = pe_.count
            if tp is None:
                return k.pe(lambda e: e.matmul(ps_ap, lhsT, rhs, start=start, stop=stop), r=r, w=w)
            return k.pe(lambda e: e.matmul(ps_ap, lhsT, rhs, start=start, stop=stop, tile_position=tp), r=r, w=w)

        def phase_mod(l, lay, MODT, GTM, GTF, modb):
            with ExitStack() as ph:
                WA = [sb(ph, "wa%d" % i, [128, 8, 512], BF16) for i in range(2)]
                WAb = [Buf(), Buf()]
                ABT = sb(ph, "abt", [128, 48])
                ABB = sb(ph, "abb", [128, 512])
                GPB = sb(ph, "gpb", [128, 512])
                tb_ = Buf()
                k.dma("sp", ABT[:], dr["ada_bt"][l], writes=[tb_])
                awv = dr["ada_w"][l].rearrange("(kc p) n -> p kc n", p=128)
                for grp in range(12):
                    w_ = WA[grp % 2]
                    wb_ = WAb[grp % 2]
                    k.dma("pool", w_[:], awv[:, :, grp * 512:(grp + 1) * 512], writes=[wb_])
                    if grp in (4, 5, 10, 11):
                        ps = PS[1 + grp % 2]
                        psb = PSb[1 + grp % 2]
                        for kc in range(8):
                            mm(ps[:], CAR[:, kc, :], w_[:, kc, :], kc == 0, kc == 7, [wb_, cb], [psb])
                        dst = GTM if grp < 6 else GTF
                        half = grp % 2
                        gsrc = dr["norm_mix_post"] if grp < 6 else dr["norm_ffn_post"]
                        k.dma("sp", ABB[:], dr["ada_b"][l:l + 1, grp * 512:(grp + 1) * 512].partition_broadcast(128), writes=[tb_])
                        k.dma("sp", GPB[:], gsrc[l:l + 1, half * 512:(half + 1) * 512].partition_broadcast(128), writes=[tb_])
                        k.dve(lambda e: e.tensor_tensor(out=ABB[:], in0=ps[:], in1=ABB[:], op=ALU.add), r=[psb, tb_], w=[tb_])
                        k.dve(lambda e: e.tensor_tensor(out=dst[:, half * 512:(half + 1) * 512], in0=ABB[:], in1=GPB[:], op=ALU.mult), r=[tb_], w=[modb])
                    else:
                        for jj in range(4):
                            col = grp * 4 + jj
                            for kc in range(8):
                                mm(PS[0][:, col:col + 1], w_[:, kc, jj * 128:(jj + 1) * 128], CA[:, kc:kc + 1], kc == 0, kc == 7, [wb_, cb], [PSb[0]])
                k.pool(lambda e: e.memset(MODT[:], 0.0), w=[modb])
                for c0 in (0, 24):
                    k.dve(lambda e: e.tensor_tensor(out=MODT[:, c0:c0 + 16], in0=PS[0][:, c0:c0 + 16], in1=ABT[:, c0:c0 + 16], op=ALU.add), r=[PSb[0], tb_], w=[modb])
                k.barrier()

        def phase_prenorm(xsrc, xsrcb, gT_ap, sc0, sh0, MODT, modb, router=None):
            with ExitStack() as ph:
                GT_ = sb(ph, "gT", [128, 8])
                A_ = sb(ph, "A_", [128, 8])
                XT = [sb(ph, "xt%d" % i, [128, D]) for i in range(2)]
                XTb = [Buf(), Buf()]
                XN = [sb(ph, "xn%d" % i, [128, D]) for i in range(2)]
                XNb = [Buf(), Buf()]
                JK = sb(ph, "jk", [128, D], BF16)
                SS = sb(ph, "ss", [128, NT])
                RS = sb(ph, "rs", [128, NT])
                H32 = [sb(ph, "h32%d" % i, [128, 8, 128]) for i in range(2)] if router is not None else None
                H32b = [Buf(), Buf()]
                pb = Buf()
                jb = Buf()
                ssm = Buf()
                k.pool(lambda e: e.memset(SS[:], 0.0), w=[ssm])
                k.dma("sp", GT_[:], gT_ap, writes=[pb])
                k.dve(lambda e: e.scalar_tensor_tensor(out=A_[:], in0=MODT[:, sc0:sc0 + 8], scalar=1.0, in1=GT_[:], op0=ALU.add, op1=ALU.mult), r=[modb, pb], w=[pb])
                stb = [Buf(), Buf()]
                htw = [Buf() for _ in range(8)]
                h32w = [[Buf() for _ in range(8)] for _ in range(2)]
                def stage_a(tt):
                        s = tt % 2
                        xt = XT[s]
                        k.dma("sp", xt[:], xsrc[tt * 128:(tt + 1) * 128, :], reads=[xsrcb[tt]], writes=[XTb[s]])
                        k.act(lambda e: e.activation(out=JK[:], in_=xt[:], func=AF.Square, accum_out=SS[:, tt:tt + 1]), r=[XTb[s], ssm], w=[jb, stb[s]])
                        k.act(lambda e: e.activation(out=RS[:, tt:tt + 1], in_=SS[:, tt:tt + 1], func=AF.Sqrt, scale=1.0 / D, bias=RMS_EPS), r=[stb[s]], w=[stb[s]])
                        k.dve(lambda e: e.reciprocal(out=RS[:, tt:tt + 1], in_=RS[:, tt:tt + 1]), r=[stb[s]], w=[stb[s]])
                        k.dve(lambda e: e.tensor_scalar(out=XN[s][:], in0=xt[:], scalar1=RS[:, tt:tt + 1], scalar2=None, op0=ALU.mult), r=[XTb[s], stb[s]], w=[XNb[s]])

                def stage_b(tt):
                        s = tt % 2
                        for half in range(2):
                            ps = PS[2 + half + 2 * s]
                            psb = PSb[2 + half + 2 * s]
                            for q in range(4):
                                kc = half * 4 + q
                                k.pe(lambda e: e.transpose(out=ps[:, q * 128:(q + 1) * 128], in_=XN[s][:, kc * 128:(kc + 1) * 128], identity=IDF[:]), r=[XNb[s], cb], w=[psb])
                            for q in range(4):
                                kc = half * 4 + q
                                if router is None:
                                    dst = HT[:, kc, tt * 128:(tt + 1) * 128]
                                    wr = [htw[kc]]
                                else:
                                    dst = H32[s][:, kc, :]
                                    wr = [h32w[s][kc]]
                                if q % 2 == 0:
                                    k.act(lambda e: e.activation(out=dst, in_=ps[:, q * 128:(q + 1) * 128], func=AF.Identity, scale=A_[:, kc:kc + 1], bias=MODT[:, sh0 + kc:sh0 + kc + 1]), r=[psb, pb, modb], w=wr)
                                else:
                                    k.dve(lambda e: e.tensor_scalar(out=dst, in0=ps[:, q * 128:(q + 1) * 128], scalar1=A_[:, kc:kc + 1], scalar2=MODT[:, sh0 + kc:sh0 + kc + 1], op0=ALU.mult, op1=ALU.add), r=[psb, pb, modb], w=wr)
                        if router is not None:
                            k.dve(lambda e: e.tensor_copy(out=HT[:, :, tt * 128:(tt + 1) * 128], in_=H32[s][:]), r=h32w[s], w=[htw[0], H32b[s]])
                            router(tt, H32[s], H32b[s])

                for step in range(NT + 1):
                    if step < NT:
                        stage_a(step)
                    if step >= 1:
                        stage_b(step - 1)
                k.barrier()

        def conv_setup(l, ph):
            WC = sb(ph, "wc", [128, 8, 512], BF16)
            U = sb(ph, "cu", [128, 2, T + 32])
            Y = sb(ph, "cy", [128, 2, T])
            CW = sb(ph, "cw", [128, 2, 31])
            CBt = sb(ph, "cbt", [128, 2])
            LG = sb(ph, "clg", [128, 2])
            LB = sb(ph, "clb", [128, 2])
            OND = sb(ph, "ond", [128, 128])
            SG = [sb(ph, "csg%d" % i, [128, 512]) for i in range(2)]
            SQ = sb(ph, "csq", [128, 2, 512])
            MEAN = sb(ph, "cmean", [128, 512])
            VAR = sb(ph, "cvar", [128, 512])
            TT_ = sb(ph, "ctt", [128, 512])
            wb_, ub, yb, pb, sgb, tb2 = Buf(), Buf(), Buf(), Buf(), [Buf(), Buf()], Buf()
            k.dma("pool", WC[:], win(l, 1552, 512), writes=[wb_])
            k.dma("sp", CW[:], dr["conv_wt"][l], writes=[pb])
            k.dma("sp", CBt[:], dr["conv_bt"][l], writes=[pb])
            k.dma("sp", LG[:], dr["conv_lg_t"][l], writes=[pb])
            k.dma("sp", LB[:], dr["conv_lb_t"][l], writes=[pb])
            k.pool(lambda e: e.memset(U[:, :, 0:32], 0.0), w=[ub])
            k.pool(lambda e: e.memset(OND[:], 1.0 / 256.0), w=[pb])
            pa, pab, pg, pgb = PS[6], PSb[6], PS[7], PSb[7]

            def gen(BR, BRb):
                i = 0
                for c in range(2):
                    for tb in range(NB):
                        s = i % 2
                        i += 1
                        for kc in range(8):
                            mm(pa[:], WC[:, kc, c * 128:(c + 1) * 128], HT[:, kc, tb * 512:(tb + 1) * 512], kc == 0, kc == 7, [wb_, HTb], [pab])
                        for kc in range(8):
                            mm(pg[:], WC[:, kc, 256 + c * 128:256 + (c + 1) * 128], HT[:, kc, tb * 512:(tb + 1) * 512], kc == 0, kc == 7, [wb_, HTb], [pgb])
                        k.act(lambda e: e.activation(out=SG[s][:], in_=pg[:], func=AF.Sigmoid), r=[pgb], w=[sgb[s]])
                        k.dve(lambda e: e.tensor_tensor(out=U[:, c, 32 + tb * 512:32 + (tb + 1) * 512], in0=pa[:], in1=SG[s][:], op=ALU.mult), r=[pab, sgb[s]], w=[ub])
                        yield
                for c in range(2):
                    k.dve(lambda e: e.tensor_scalar(out=Y[:, c, :], in0=U[:, c, 2:2 + T], scalar1=CW[:, c, 0:1], scalar2=CBt[:, c:c + 1], op0=ALU.mult, op1=ALU.add), r=[ub, pb], w=[yb])
                    yield
                    for j in range(1, 31):
                        k.dve(lambda e: e.scalar_tensor_tensor(out=Y[:, c, :], in0=U[:, c, 2 + j:2 + j + T], scalar=CW[:, c, j:j + 1], in1=Y[:, c, :], op0=ALU.mult, op1=ALU.add), r=[ub, pb, yb], w=[yb])
                        yield
                for tb in range(NB):
                    sl = slice(tb * 512, (tb + 1) * 512)
                    k.act(lambda e: e.activation(out=SQ[:], in_=Y[:, :, sl], func=AF.Square), r=[yb], w=[tb2])
                    for c in range(2):
                        mm(pa[:], OND[:], Y[:, c, sl], c == 0, c == 1, [pb, yb], [pab])
                    for c in range(2):
                        mm(pg[:], OND[:], SQ[:, c, :], c == 0, c == 1, [pb, tb2], [pgb])
                    k.act(lambda e: e.activation(out=MEAN[:], in_=pa[:], func=AF.Copy), r=[pab], w=[tb2])
                    yield
                    k.dve(lambda e: e.tensor_tensor(out=VAR[:], in0=MEAN[:], in1=MEAN[:], op=ALU.mult), r=[tb2], w=[tb2])
                    k.dve(lambda e: e.tensor_tensor(out=VAR[:], in0=pg[:], in1=VAR[:], op=ALU.subtract), r=[pgb, tb2], w=[tb2])
                    k.act(lambda e: e.activation(out=VAR[:], in_=VAR[:], func=AF.Sqrt, bias=LN_EPS), r=[tb2], w=[tb2])
                    k.dve(lambda e: e.reciprocal(out=VAR[:], in_=VAR[:]), r=[tb2], w=[tb2])
                    yield
                    for c in range(2):
                        k.dve(lambda e: e.tensor_tensor(out=TT_[:], in0=Y[:, c, sl], in1=MEAN[:], op=ALU.subtract), r=[yb, tb2], w=[tb2])
                        k.dve(lambda e: e.tensor_tensor(out=TT_[:], in0=TT_[:], in1=VAR[:], op=ALU.mult), r=[tb2], w=[tb2])
                        k.act(lambda e: e.activation(out=BR[:, 4 + c, sl], in_=TT_[:], func=AF.Silu, scale=LG[:, c:c + 1], bias=LB[:, c:c + 1]), r=[tb2, pb], w=[BRb])
                        yield

            return gen

        def phase_conv(l, BR, BRb):
            with ExitStack() as ph:
                g_ = conv_setup(l, ph)(BR, BRb)
                for _ in g_:
                    pass
                k.barrier()

        def phase_diff(l, BR, BRb, pump=lambda n: None):
            lam_init = 0.8 - 0.6 * math.exp(-0.3 * l)
            PI_S = 3.1415925
            with ExitStack() as ph:
                QT = sb(ph, "dqt", [128, 2, T], BF16)
                KT = sb(ph, "dkt", [128, 2, T], BF16)
                VA = sb(ph, "dva", [128, NT, 4, 65], BF16)
                LP = sb(ph, "dlp", [128, 128])
                PR = sb(ph, "dpr", [128, 2, 32])
                SR = sb(ph, "dsr", [128, 2])
                COEF = sb(ph, "dcoef", [128, 8])
                SUB = sb(ph, "dsub", [128, 1])
                qtb, ktb, vab, pb = Buf(), Buf(), Buf(), Buf()
                k.dma("sp", LP[:], dr["diff_lambda"][l:l + 1, :].partition_broadcast(128), writes=[pb])
                k.dma("sp", SUB[:], dr["diff_subln_t"][l], writes=[pb])
                LPv = LP[:].rearrange("p (a b d) -> p a b d", a=2, b=2)
                k.dve(lambda e: e.tensor_tensor(out=PR[:], in0=LPv[:, :, 0, :], in1=LPv[:, :, 1, :], op=ALU.mult), r=[pb], w=[pb])
                k.dve(lambda e: e.tensor_reduce(out=SR[:], in_=PR[:], axis=AX.X, op=ALU.add), r=[pb], w=[pb])
                k.act(lambda e: e.activation(out=SR[:], in_=SR[:], func=AF.Exp), r=[pb], w=[pb])
                k.pool(lambda e: e.memset(COEF[:], 1.0), w=[pb])
                k.dve(lambda e: e.tensor_tensor(out=SR[:, 0:1], in0=SR[:, 1:2], in1=SR[:, 0:1], op=ALU.subtract), r=[pb], w=[pb])
                COEFv = COEF[:].rearrange("p (h c) -> p h c", c=2)
                k.dve(lambda e: e.tensor_scalar(out=COEFv[:, :, 1], in0=SR[:, 0:1].to_broadcast([128, 4]), scalar1=-lam_init, scalar2=None, op0=ALU.add), r=[pb], w=[pb])
                with ExitStack() as p1:
                    COS = sb(p1, "dcos", [128, T])
                    SIN = sb(p1, "dsin", [128, T])
                    W2 = sb(p1, "dw2", [128, 8, 512], BF16)
                    T1 = [sb(p1, "dt1%d" % i_, [128, 512]) for i_ in range(2)]
                    T2 = [sb(p1, "dt2%d" % i_, [128, 512]) for i_ in range(2)]
                    tabb, w2b, t1b, t2b = Buf(), Buf(), [Buf(), Buf()], [Buf(), Buf()]
                    if l == 0:
                        with ExitStack() as p0:
                            POSI = sb(p0, "dposi", [128, 1024], I32)
                            ANG = sb(p0, "dang", [128, 1024])
                            TQ = sb(p0, "dtq", [128, 1024])
                            TI = sb(p0, "dti", [128, 1024], I32)
                            ab = Buf()
                            for tb in range(2):
                                sl = slice(tb * 1024, (tb + 1) * 1024)
                                k.dma("sp", POSI[:], dr["pos"][0:1, sl].partition_broadcast(128), writes=[ab])
                                k.dve(lambda e: e.tensor_copy(out=ANG[:], in_=POSI[:]), r=[ab], w=[ab])
                                k.dve(lambda e: e.tensor_scalar(out=ANG[:], in0=ANG[:], scalar1=INVF[:, 0:1], scalar2=None, op0=ALU.mult), r=[ab, cb], w=[ab])
                                for dst, shift in ((SIN, 0.0), (COS, PI / 2)):
                                    k.dve(lambda e: e.tensor_scalar(out=TQ[:], in0=ANG[:], scalar1=1.0 / (2 * PI), scalar2=shift / (2 * PI), op0=ALU.mult, op1=ALU.add), r=[ab], w=[ab])
                                    k.dve(lambda e: e.tensor_copy(out=TI[:], in_=TQ[:]), r=[ab], w=[ab])
                                    k.dve(lambda e: e.tensor_copy(out=TQ[:], in_=TI[:]), r=[ab], w=[ab])
                                    k.dve(lambda e: e.scalar_tensor_tensor(out=TQ[:], in0=TQ[:], scalar=-2 * PI, in1=ANG[:], op0=ALU.mult, op1=ALU.add), r=[ab], w=[ab])
                                    k.dve(lambda e: e.tensor_scalar(out=TQ[:], in0=TQ[:], scalar1=shift, scalar2=-PI_S, op0=ALU.add, op1=ALU.max), r=[ab], w=[ab])
                                    k.dve(lambda e: e.tensor_scalar(out=TQ[:], in0=TQ[:], scalar1=PI_S, scalar2=None, op0=ALU.min), r=[ab], w=[ab])
                                    k.act(lambda e: e.activation(out=dst[:, sl], in_=TQ[:], func=AF.Sin), r=[ab], w=[tabb])
                                pump(4)
                            k.dma("sp", rope_dram[0], COS[:], reads=[tabb], writes=[ropeb])
                            k.dma("sp", rope_dram[1], SIN[:], reads=[tabb], writes=[ropeb])
                            k.barrier()
                    else:
                        k.dma("sp", COS[:], rope_dram[0], reads=[ropeb], writes=[tabb])
                        k.dma("sp", SIN[:], rope_dram[1], reads=[ropeb], writes=[tabb])
                        pump(8)
                        k.barrier()
                    i_ = 0
                    for off, dst, dstb in ((784, QT, qtb), (1040, KT, ktb)):
                        k.dma("pool", W2[:, :, 0:256], win(l, off, 256), writes=[w2b])
                        Wv = W2[:, :, 0:256].rearrange("p k (g t j) -> p k g t j", g=8, t=2)
                        Rv = W2[:, :, 256:512].rearrange("p k (g t j) -> p k g t j", g=8, t=2)
                        k.dve(lambda e: e.tensor_scalar(out=Rv[:, :, :, 0, :], in0=Wv[:, :, :, 1, :], scalar1=-1.0, scalar2=None, op0=ALU.mult), r=[w2b], w=[w2b])
                        k.dve(lambda e: e.tensor_copy(out=Rv[:, :, :, 1, :], in_=Wv[:, :, :, 0, :]), r=[w2b], w=[w2b])
                        for c in range(2):
                            for tb in range(NB):
                                s = i_ % 2
                                i_ += 1
                                sl = slice(tb * 512, (tb + 1) * 512)
                                pa, pab = PS[2 * s], PSb[2 * s]
                                pr_, prb = PS[2 * s + 1], PSb[2 * s + 1]
                                for kc in range(8):
                                    mm(pa[:], W2[:, kc, c * 128:(c + 1) * 128], HT[:, kc, sl], kc == 0, kc == 7, [w2b, HTb], [pab])
                                for kc in range(8):
                                    mm(pr_[:], W2[:, kc, 256 + c * 128:256 + (c + 1) * 128], HT[:, kc, sl], kc == 0, kc == 7, [w2b, HTb], [prb])
                                k.dve(lambda e: e.tensor_tensor(out=T1[s][:], in0=pa[:], in1=COS[:, sl], op=ALU.mult), r=[pab, tabb], w=[t1b[s]])
                                k.dve(lambda e: e.tensor_tensor(out=T2[s][:], in0=pr_[:], in1=SIN[:, sl], op=ALU.mult), r=[prb, tabb], w=[t2b[s]])
                                k.pool(lambda e: e.tensor_tensor(out=dst[:, c, sl], in0=T1[s][:], in1=T2[s][:], op=ALU.add), r=[t1b[s], t2b[s]], w=[dstb])
                    k.dma("pool", W2[:, :, 0:256], win(l, 1296, 256), writes=[w2b])
                    k.pool(lambda e: e.memset(VA[:, :, :, 64:65], 1.0), w=[vab])
                    for tt in range(NT):
                        s = tt % 2
                        ps, psb = PS[4 + s], PSb[4 + s]
                        for kc in range(8):
                            mm(ps[:, 0:256], HT[:, kc, tt * 128:(tt + 1) * 128], W2[:, kc, 0:256], kc == 0, kc == 7, [w2b, HTb], [psb])
                        k.act(lambda e: e.activation(out=VA[:, tt, :, 0:64], in_=ps[:, 0:256].rearrange("p (h d) -> p h d", h=4), func=AF.Copy), r=[psb], w=[vab])
                    k.barrier()
                tap("dqt%d" % l, QT[:], qtb, [128, 2, T])
                tap("dkt%d" % l, KT[:], ktb, [128, 2, T])
                with ExitStack() as p2:
                    PT = sb(p2, "dpt", [128, 16, 512], BF16)
                    ptb = [Buf() for _ in range(16)]
                    OACC2 = [sb(p2, "doacc%d" % i_, [128, 4, 8, 65]) for i_ in range(2)]
                    ob2 = [Buf(), Buf()]
                    RSs = sb(p2, "drs", [128, 4, 8])
                    ON = sb(p2, "don", [128, 4, 8, 64])
                    OD = sb(p2, "dod", [128, 4, 4, 64])
                    SQd = sb(p2, "dsq", [128, 4, 4, 64])
                    SSd = sb(p2, "dss", [128, 4, 4])
                    fb = Buf()
                    sc = 32.0 ** -0.5
                    rot = 0
                    for qb in range(NB):
                        OACC, ob = OACC2[qb % 2], ob2[qb % 2]
                        for g in range(8):
                            c, gl, h = g // 4, g % 4, g // 2
                            nk = 4 * qb + 4
                            for kt in range(nk):
                                r_ = kt - 4 * qb
                                n0 = max(0, r_) * 128
                                ps, psb = PS[rot % 3], PSb[rot % 3]
                                rot += 1
                                mm(ps[:, n0:512], KT[32 * gl:32 * gl + 32, c, kt * 128:(kt + 1) * 128], QT[32 * gl:32 * gl + 32, c, qb * 512 + n0:(qb + 1) * 512], True, True, [ktb, qtb], [psb], tp=(32 * gl, 0))
                                k.act(lambda e: e.activation(out=PT[:, kt, n0:512], in_=ps[:, n0:512], func=AF.Exp, scale=sc), r=[psb], w=[ptb[kt]])
                                if r_ >= 0:
                                    k.pool(lambda e: e.tensor_tensor(out=PT[:, kt, n0:n0 + 128], in0=PT[:, kt, n0:n0 + 128], in1=MLEB[:], op=ALU.mult), r=[ptb[kt], cb], w=[ptb[kt]])
                            po, pob = PS[3 + (g % 2)], PSb[3 + (g % 2)]
                            for qi in range(4):
                                last = 4 * qb + qi
                                for kt in range(last + 1):
                                    mm(po[:, qi * 65:(qi + 1) * 65], PT[:, kt, qi * 128:(qi + 1) * 128], VA[:, kt, h, :], kt == 0, kt == last, [ptb[kt], vab], [pob])
                            k.act(lambda e: e.activation(out=OACC[:, :, g, :], in_=po[:, 0:260].rearrange("p (q e) -> p q e", q=4), func=AF.Copy), r=[pob], w=[ob])
                            pump(3)
                        k.dve(lambda e: e.reciprocal(out=RSs[:], in_=OACC[:, :, :, 64]), r=[ob], w=[fb])
                        k.dve(lambda e: e.tensor_tensor(out=RSs[:], in0=RSs[:], in1=COEF[:].unsqueeze(1).to_broadcast([128, 4, 8]), op=ALU.mult), r=[fb, pb], w=[fb])
                        k.dve(lambda e: e.tensor_tensor(out=ON[:], in0=OACC[:, :, :, 0:64], in1=RSs[:].unsqueeze(3).to_broadcast([128, 4, 8, 64]), op=ALU.mult), r=[ob, fb], w=[fb])
                        ONv = ON[:].rearrange("p q (h c) d -> p q h c d", c=2)
                        k.dve(lambda e: e.tensor_tensor(out=OD[:], in0=ONv[:, :, :, 0, :], in1=ONv[:, :, :, 1, :], op=ALU.add), r=[fb], w=[fb])
                        k.dve(lambda e: e.tensor_tensor(out=SQd[:], in0=OD[:], in1=OD[:], op=ALU.mult), r=[fb], w=[fb])
                        k.dve(lambda e: e.tensor_reduce(out=SSd[:], in_=SQd[:], axis=AX.X, op=ALU.add), r=[fb], w=[fb])
                        k.act(lambda e: e.activation(out=SSd[:], in_=SSd[:], func=AF.Sqrt, scale=1.0 / 64, bias=RMS_EPS), r=[fb], w=[fb])
                        k.dve(lambda e: e.reciprocal(out=SSd[:], in_=SSd[:]), r=[fb], w=[fb])
                        k.dve(lambda e: e.tensor_tensor(out=OD[:], in0=OD[:], in1=SSd[:].unsqueeze(3).to_broadcast([128, 4, 4, 64]), op=ALU.mult), r=[fb], w=[fb])
                        for qi in range(4):
                            tt = 4 * qb + qi
                            ps, psb = PS[5], PSb[5]
                            ODf = OD[:, qi].rearrange("p h d -> p (h d)")
                            for cc in range(2):
                                k.pe(lambda e: e.transpose(out=ps[:, cc * 128:(cc + 1) * 128], in_=ODf[:, cc * 128:(cc + 1) * 128], identity=IDF[:]), r=[fb, cb], w=[psb])
                            k.dve(lambda e: e.tensor_scalar(out=BR[:, 2:4, tt * 128:(tt + 1) * 128], in0=ps[:, 0:256].rearrange("p (c n) -> p c n", c=2), scalar1=SUB[:, 0:1], scalar2=(1.0 - lam_init), op0=ALU.mult, op1=ALU.mult), r=[psb, pb], w=[BRb])
                    pump(100000)
                    k.barrier()

        def phase_gla(l, BR, BRb):
            with ExitStack() as ph:
                WG = sb(ph, "gw", [128, 8, 784], BF16)
                W2F = sb(ph, "gw2f", [17, 128])
                GN = sb(ph, "ggn", [128, 64])
                S32 = sb(ph, "gs32", [128, 64])
                SBF = sb(ph, "gsbf", [128, 64], BF16)
                GZT = sb(ph, "ggzt", [17, 128])
                E1 = sb(ph, "ge1", [128, 128])
                LL = sb(ph, "gll", [128, 128])
                EP = sb(ph, "gep", [128, 128])
                EM = sb(ph, "gem", [128, 128])
                E3 = sb(ph, "ge3", [128, 128])
                QS = sb(ph, "gqs", [128, 128], BF16)
                KS = sb(ph, "gks", [128, 128], BF16)
                KH = sb(ph, "gkh", [128, 128], BF16)
                V = sb(ph, "gv", [128, 256], BF16)
                SOG = sb(ph, "gsog", [128, 256])
                AM = sb(ph, "gam", [128, 4, 128], BF16)
                O = sb(ph, "go", [128, 4, 64])
                SQ = sb(ph, "gsq", [128, 4, 64])
                SS = sb(ph, "gss", [128, 4])
                wb_, pb, sb_, zb, eb, qb_, vb_, ob = Buf(), Buf(), Buf(), Buf(), Buf(), Buf(), Buf(), Buf()
                k.dma("pool", WG[:], win(l, 0, 784), writes=[wb_])
                k.dma("sp", W2F[0:16, :], dr["gla_gate_w2"][l], writes=[pb])
                k.dma("sp", W2F[16:17, :], dr["gla_gate_b"][l:l + 1, :], writes=[pb])
                k.dma("sp", GN[:], dr["gla_norm"][l:l + 1, :].partition_broadcast(128), writes=[pb])
                k.pool(lambda e: e.memset(S32[:], 0.0), w=[sb_])
                k.pool(lambda e: e.memset(SBF[:], 0.0), w=[sb_])
                k.pool(lambda e: e.memset(GZT[:], 1.0), w=[zb])
                import os
                gstage = float(os.environ.get("GLA_STAGE", "99"))
                for tt in range(NT if gstage >= 99 else 1):
                    tsl = slice(tt * 128, (tt + 1) * 128)
                    for kc in range(8):
                        mm(PS[0][:, 0:384], HT[:, kc, tsl], WG[:, kc, 128:512], kc == 0, kc == 7, [wb_, HTb], [PSb[0]])
                    for kc in range(8):
                        mm(PS[1][:, 0:256], HT[:, kc, tsl], WG[:, kc, 512:768], kc == 0, kc == 7, [wb_, HTb], [PSb[1]])
                    for kc in range(8):
                        mm(PS[2][:, 0:128], WG[:, kc, 0:128], HT[:, kc, tsl], kc == 0, kc == 7, [wb_, HTb], [PSb[2]])
                    for kc in range(8):
                        mm(PS[2][:, 128:256], WG[:, kc, 128:256], HT[:, kc, tsl], kc == 0, kc == 7, [wb_, HTb], [PSb[2]])
                    for kc in range(8):
                        mm(PS[3][0:16, 0:128], WG[:, kc, 768:784], HT[:, kc, tsl], kc == 0, kc == 7, [wb_, HTb], [PSb[3]])
                    if gstage < 1:
                        break
                    k.act(lambda e: e.activation(out=GZT[0:16, :], in_=PS[3][0:16, 0:128], func=AF.Copy), r=[PSb[3]], w=[zb])
                    mm(PS[3][:, 128:256], GZT[0:17, :], W2F[0:17, :], True, True, [zb, pb], [PSb[3]])
                    k.act(lambda e: e.activation(out=E1[:], in_=PS[3][:, 128:256], func=AF.Exp, scale=-1.0), r=[PSb[3]], w=[eb])
                    k.act(lambda e: e.activation(out=LL[:], in_=E1[:], func=AF.Ln, bias=1.0), r=[eb], w=[eb])
                    if gstage < 2:
                        break
                    mm(PS[4][:, 0:128], LL[:], MLE[:], True, True, [eb, cb], [PSb[4]])
                    mm(PS[4][:, 128:256], MGT[:], LL[:], True, True, [eb, cb], [PSb[4]])
                    if gstage < 2.1:
                        break
                    k.act(lambda e: e.activation(out=EP[:], in_=PS[4][:, 0:128], func=AF.Exp, scale=-1.0 / 16), r=[PSb[4]], w=[eb])
                    if gstage < 2.2:
                        break
                    k.act(lambda e: e.activation(out=EM[:], in_=PS[4][:, 0:128], func=AF.Exp, scale=1.0 / 16), r=[PSb[4]], w=[eb])
                    if gstage < 2.3:
                        break
                    k.act(lambda e: e.activation(out=E3[:], in_=PS[4][:, 128:256], func=AF.Exp, scale=-1.0 / 16), r=[PSb[4]], w=[eb])
                    if gstage < 2.4:
                        break
                    k.dve(lambda e: e.scalar_tensor_tensor(out=QS[:], in0=PS[2][:, 0:128], scalar=32.0 ** -0.5, in1=EP[:], op0=ALU.mult, op1=ALU.mult), r=[PSb[2], eb], w=[qb_])
                    if gstage < 2.5:
                        break
                    k.dve(lambda e: e.tensor_tensor(out=KS[:], in0=PS[2][:, 128:256], in1=EM[:], op=ALU.mult), r=[PSb[2], eb], w=[qb_])
                    if gstage < 2.6:
                        break
                    k.dve(lambda e: e.tensor_tensor(out=KH[:], in0=PS[0][:, 0:128], in1=E3[:], op=ALU.mult), r=[PSb[0], eb], w=[qb_])
                    if gstage < 2.7:
                        break
                    k.act(lambda e: e.activation(out=V[:], in_=PS[0][:, 128:384], func=AF.Copy), r=[PSb[0]], w=[vb_])
                    if gstage < 2.8:
                        break
                    k.act(lambda e: e.activation(out=SOG[:], in_=PS[1][:, 0:256], func=AF.Silu), r=[PSb[1]], w=[vb_])
                    if gstage < 3:
                        break
                    for h in range(4):
                        mm(PS[5][:, h * 128:(h + 1) * 128], KS[32 * h:32 * h + 32, :], QS[32 * h:32 * h + 32, :], True, True, [qb_], [PSb[5]], tp=(32 * h, 0), sw=True)
                    k.dve(lambda e: e.tensor_tensor(out=AM[:], in0=PS[5][:].rearrange("p (h n) -> p h n", h=4), in1=MLE[:].unsqueeze(1).to_broadcast([128, 4, 128]), op=ALU.mult), r=[PSb[5], cb], w=[qb_])
                    if gstage < 4:
                        break
                    for h in range(4):
                        mm(PS[6][:, h * 64:(h + 1) * 64], AM[:, h, :], V[:, h * 64:(h + 1) * 64], True, False, [qb_, vb_], [PSb[6]])
                        mm(PS[6][:, h * 64:(h + 1) * 64], QS[32 * h:32 * h + 32, :], SBF[32 * h:32 * h + 32, :], False, True, [qb_, sb_], [PSb[6]], tp=(32 * h, 0))
                    if gstage < 5:
                        break
                    mm(PS[7][:, 0:256], KH[:], V[:], True, True, [qb_, vb_], [PSb[7]])
                    for h in range(4):
                        hs = slice(32 * h, 32 * h + 32)
                        k.dve(lambda e: e.scalar_tensor_tensor(out=S32[hs, :], in0=S32[hs, :], scalar=EP[hs, 127:128], in1=PS[7][hs, h * 64:(h + 1) * 64], op0=ALU.mult, op1=ALU.add), r=[sb_, eb, PSb[7]], w=[sb_])
                    k.dve(lambda e: e.tensor_copy(out=SBF[:], in_=S32[:]), r=[sb_], w=[sb_])
                    if gstage < 6:
                        break
                    k.act(lambda e: e.activation(out=O[:], in_=PS[6][:, 0:256].rearrange("p (h d) -> p h d", h=4), func=AF.Copy), r=[PSb[6]], w=[ob])
                    k.dve(lambda e: e.tensor_tensor(out=SQ[:], in0=O[:], in1=O[:], op=ALU.mult), r=[ob], w=[ob])
                    k.dve(lambda e: e.tensor_reduce(out=SS[:], in_=SQ[:], axis=AX.X, op=ALU.add), r=[ob], w=[ob])
                    k.act(lambda e: e.activation(out=SS[:], in_=SS[:], func=AF.Sqrt, scale=1.0 / 64, bias=RMS_EPS), r=[ob], w=[ob])
                    k.dve(lambda e: e.reciprocal(out=SS[:], in_=SS[:]), r=[ob], w=[ob])
                    k.dve(lambda e: e.tensor_tensor(out=O[:], in0=O[:], in1=SS[:].unsqueeze(2).to_broadcast([128, 4, 64]), op=ALU.mult), r=[ob], w=[ob])
                    k.dve(lambda e: e.tensor_tensor(out=O[:], in0=O[:], in1=GN[:].unsqueeze(1).to_broadcast([128, 4, 64]), op=ALU.mult), r=[ob, pb], w=[ob])
                    k.dve(lambda e: e.tensor_tensor(out=O[:], in0=O[:], in1=SOG[:].rearrange("p (h d) -> p h d", h=4), op=ALU.mult), r=[ob, vb_], w=[ob])
                    Of = O[:].rearrange("p h d -> p (h d)")
                    for cc in range(2):
                        k.pe(lambda e: e.transpose(out=PS[1][:, cc * 128:(cc + 1) * 128], in_=Of[:, cc * 128:(cc + 1) * 128], identity=IDF[:]), r=[ob, cb], w=[PSb[1]])
                    k.act(lambda e: e.activation(out=BR[:, 0:2, tsl], in_=PS[1][:, 0:256].rearrange("p (c n) -> p c n", c=2), func=AF.Copy), r=[PSb[1]], w=[BRb])
                k.barrier()


        def phase_rwkv(l, BR, BRb):
            SDEC = -math.exp(-0.5)
            with ExitStack() as ph:
                f32 = lambda n, shp: sb(ph, n, shp)
                b16 = lambda n, shp: sb(ph, n, shp, BF16)
                WR = b16("rw", [128, 8, 1024])
                MU = f32("rmu", [128, 8])
                W2E = f32("rw2e", [65, 256])
                A2 = f32("ra2", [128, 256])
                G2 = b16("rg2", [128, 256])
                A0 = f32("ra0", [128, 2])
                KKp = f32("rkk", [128, 2])
                KA = f32("rka", [128, 2])
                OMK = f32("romk", [128, 2])
                RKp = f32("rrk", [128, 2])
                LNG = f32("rlng", [128, 256])
                LNB = f32("rlnb", [128, 256])
                MLELT = f32("rmlelt", [128, 256])
                MLTLE = f32("rmltle", [128, 256])
                RAW = f32("rraw", [128, 8, 129])
                XM = f32("rxm", [128, 8, 128])
                DX = f32("rdx", [128, 8, 128])
                M32 = f32("rm32", [128, 2, 64])
                MBF = b16("rmbf", [128, 2, 64])
                TZW = f32("rtzw", [65, 128])
                SG = f32("rsg", [128, 256])
                EP = f32("rep", [128, 2, 128])
                EM = f32("rem", [128, 2, 128])
                EX = f32("rex", [128, 2, 128])
                AT = f32("rat", [128, 2, 128])
                KK0 = f32("rkk0", [128, 2, 128])
                SQ = f32("rsq", [128, 2, 128])
                RN = f32("rrn", [128, 2, 128])
                T1 = f32("rt1", [128, 2, 128])
                K2 = f32("rk2", [128, 2, 128])
                NB_ = f32("rnb", [128, 2, 128])
                BRt = b16("rbrt", [128, 2, 2, 128])
                KTt = b16("rktt", [128, 2, 128])
                ALt = b16("ralt", [128, 2, 128])
                KT32 = f32("rkt32", [128, 2, 128])
                AL32 = f32("ral32", [128, 2, 128])
                KP32 = f32("rkp32", [128, 2, 128])
                AP32 = f32("rap32", [128, 2, 128])
                KPT = b16("rkpt", [128, 256])
                APT = b16("rapt", [128, 256])
                VT = b16("rvt", [128, 256])
                V32 = f32("rv32", [128, 256])
                T2 = f32("rt2", [128, 2, 128])
                BON = f32("rbon", [128, 4])
                SZ = b16("rsz", [128, 128])
                GTM_ = f32("rgtm", [128, 256])
                A1m = b16("ra1m", [128, 4, 256])
                A2m = b16("ra2m", [128, 4, 256])
                Z = [b16("rz%d" % i_, [128, 4, 128]) for i_ in range(2)]
                ZT = [b16("rzt%d" % i_, [128, 4, 128]) for i_ in range(2)]
                TT = [b16("rtt%d" % i_, [128, 4, 128]) for i_ in range(2)]
                X0 = b16("rx0", [128, 4, 64])
                UH = b16("ruh", [128, 4, 64])
                Y = f32("ry", [128, 4, 64])
                SQY = f32("rsqy", [128, 4, 64])
                S1 = f32("rs1", [128, 4])
                S2 = f32("rs2", [128, 4])
                MS = f32("rms", [128, 4])
                wb_, pb, rawb, xb, xmb, tzb, sgb, eb, ab, kb, opb, tmb = [Buf() for _ in range(12)]
                a1b, a2b, x0b, uhb, mb, mbfb, yb = [Buf() for _ in range(7)]
                zb, ztb, ttb = [Buf(), Buf()], [Buf(), Buf()], [Buf(), Buf()]
                k.dma("pool", WR[:], win(l, 2064, 1024), writes=[wb_])
                k.dma("sp", MU[:], dr["rwkv_mu_t"][l], writes=[pb])
                k.dma("sp", W2E[0:64, :], dr["rwkv_w2"][l], writes=[pb])
                k.dma("sp", W2E[64:65, :], dr["rwkv_w0"][l:l + 1, :], writes=[pb])
                k.dma("sp", A2[64:128, :], dr["rwkv_a2"][l], writes=[pb])
                k.dma("pool", G2[:], dr["rwkv_g2"][l], writes=[pb])
                k.dma("sp", A0[:], dr["rwkv_a0_t"][l], writes=[pb])
                k.dma("sp", KKp[:], dr["rwkv_kk_t"][l], writes=[pb])
                k.dma("sp", KA[:], dr["rwkv_ka_t"][l], writes=[pb])
                k.dma("sp", RKp[:], dr["rwkv_rk_t"][l], writes=[pb])
                k.dma("sp", LNG[:], dr["rwkv_ln_g"][l:l + 1, :].partition_broadcast(128), writes=[pb])
                k.dma("sp", LNB[:], dr["rwkv_ln_b"][l:l + 1, :].partition_broadcast(128), writes=[pb])
                k.dve(lambda e: e.tensor_scalar(out=OMK[:], in0=KA[:], scalar1=-1.0, scalar2=1.0, op0=ALU.mult, op1=ALU.add), r=[pb], w=[pb])
                k.dve(lambda e: e.tensor_copy(out=MLELT[:, 0:128], in_=MLE[:]), r=[cb], w=[pb])
                k.dve(lambda e: e.tensor_copy(out=MLELT[:, 128:256], in_=MLT[:]), r=[cb], w=[pb])
                k.dve(lambda e: e.tensor_copy(out=MLTLE[:, 0:128], in_=MLT[:]), r=[cb], w=[pb])
                k.dve(lambda e: e.tensor_copy(out=MLTLE[:, 128:256], in_=MLE[:]), r=[cb], w=[pb])
                k.pool(lambda e: e.memset(RAW[:, :, 0:1], 0.0), w=[rawb])
                k.pool(lambda e: e.memset(M32[:], 0.0), w=[mb])
                k.pool(lambda e: e.memset(MBF[:], 0.0), w=[mbfb])
                k.pool(lambda e: e.memset(TZW[:], 1.0), w=[tzb])
                bc2 = lambda t_, n=128: t_[:].unsqueeze(2).to_broadcast([128, 2, n])
                v4 = lambda ap, h: ap.rearrange("p (h n) -> p h n", h=h)
                import os
                rstage = float(os.environ.get("RWKV_STAGE", "99"))
                for tt in range(NT if rstage >= 99 else 1):
                    tsl = slice(tt * 128, (tt + 1) * 128)
                    for j in range(8):
                        ps, psb = (PS[0], PSb[0]) if j < 4 else (PS[1], PSb[1])
                        for kc in range(8):
                            mm(ps[:, (j % 4) * 128:(j % 4 + 1) * 128], WR[:, kc, j * 128:(j + 1) * 128], HT[:, kc, tsl], kc == 0, kc == 7, [wb_, HTb], [psb])
                    k.act(lambda e: e.activation(out=RAW[:, 0:4, 1:129], in_=v4(PS[0][:], 4), func=AF.Copy), r=[PSb[0]], w=[rawb])
                    k.act(lambda e: e.activation(out=RAW[:, 4:8, 1:129], in_=v4(PS[1][:], 4), func=AF.Copy), r=[PSb[1]], w=[rawb])
                    k.dve(lambda e: e.tensor_tensor(out=DX[:], in0=RAW[:, :, 0:128], in1=RAW[:, :, 1:129], op=ALU.subtract), r=[rawb], w=[xb])
                    k.dve(lambda e: e.tensor_tensor(out=DX[:], in0=DX[:], in1=MU[:].unsqueeze(2).to_broadcast([128, 8, 128]), op=ALU.mult), r=[xb, pb], w=[xb])
                    k.dve(lambda e: e.tensor_tensor(out=XM[:], in0=DX[:], in1=RAW[:, :, 1:129], op=ALU.add), r=[xb, rawb], w=[xmb])
                    k.act(lambda e: e.activation(out=RAW[:, :, 0:1], in_=RAW[:, :, 128:129], func=AF.Copy), r=[rawb], w=[rawb])
                    if rstage < 1:
                        tap("rxm", XM[:], xmb, [128, 8, 128])
                        break
                    k.act(lambda e: e.activation(out=TZW[0:64, :], in_=XM[0:64, 6, :], func=AF.Tanh), r=[xmb], w=[tzb])
                    mm(PS[2][:, 0:256], TZW[0:65, :], W2E[0:65, :], True, True, [tzb, pb], [PSb[2]], sw=True)
                    k.act(lambda e: e.activation(out=SG[:], in_=PS[2][:, 0:256], func=AF.Sigmoid), r=[PSb[2]], w=[sgb])
                    for c in range(2):
                        mm(PS[3][:, c * 256:(c + 1) * 256], SG[:, c * 128:(c + 1) * 128], MLELT[:], True, True, [sgb, pb], [PSb[3]])
                    P3 = PS[3][:].rearrange("p (c t n) -> p c t n", c=2, t=2)
                    k.act(lambda e: e.activation(out=EP[:], in_=P3[:, :, 0, :], func=AF.Exp, scale=SDEC), r=[PSb[3]], w=[eb])
                    k.act(lambda e: e.activation(out=EM[:], in_=P3[:, :, 0, :], func=AF.Exp, scale=-SDEC), r=[PSb[3]], w=[eb])
                    k.act(lambda e: e.activation(out=EX[:], in_=P3[:, :, 1, :], func=AF.Exp, scale=SDEC), r=[PSb[3]], w=[eb])
                    for c in range(2):
                        mm(PS[4][:, c * 128:(c + 1) * 128], A2[64:128, c * 128:(c + 1) * 128], XM[64:128, 6, :], True, True, [pb, xmb], [PSb[4]], sw=True)
                    for c in range(2):
                        k.act(lambda e: e.activation(out=AT[:, c, :], in_=PS[4][:, c * 128:(c + 1) * 128], func=AF.Sigmoid, bias=A0[:, c:c + 1]), r=[PSb[4], pb], w=[ab])
                    k.dve(lambda e: e.tensor_tensor(out=KK0[:], in0=XM[:, 2:4, :], in1=bc2(KKp), op=ALU.mult), r=[xmb, pb], w=[kb])
                    k.dve(lambda e: e.tensor_tensor(out=SQ[:], in0=KK0[:], in1=KK0[:], op=ALU.mult), r=[kb], w=[kb])
                    for c in range(2):
                        mm(PS[4][:, 256 + c * 128:256 + (c + 1) * 128], BLK[:], SQ[:, c, :], True, True, [cb, kb], [PSb[4]])
                    k.act(lambda e: e.activation(out=RN[:], in_=v4(PS[4][:, 256:512], 2), func=AF.Sqrt), r=[PSb[4]], w=[kb])
                    k.dve(lambda e: e.tensor_scalar(out=RN[:], in0=RN[:], scalar1=1e-12, scalar2=None, op0=ALU.max), r=[kb], w=[kb])
                    k.dve(lambda e: e.reciprocal(out=RN[:], in_=RN[:]), r=[kb], w=[kb])
                    k.dve(lambda e: e.tensor_tensor(out=KK0[:], in0=KK0[:], in1=RN[:], op=ALU.mult), r=[kb], w=[kb])
                    k.dve(lambda e: e.tensor_tensor(out=T1[:], in0=AT[:], in1=bc2(KA), op=ALU.mult), r=[ab, pb], w=[kb])
                    k.dve(lambda e: e.tensor_tensor(out=T1[:], in0=T1[:], in1=bc2(OMK), op=ALU.add), r=[kb, pb], w=[kb])
                    k.dve(lambda e: e.tensor_tensor(out=K2[:], in0=XM[:, 2:4, :], in1=T1[:], op=ALU.mult), r=[xmb, kb], w=[kb])
                    PCb = EP[:, :, 127:128].to_broadcast([128, 2, 128])
                    k.dve(lambda e: e.tensor_tensor(out=BRt[:, :, 1, :], in0=XM[:, 0:2, :], in1=EP[:], op=ALU.mult), r=[xmb, eb], w=[opb])
                    k.dve(lambda e: e.tensor_tensor(out=BRt[:, :, 0, :], in0=KK0[:], in1=EX[:], op=ALU.mult), r=[kb, eb], w=[opb])
                    k.dve(lambda e: e.tensor_tensor(out=KT32[:], in0=K2[:], in1=EM[:], op=ALU.mult), r=[kb, eb], w=[opb])
                    k.act(lambda e: e.activation(out=KTt[:], in_=KT32[:], func=AF.Copy), r=[opb], w=[opb])
                    k.dve(lambda e: e.tensor_tensor(out=KP32[:], in0=KT32[:], in1=PCb, op=ALU.mult), r=[opb, eb], w=[opb])
                    k.dve(lambda e: e.scalar_tensor_tensor(out=NB_[:], in0=KK0[:], scalar=-1.0, in1=AT[:], op0=ALU.mult, op1=ALU.mult), r=[kb, ab], w=[opb])
                    k.dve(lambda e: e.tensor_tensor(out=AL32[:], in0=NB_[:], in1=EM[:], op=ALU.mult), r=[opb, eb], w=[opb])
                    k.act(lambda e: e.activation(out=ALt[:], in_=AL32[:], func=AF.Copy), r=[opb], w=[opb])
                    k.dve(lambda e: e.tensor_tensor(out=AP32[:], in0=AL32[:], in1=PCb, op=ALU.mult), r=[opb, eb], w=[opb])
                    for c in range(2):
                        k.pe(lambda e: e.transpose(out=PS[5][:, c * 128:(c + 1) * 128], in_=KP32[:, c, :], identity=IDF[:]), r=[opb, cb], w=[PSb[5]])
                        k.pe(lambda e: e.transpose(out=PS[5][:, 256 + c * 128:256 + (c + 1) * 128], in_=AP32[:, c, :], identity=IDF[:]), r=[opb, cb], w=[PSb[5]])
                        k.pe(lambda e: e.transpose(out=PS[6][:, c * 128:(c + 1) * 128], in_=XM[:, 4 + c, :], identity=IDF[:]), r=[xmb, cb], w=[PSb[6]])
                    k.act(lambda e: e.activation(out=KPT[:], in_=PS[5][:, 0:256], func=AF.Copy), r=[PSb[5]], w=[tmb])
                    k.act(lambda e: e.activation(out=APT[:], in_=PS[5][:, 256:512], func=AF.Copy), r=[PSb[5]], w=[tmb])
                    k.act(lambda e: e.activation(out=VT[:], in_=PS[6][:, 0:256], func=AF.Copy), r=[PSb[6]], w=[tmb])
                    k.dve(lambda e: e.tensor_copy(out=V32[:], in_=PS[6][:, 0:256]), r=[PSb[6]], w=[tmb])
                    k.dve(lambda e: e.tensor_tensor(out=T2[:], in0=XM[:, 0:2, :], in1=K2[:], op=ALU.mult), r=[xmb, kb], w=[kb])
                    k.dve(lambda e: e.tensor_tensor(out=T2[:], in0=T2[:], in1=bc2(RKp), op=ALU.mult), r=[kb, pb], w=[kb])
                    for c in range(2):
                        mm(PS[6][:, 256 + 2 * c:256 + 2 * c + 2], T2[:, c, :], HSEL[:], True, True, [kb, cb], [PSb[6]])
                    k.act(lambda e: e.activation(out=BON[:], in_=PS[6][:, 256:260], func=AF.Copy), r=[PSb[6]], w=[tmb])
                    k.act(lambda e: e.activation(out=SZ[:], in_=XM[:, 7, :], func=AF.Sigmoid), r=[xmb], w=[tmb])
                    mm(PS[2][:, 256:512], SZ[:], G2[:], True, True, [tmb, pb], [PSb[2]])
                    k.act(lambda e: e.activation(out=GTM_[:], in_=PS[2][:, 256:512], func=AF.Copy), r=[PSb[2]], w=[tmb])
                    if rstage < 2:
                        tap("rkpt", KPT[:], tmb, [128, 256])
                        tap("rbrt", BRt[:], opb, [128, 2, 2, 128])
                        break
                    hsl = lambda h: slice(64 * (h % 2), 64 * (h % 2) + 64)
                    for h in range(4):
                        c = h // 2
                        mm(PS[h // 2][:, (h % 2) * 256:(h % 2 + 1) * 256], KTt[hsl(h), c, :], BRt[hsl(h), c].rearrange("p t n -> p (t n)"), True, True, [opb], [PSb[h // 2]], sw=True)
                    for h in range(4):
                        c = h // 2
                        mm(PS[2 + h // 2][:, (h % 2) * 256:(h % 2 + 1) * 256], ALt[hsl(h), c, :], BRt[hsl(h), c].rearrange("p t n -> p (t n)"), True, True, [opb], [PSb[2 + h // 2]], sw=True)
                    for h in range(4):
                        c = h // 2
                        mm(PS[4][:, h * 128:(h + 1) * 128], BRt[hsl(h), c, 0, :], ALt[hsl(h), c, :], True, True, [opb], [PSb[4]], sw=True)
                    for i_ in range(2):
                        k.dve(lambda e: e.tensor_tensor(out=A1m[:, 2 * i_:2 * i_ + 2, :], in0=v4(PS[i_][:], 2), in1=MLTLE[:].unsqueeze(1).to_broadcast([128, 2, 256]), op=ALU.mult), r=[PSb[i_], pb], w=[a1b])
                    for i_ in range(2):
                        k.dve(lambda e: e.tensor_tensor(out=A2m[:, 2 * i_:2 * i_ + 2, :], in0=v4(PS[2 + i_][:], 2), in1=MLTLE[:].unsqueeze(1).to_broadcast([128, 2, 256]), op=ALU.mult), r=[PSb[2 + i_], pb], w=[a2b])
                    k.dve(lambda e: e.tensor_tensor(out=Z[0][:], in0=v4(PS[4][:], 4), in1=MGT[:].unsqueeze(1).to_broadcast([128, 4, 128]), op=ALU.mult), r=[PSb[4], cb], w=[zb[0]])
                    k.dve(lambda e: e.tensor_tensor(out=TT[0][:], in0=A2m[:, :, 0:128], in1=IDF[:].unsqueeze(1).to_broadcast([128, 4, 128]), op=ALU.add), r=[a2b, cb], w=[ttb[0]])
                    for i_ in range(1, 7):
                        o_, n_ = (i_ - 1) % 2, i_ % 2
                        if i_ == 1:
                            zt_old, zt_oldb = (lambda h: A2m[:, h, 0:128]), a2b
                        else:
                            zt_old, zt_oldb = (lambda h, o_=o_: ZT[o_][:, h, :]), ztb[o_]
                        for h in range(4):
                            mm(PS[5][:, h * 128:(h + 1) * 128], zt_old(h), Z[o_][:, h, :], True, True, [zt_oldb, zb[o_]], [PSb[5]])
                        k.act(lambda e: e.activation(out=Z[n_][:], in_=v4(PS[5][:], 4), func=AF.Copy), r=[PSb[5]], w=[zb[n_]])
                        if i_ < 6:
                            for h in range(4):
                                mm(PS[6][:, h * 128:(h + 1) * 128], Z[o_][:, h, :], zt_old(h), True, True, [zt_oldb, zb[o_]], [PSb[6]])
                            k.dve(lambda e: e.tensor_copy(out=ZT[n_][:], in_=v4(PS[6][:], 4)), r=[PSb[6]], w=[ztb[n_]])
                        for h in range(4):
                            mm(PS[7][:, h * 128:(h + 1) * 128], Z[n_][:, h, :], TT[o_][:, h, :], True, True, [zb[n_], ttb[o_]], [PSb[7]])
                        k.dve(lambda e: e.tensor_tensor(out=TT[n_][:], in0=v4(PS[7][:], 4), in1=TT[o_][:], op=ALU.add), r=[PSb[7], ttb[o_]], w=[ttb[n_]])
                    TTf, ttfb = TT[0], ttb[0]
                    hc = lambda h: slice(h * 64, (h + 1) * 64)
                    for h in range(4):
                        c = h // 2
                        mm(PS[0][:, hc(h)], BRt[hsl(h), c, 0, :], MBF[hsl(h), c, :], True, False, [opb, mbfb], [PSb[0]], sw=True)
                        mm(PS[0][:, hc(h)], A1m[:, h, 0:128], VT[:, hc(h)], False, True, [a1b, tmb], [PSb[0]])
                    k.act(lambda e: e.activation(out=X0[:], in_=v4(PS[0][:, 0:256], 4), func=AF.Copy), r=[PSb[0]], w=[x0b])
                    for h in range(4):
                        mm(PS[1][:, hc(h)], TTf[:, h, :], X0[:, h, :], True, True, [ttfb, x0b], [PSb[1]])
                    k.act(lambda e: e.activation(out=UH[:], in_=v4(PS[1][:, 0:256], 4), func=AF.Copy), r=[PSb[1]], w=[uhb])
                    for h in range(4):
                        c = h // 2
                        mm(PS[2][:, hc(h)], BRt[hsl(h), c, 1, :], MBF[hsl(h), c, :], True, False, [opb, mbfb], [PSb[2]], sw=True)
                        mm(PS[2][:, hc(h)], A1m[:, h, 128:256], VT[:, hc(h)], False, False, [a1b, tmb], [PSb[2]])
                        mm(PS[2][:, hc(h)], A2m[:, h, 128:256], UH[:, h, :], False, True, [a2b, uhb], [PSb[2]])
                    for h in range(4):
                        c = h // 2
                        mm(PS[3][hsl(h), c * 64:(c + 1) * 64], KPT[:, hc(h)], VT[:, hc(h)], True, False, [tmb], [PSb[3]])
                        mm(PS[3][hsl(h), c * 64:(c + 1) * 64], APT[:, hc(h)], UH[:, h, :], False, True, [tmb, uhb], [PSb[3]])
                    k.dve(lambda e: e.tensor_tensor(out=M32[:], in0=M32[:], in1=EP[:, :, 127:128].to_broadcast([128, 2, 64]), op=ALU.mult), r=[mb, eb], w=[mb])
                    k.dve(lambda e: e.tensor_tensor(out=M32[:], in0=M32[:], in1=v4(PS[3][:, 0:128], 2), op=ALU.add), r=[mb, PSb[3]], w=[mb])
                    k.act(lambda e: e.activation(out=MBF[:], in_=M32[:], func=AF.Copy), r=[mb], w=[mbfb])
                    k.act(lambda e: e.activation(out=Y[:], in_=v4(PS[2][:, 0:256], 4), func=AF.Copy), r=[PSb[2]], w=[yb])
                    if rstage < 3:
                        tap("ry", Y[:], yb, [128, 4, 64])
                        break
                    k.dve(lambda e: e.tensor_reduce(out=S1[:], in_=Y[:], axis=AX.X, op=ALU.add), r=[yb], w=[yb])
                    k.dve(lambda e: e.tensor_tensor(out=SQY[:], in0=Y[:], in1=Y[:], op=ALU.mult), r=[yb], w=[yb])
                    k.dve(lambda e: e.tensor_reduce(out=S2[:], in_=SQY[:], axis=AX.X, op=ALU.add), r=[yb], w=[yb])
                    k.dve(lambda e: e.tensor_scalar(out=S1[:], in0=S1[:], scalar1=1.0 / 64, scalar2=None, op0=ALU.mult), r=[yb], w=[yb])
                    k.dve(lambda e: e.tensor_tensor(out=MS[:], in0=S1[:], in1=S1[:], op=ALU.mult), r=[yb], w=[yb])
                    k.dve(lambda e: e.scalar_tensor_tensor(out=S2[:], in0=S2[:], scalar=1.0 / 64, in1=MS[:], op0=ALU.mult, op1=ALU.subtract), r=[yb], w=[yb])
                    k.act(lambda e: e.activation(out=S2[:], in_=S2[:], func=AF.Sqrt, bias=RWKV_GN_EPS), r=[yb], w=[yb])
                    k.dve(lambda e: e.reciprocal(out=S2[:], in_=S2[:]), r=[yb], w=[yb])
                    k.dve(lambda e: e.tensor_tensor(out=Y[:], in0=Y[:], in1=S1[:].unsqueeze(2).to_broadcast([128, 4, 64]), op=ALU.subtract), r=[yb], w=[yb])
                    k.dve(lambda e: e.tensor_tensor(out=Y[:], in0=Y[:], in1=S2[:].unsqueeze(2).to_broadcast([128, 4, 64]), op=ALU.mult), r=[yb], w=[yb])
                    Yf = Y[:].rearrange("p h d -> p (h d)")
                    k.dve(lambda e: e.tensor_tensor(out=Yf, in0=Yf, in1=LNG[:], op=ALU.mult), r=[yb, pb], w=[yb])
                    k.dve(lambda e: e.tensor_tensor(out=Yf, in0=Yf, in1=LNB[:], op=ALU.add), r=[yb, pb], w=[yb])
                    k.dve(lambda e: e.tensor_tensor(out=SQY[:], in0=v4(V32[:], 4), in1=BON[:].unsqueeze(2).to_broadcast([128, 4, 64]), op=ALU.mult), r=[tmb, yb], w=[yb])
                    k.dve(lambda e: e.tensor_tensor(out=Y[:], in0=Y[:], in1=SQY[:], op=ALU.add), r=[yb], w=[yb])
                    k.dve(lambda e: e.tensor_tensor(out=Yf, in0=Yf, in1=GTM_[:], op=ALU.mult), r=[yb, tmb], w=[yb])
                    for cc in range(2):
                        k.pe(lambda e: e.transpose(out=PS[5][:, cc * 128:(cc + 1) * 128], in_=Yf[:, cc * 128:(cc + 1) * 128], identity=IDF[:]), r=[yb, cb], w=[PSb[5]])
                    k.act(lambda e: e.activation(out=BR[:, 6:8, tsl], in_=v4(PS[5][:, 0:256], 2), func=AF.Copy), r=[PSb[5]], w=[BRb])
                k.barrier()


        def gla_setup(l, ph):
            G = Prog()
            WG = sb(ph, "gw", [128, 8, 784], BF16)
            W2F = sb(ph, "gw2f", [17, 128])
            GN = sb(ph, "ggn", [128, 64])
            S32 = sb(ph, "gs32", [128, 64])
            SBF = sb(ph, "gsbf", [128, 64], BF16)
            GZT = sb(ph, "ggzt", [17, 128])
            E1 = sb(ph, "ge1", [128, 128])
            LL = sb(ph, "gll", [128, 128])
            EP = sb(ph, "gep", [128, 128])
            EM = sb(ph, "gem", [128, 128])
            E3 = sb(ph, "ge3", [128, 128])
            KTM = sb(ph, "gktm", [128, 128])
            QKr = sb(ph, "gqkr", [128, 256])
            QS = sb(ph, "gqs", [128, 128], BF16)
            KS = sb(ph, "gks", [128, 128], BF16)
            KH = sb(ph, "gkh", [128, 128], BF16)
            V = sb(ph, "gv", [128, 256], BF16)
            SOG = sb(ph, "gsog", [128, 256])
            AM = sb(ph, "gam", [128, 4, 128], BF16)
            O = sb(ph, "go", [128, 4, 64])
            SQ = sb(ph, "gsq", [128, 4, 64])
            SS = sb(ph, "gss", [128, 4])
            wb_, pb, sb_, zb, eb, qb_, vb_, ob, rb_ = [Buf() for _ in range(9)]
            k.dma("pool", WG[:, :, 128:512], win(l, 128, 384), writes=[wb_])
            k.dma("pool", WG[:, :, 512:784], win(l, 512, 272), writes=[wb_])
            k.dma("pool", WG[:, :, 0:128], win(l, 0, 128), writes=[wb_])
            k.dma("sp", W2F[0:16, :], dr["gla_gate_w2"][l], writes=[pb])
            k.dma("sp", W2F[16:17, :], dr["gla_gate_b"][l:l + 1, :], writes=[pb])
            k.dma("sp", GN[:], dr["gla_norm"][l:l + 1, :].partition_broadcast(128), writes=[pb])
            k.pool(lambda e: e.memset(S32[:], 0.0), w=[sb_])
            k.pool(lambda e: e.memset(SBF[:], 0.0), w=[sb_])
            k.pool(lambda e: e.memset(GZT[:], 1.0), w=[zb])
            X_, Xb, Y_, Yb = PS[6], PSb[6], PS[7], PSb[7]

            def gen(BR, BRb):
                for tt in range(NT):
                    tsl = slice(tt * 128, (tt + 1) * 128)
                    for kc in range(8):
                        mm(X_[:, 0:384], HT[:, kc, tsl], WG[:, kc, 128:512], kc == 0, kc == 7, [wb_, HTb], [Xb])
                    k.act(lambda e: e.activation(out=KTM[:], in_=X_[:, 0:128], func=AF.Copy), r=[Xb], w=[rb_])
                    k.act(lambda e: e.activation(out=V[:], in_=X_[:, 128:384], func=AF.Copy), r=[Xb], w=[vb_])
                    yield
                    for kc in range(8):
                        mm(Y_[:, 0:256], HT[:, kc, tsl], WG[:, kc, 512:768], kc == 0, kc == 7, [wb_, HTb], [Yb])
                    k.act(lambda e: e.activation(out=SOG[:], in_=Y_[:, 0:256], func=AF.Silu), r=[Yb], w=[vb_])
                    yield
                    for kc in range(8):
                        mm(X_[:, 0:128], WG[:, kc, 0:128], HT[:, kc, tsl], kc == 0, kc == 7, [wb_, HTb], [Xb])
                    for kc in range(8):
                        mm(X_[:, 128:256], WG[:, kc, 128:256], HT[:, kc, tsl], kc == 0, kc == 7, [wb_, HTb], [Xb])
                    k.dve(lambda e: e.tensor_copy(out=QKr[:], in_=X_[:, 0:256]), r=[Xb], w=[rb_])
                    yield
                    for kc in range(8):
                        mm(Y_[0:16, 0:128], WG[:, kc, 768:784], HT[:, kc, tsl], kc == 0, kc == 7, [wb_, HTb], [Yb])
                    k.act(lambda e: e.activation(out=GZT[0:16, :], in_=Y_[0:16, 0:128], func=AF.Copy), r=[Yb], w=[zb])
                    mm(Y_[:, 128:256], GZT[0:17, :], W2F[0:17, :], True, True, [zb, pb], [Yb])
                    k.act(lambda e: e.activation(out=E1[:], in_=Y_[:, 128:256], func=AF.Exp, scale=-1.0), r=[Yb], w=[eb])
                    k.act(lambda e: e.activation(out=LL[:], in_=E1[:], func=AF.Ln, bias=1.0), r=[eb], w=[eb])
                    yield
                    mm(X_[:, 0:128], LL[:], MLE[:], True, True, [eb, cb], [Xb])
                    mm(X_[:, 128:256], MGT[:], LL[:], True, True, [eb, cb], [Xb])
                    k.act(lambda e: e.activation(out=EP[:], in_=X_[:, 0:128], func=AF.Exp, scale=-1.0 / 16), r=[Xb], w=[eb])
                    k.act(lambda e: e.activation(out=EM[:], in_=X_[:, 0:128], func=AF.Exp, scale=1.0 / 16), r=[Xb], w=[eb])
                    k.act(lambda e: e.activation(out=E3[:], in_=X_[:, 128:256], func=AF.Exp, scale=-1.0 / 16), r=[Xb], w=[eb])
                    yield
                    k.dve(lambda e: e.scalar_tensor_tensor(out=QS[:], in0=QKr[:, 0:128], scalar=32.0 ** -0.5, in1=EP[:], op0=ALU.mult, op1=ALU.mult), r=[rb_, eb], w=[qb_])
                    k.dve(lambda e: e.tensor_tensor(out=KS[:], in0=QKr[:, 128:256], in1=EM[:], op=ALU.mult), r=[rb_, eb], w=[qb_])
                    k.dve(lambda e: e.tensor_tensor(out=KH[:], in0=KTM[:], in1=E3[:], op=ALU.mult), r=[rb_, eb], w=[qb_])
                    yield
                    for h in range(4):
                        mm(Y_[:, h * 128:(h + 1) * 128], KS[32 * h:32 * h + 32, :], QS[32 * h:32 * h + 32, :], True, True, [qb_], [Yb], tp=(32 * h, 0), sw=True)
                    k.dve(lambda e: e.tensor_tensor(out=AM[:], in0=Y_[:].rearrange("p (h n) -> p h n", h=4), in1=MLE[:].unsqueeze(1).to_broadcast([128, 4, 128]), op=ALU.mult), r=[Yb, cb], w=[qb_])
                    yield
                    for h in range(4):
                        mm(X_[:, h * 64:(h + 1) * 64], AM[:, h, :], V[:, h * 64:(h + 1) * 64], True, False, [qb_, vb_], [Xb])
                        mm(X_[:, h * 64:(h + 1) * 64], QS[32 * h:32 * h + 32, :], SBF[32 * h:32 * h + 32, :], False, True, [qb_, sb_], [Xb], tp=(32 * h, 0))
                    mm(Y_[:, 0:256], KH[:], V[:], True, True, [qb_, vb_], [Yb])
                    for h in range(4):
                        hs = slice(32 * h, 32 * h + 32)
                        k.dve(lambda e: e.scalar_tensor_tensor(out=S32[hs, :], in0=S32[hs, :], scalar=EP[hs, 127:128], in1=Y_[hs, h * 64:(h + 1) * 64], op0=ALU.mult, op1=ALU.add), r=[sb_, eb, Yb], w=[sb_])
                    k.dve(lambda e: e.tensor_copy(out=SBF[:], in_=S32[:]), r=[sb_], w=[sb_])
                    k.act(lambda e: e.activation(out=O[:], in_=X_[:, 0:256].rearrange("p (h d) -> p h d", h=4), func=AF.Copy), r=[Xb], w=[ob])
                    yield
                    k.dve(lambda e: e.tensor_tensor(out=SQ[:], in0=O[:], in1=O[:], op=ALU.mult), r=[ob], w=[ob])
                    k.dve(lambda e: e.tensor_reduce(out=SS[:], in_=SQ[:], axis=AX.X, op=ALU.add), r=[ob], w=[ob])
                    k.act(lambda e: e.activation(out=SS[:], in_=SS[:], func=AF.Sqrt, scale=1.0 / 64, bias=RMS_EPS), r=[ob], w=[ob])
                    k.dve(lambda e: e.reciprocal(out=SS[:], in_=SS[:]), r=[ob], w=[ob])
                    yield
                    k.dve(lambda e: e.tensor_tensor(out=O[:], in0=O[:], in1=SS[:].unsqueeze(2).to_broadcast([128, 4, 64]), op=ALU.mult), r=[ob], w=[ob])
                    k.dve(lambda e: e.tensor_tensor(out=O[:], in0=O[:], in1=GN[:].unsqueeze(1).to_broadcast([128, 4, 64]), op=ALU.mult), r=[ob, pb], w=[ob])
                    k.dve(lambda e: e.tensor_tensor(out=O[:], in0=O[:], in1=SOG[:].rearrange("p (h d) -> p h d", h=4), op=ALU.mult), r=[ob, vb_], w=[ob])
                    Of = O[:].rearrange("p h d -> p (h d)")
                    for cc in range(2):
                        k.pe(lambda e: e.transpose(out=X_[:, cc * 128:(cc + 1) * 128], in_=Of[:, cc * 128:(cc + 1) * 128], identity=IDF[:]), r=[ob, cb], w=[Xb])
                    k.act(lambda e: e.activation(out=BR[:, 0:2, tsl], in_=X_[:, 0:256].rearrange("p (c n) -> p c n", c=2), func=AF.Copy), r=[Xb], w=[BRb])
                    yield

            return gen

        def run_gens(gens, weights=None):
            gens = list(gens)
            weights = list(weights) if weights is not None else [1] * len(gens)
            live = list(range(len(gens)))
            while live:
                for gi in list(live):
                    for _ in range(weights[gi]):
                        try:
                            next(gens[gi])
                        except StopIteration:
                            live.remove(gi)
                            break

        def phase_rwkv2(l, BR, BRb, with_gla=False):
            SDEC = -math.exp(-0.5)
            with ExitStack() as ph:
                extra_gens = []
                if with_gla:
                    extra_gens.append(gla_setup(l, ph)(BR, BRb))
                f32 = lambda n, shp: sb(ph, n, shp)
                b16 = lambda n, shp: sb(ph, n, shp, BF16)
                WR = b16("rw", [128, 8, 1024])
                MU = f32("rmu", [128, 8])
                W2E = f32("rw2e", [65, 256])
                A2 = f32("ra2", [128, 256])
                G2 = b16("rg2", [128, 256])
                A0 = f32("ra0", [128, 2])
                KKp = f32("rkk", [128, 2])
                KA = f32("rka", [128, 2])
                OMK = f32("romk", [128, 2])
                RKp = f32("rrk", [128, 2])
                LNG = f32("rlng", [128, 256])
                LNB = f32("rlnb", [128, 256])
                MLELT = f32("rmlelt", [128, 256])
                MLTLE = f32("rmltle", [128, 256])
                M32 = f32("rm32", [128, 2, 64])
                MBF = b16("rmbf", [128, 2, 64])
                X0 = b16("rx0", [128, 4, 64])
                UH = b16("ruh", [128, 4, 64])
                Y = f32("ry", [128, 4, 64])
                SQY = f32("rsqy", [128, 4, 64])
                S1 = f32("rs1", [128, 4])
                S2 = f32("rs2", [128, 4])
                MS = f32("rms", [128, 4])
                wb_, pb, x0b, uhb, mb, mbfb, yb = [Buf() for _ in range(7)]

                class SetT:
                    pass

                sets = []
                for p in range(2):
                    S_ = SetT()
                    n_ = lambda nm: "r%d%s" % (p, nm)
                    S_.RAW = f32(n_("raw"), [128, 8, 129])
                    S_.XM = f32(n_("xm"), [128, 8, 128])
                    S_.TZW = f32(n_("tzw"), [65, 128])
                    S_.SG = f32(n_("sg"), [128, 256])
                    S_.EP = f32(n_("ep"), [128, 2, 128])
                    S_.EM = f32(n_("em"), [128, 2, 128])
                    S_.EX = f32(n_("ex"), [128, 2, 128])
                    S_.AT = f32(n_("at"), [128, 2, 128])
                    S_.KK0 = f32(n_("kk0"), [128, 2, 128])
                    S_.SQ = f32(n_("sq"), [128, 2, 128])
                    S_.T1 = f32(n_("t1"), [128, 2, 128])
                    S_.K2 = f32(n_("k2"), [128, 2, 128])
                    S_.KT32 = f32(n_("kt32"), [128, 2, 128])
                    S_.AL32 = f32(n_("al32"), [128, 2, 128])
                    S_.KTt = b16(n_("ktt"), [128, 2, 128])
                    S_.ALt = b16(n_("alt"), [128, 2, 128])
                    S_.SZ = b16(n_("sz"), [128, 128])
                    S_.Z = [b16(n_("z%d" % i_), [128, 4, 128]) for i_ in range(2)]
                    S_.ZT = [b16(n_("zt%d" % i_), [128, 4, 128]) for i_ in range(2)]
                    S_.TT = [b16(n_("tt%d" % i_), [128, 4, 128]) for i_ in range(2)]
                    S_.BRt = b16(n_("brt"), [128, 2, 2, 128])
                    S_.A1m = b16(n_("a1m"), [128, 4, 256])
                    S_.A2m = b16(n_("a2m"), [128, 4, 256])
                    S_.KPT = b16(n_("kpt"), [128, 256])
                    S_.APT = b16(n_("apt"), [128, 256])
                    S_.VT = b16(n_("vt"), [128, 256])
                    S_.V32 = f32(n_("v32"), [128, 256])
                    S_.BON = f32(n_("bon"), [128, 4])
                    S_.GTM_ = f32(n_("gtm"), [128, 256])
                    S_.PCs = f32(n_("pcs"), [128, 2])
                    (S_.rawb, S_.xmb, S_.tzb, S_.sgb, S_.eb, S_.ab, S_.kb, S_.opb, S_.tmb, S_.a1b, S_.a2b) = [Buf() for _ in range(11)]
                    S_.zb, S_.ztb, S_.ttb = [Buf(), Buf()], [Buf(), Buf()], [Buf(), Buf()]
                    S_.B = [2 * p, 2 * p + 1, 2 * p]
                    k.pool(lambda e: e.memset(S_.TZW[:], 1.0), w=[S_.tzb])
                    sets.append(S_)
                k.pool(lambda e: e.memset(sets[0].RAW[:, :, 0:1], 0.0), w=[sets[0].rawb])
                wbq = [Buf() for _ in range(4)]
                for q_ in range(4):
                    k.dma("pool", WR[:, :, q_ * 256:(q_ + 1) * 256], win(l, 2064 + q_ * 256, 256), writes=[wbq[q_]])
                k.dma("sp", MU[:], dr["rwkv_mu_t"][l], writes=[pb])
                k.dma("sp", W2E[0:64, :], dr["rwkv_w2"][l], writes=[pb])
                k.dma("sp", W2E[64:65, :], dr["rwkv_w0"][l:l + 1, :], writes=[pb])
                k.dma("sp", A2[64:128, :], dr["rwkv_a2"][l], writes=[pb])
                k.dma("pool", G2[:], dr["rwkv_g2"][l], writes=[pb])
                k.dma("sp", A0[:], dr["rwkv_a0_t"][l], writes=[pb])
                k.dma("sp", KKp[:], dr["rwkv_kk_t"][l], writes=[pb])
                k.dma("sp", KA[:], dr["rwkv_ka_t"][l], writes=[pb])
                k.dma("sp", RKp[:], dr["rwkv_rk_t"][l], writes=[pb])
                k.dma("sp", LNG[:], dr["rwkv_ln_g"][l:l + 1, :].partition_broadcast(128), writes=[pb])
                k.dma("sp", LNB[:], dr["rwkv_ln_b"][l:l + 1, :].partition_broadcast(128), writes=[pb])
                k.dve(lambda e: e.tensor_scalar(out=OMK[:], in0=KA[:], scalar1=-1.0, scalar2=1.0, op0=ALU.mult, op1=ALU.add), r=[pb], w=[pb])
                k.dve(lambda e: e.tensor_copy(out=MLELT[:, 0:128], in_=MLE[:]), r=[cb], w=[pb])
                k.dve(lambda e: e.tensor_copy(out=MLELT[:, 128:256], in_=MLT[:]), r=[cb], w=[pb])
                k.dve(lambda e: e.tensor_copy(out=MLTLE[:, 0:128], in_=MLT[:]), r=[cb], w=[pb])
                k.dve(lambda e: e.tensor_copy(out=MLTLE[:, 128:256], in_=MLE[:]), r=[cb], w=[pb])
                k.pool(lambda e: e.memset(M32[:], 0.0), w=[mb])
                k.pool(lambda e: e.memset(MBF[:], 0.0), w=[mbfb])
                bc2 = lambda t_, n=128: t_[:].unsqueeze(2).to_broadcast([128, 2, n])
                v4 = lambda ap, h: ap.rearrange("p (h n) -> p h n", h=h)
                hsl = lambda h: slice(64 * (h % 2), 64 * (h % 2) + 64)
                hc = lambda h: slice(h * 64, (h + 1) * 64)
                prep_done = [False] * NT
                chain_done = [False] * NT

                def prep_gen(p):
                    st = sets[p]
                    a_, b_, c_ = st.B
                    for _ in range(p * int(os.environ.get("K_STAG", "0"))):
                        yield
                    RAW, XM, TZW, SG, EP, EM, EX, AT, KK0, SQ, T1, K2 = st.RAW, st.XM, st.TZW, st.SG, st.EP, st.EM, st.EX, st.AT, st.KK0, st.SQ, st.T1, st.K2
                    KT32, AL32, KTt, ALt, SZ, Z, ZT, TT = st.KT32, st.AL32, st.KTt, st.ALt, st.SZ, st.Z, st.ZT, st.TT
                    BRt, A1m, A2m, KPT, APT, VT, V32, BON, GTM_, PCs = st.BRt, st.A1m, st.A2m, st.KPT, st.APT, st.VT, st.V32, st.BON, st.GTM_, st.PCs
                    rawb, xmb, tzb, sgb, eb, ab, kb, opb, tmb, a1b, a2b, zb, ztb, ttb = st.rawb, st.xmb, st.tzb, st.sgb, st.eb, st.ab, st.kb, st.opb, st.tmb, st.a1b, st.a2b, st.zb, st.ztb, st.ttb
                    for tt in range(p, NT, 2):
                        t0 = tt * 128
                        nco = 128 if tt == 0 else 129
                        src0 = t0 if tt == 0 else t0 - 1
                        for groups in (((a_, (0, 1, 2)), (b_, (3, 4, 5))), ((a_, (6, 7)),)):
                            for bk, chs in groups:
                                for ji, j in enumerate(chs):
                                    for kc in range(8):
                                        mm(PS[bk][:, ji * 129:ji * 129 + nco], WR[:, kc, j * 128:(j + 1) * 128], HT[:, kc, src0:t0 + 128], kc == 0, kc == 7, [wbq[j // 2], HTb], [PSb[bk]])
                                yield
                            for bk, chs in groups:
                                n3 = len(chs)
                                src = PS[bk][:, 0:n3 * 129].rearrange("p (j n) -> p j n", j=n3)[:, :, 0:nco]
                                k.act(lambda e: e.activation(out=RAW[:, chs[0]:chs[0] + n3, 129 - nco:129], in_=src, func=AF.Copy), r=[PSb[bk]], w=[rawb])
                            yield
                        sh_eng = k.pool if os.environ.get("K_POOLSHIFT", "0") == "1" else k.dve
                        sh_eng(lambda e: e.tensor_tensor(out=XM[:], in0=RAW[:, :, 0:128], in1=RAW[:, :, 1:129], op=ALU.subtract), r=[rawb], w=[xmb])
                        sh_eng(lambda e: e.tensor_tensor(out=XM[:], in0=XM[:], in1=MU[:].unsqueeze(2).to_broadcast([128, 8, 128]), op=ALU.mult), r=[xmb, pb], w=[xmb])
                        sh_eng(lambda e: e.tensor_tensor(out=XM[:], in0=XM[:], in1=RAW[:, :, 1:129], op=ALU.add), r=[xmb, rawb], w=[xmb])
                        yield
                        k.act(lambda e: e.activation(out=TZW[0:64, :], in_=XM[0:64, 6, :], func=AF.Tanh), r=[xmb], w=[tzb])
                        mm(PS[a_][:, 0:256], TZW[0:65, :], W2E[0:65, :], True, True, [tzb, pb], [PSb[a_]], sw=True)
                        k.act(lambda e: e.activation(out=SG[:], in_=PS[a_][:, 0:256], func=AF.Sigmoid), r=[PSb[a_]], w=[sgb])
                        yield
                        for c in range(2):
                            mm(PS[b_][:, c * 256:(c + 1) * 256], SG[:, c * 128:(c + 1) * 128], MLELT[:], True, True, [sgb, pb], [PSb[b_]])
                        P3 = PS[b_][:].rearrange("p (c t n) -> p c t n", c=2, t=2)
                        k.act(lambda e: e.activation(out=EP[:], in_=P3[:, :, 0, :], func=AF.Exp, scale=SDEC), r=[PSb[b_]], w=[eb])
                        k.act(lambda e: e.activation(out=EM[:], in_=P3[:, :, 0, :], func=AF.Exp, scale=-SDEC), r=[PSb[b_]], w=[eb])
                        k.act(lambda e: e.activation(out=EX[:], in_=P3[:, :, 1, :], func=AF.Exp, scale=SDEC), r=[PSb[b_]], w=[eb])
                        yield
                        for c in range(2):
                            mm(PS[c_][:, c * 128:(c + 1) * 128], A2[64:128, c * 128:(c + 1) * 128], XM[64:128, 6, :], True, True, [pb, xmb], [PSb[c_]], sw=True)
                        for c in range(2):
                            k.act(lambda e: e.activation(out=AT[:, c, :], in_=PS[c_][:, c * 128:(c + 1) * 128], func=AF.Sigmoid, bias=A0[:, c:c + 1]), r=[PSb[c_], pb], w=[ab])
                        yield
                        k.dve(lambda e: e.tensor_tensor(out=KK0[:], in0=XM[:, 2:4, :], in1=bc2(KKp), op=ALU.mult), r=[xmb, pb], w=[kb])
                        k.dve(lambda e: e.tensor_tensor(out=SQ[:], in0=KK0[:], in1=KK0[:], op=ALU.mult), r=[kb], w=[kb])
                        for c in range(2):
                            mm(PS[c_][:, 256 + c * 128:256 + (c + 1) * 128], BLK[:], SQ[:, c, :], True, True, [cb, kb], [PSb[c_]])
                        k.act(lambda e: e.activation(out=SQ[:], in_=v4(PS[c_][:, 256:512], 2), func=AF.Sqrt), r=[PSb[c_], kb], w=[kb])
                        yield
                        k.dve(lambda e: e.tensor_scalar(out=SQ[:], in0=SQ[:], scalar1=1e-12, scalar2=None, op0=ALU.max), r=[kb], w=[kb])
                        k.dve(lambda e: e.reciprocal(out=SQ[:], in_=SQ[:]), r=[kb], w=[kb])
                        k.dve(lambda e: e.tensor_tensor(out=KK0[:], in0=KK0[:], in1=SQ[:], op=ALU.mult), r=[kb], w=[kb])
                        yield
                        k.dve(lambda e: e.tensor_tensor(out=T1[:], in0=AT[:], in1=bc2(KA), op=ALU.mult), r=[ab, pb], w=[kb])
                        k.dve(lambda e: e.tensor_tensor(out=T1[:], in0=T1[:], in1=bc2(OMK), op=ALU.add), r=[kb, pb], w=[kb])
                        k.dve(lambda e: e.tensor_tensor(out=K2[:], in0=XM[:, 2:4, :], in1=T1[:], op=ALU.mult), r=[xmb, kb], w=[kb])
                        yield
                        if tt >= 2:
                            while not chain_done[tt - 2]:
                                yield
                        PCb = EP[:, :, 127:128].to_broadcast([128, 2, 128])
                        k.dve(lambda e: e.tensor_copy(out=PCs[:], in_=EP[:, :, 127]), r=[eb], w=[tmb])
                        k.dve(lambda e: e.tensor_tensor(out=BRt[:, :, 1, :], in0=XM[:, 0:2, :], in1=EP[:], op=ALU.mult), r=[xmb, eb], w=[opb])
                        k.dve(lambda e: e.tensor_tensor(out=BRt[:, :, 0, :], in0=KK0[:], in1=EX[:], op=ALU.mult), r=[kb, eb], w=[opb])
                        yield
                        k.dve(lambda e: e.tensor_tensor(out=KT32[:], in0=K2[:], in1=EM[:], op=ALU.mult), r=[kb, eb], w=[opb])
                        k.act(lambda e: e.activation(out=KTt[:], in_=KT32[:], func=AF.Copy), r=[opb], w=[opb])
                        k.dve(lambda e: e.tensor_tensor(out=KT32[:], in0=KT32[:], in1=PCb, op=ALU.mult), r=[opb, eb], w=[opb])
                        yield
                        k.dve(lambda e: e.scalar_tensor_tensor(out=T1[:], in0=KK0[:], scalar=-1.0, in1=AT[:], op0=ALU.mult, op1=ALU.mult), r=[kb, ab], w=[kb])
                        k.dve(lambda e: e.tensor_tensor(out=AL32[:], in0=T1[:], in1=EM[:], op=ALU.mult), r=[kb, eb], w=[opb])
                        k.act(lambda e: e.activation(out=ALt[:], in_=AL32[:], func=AF.Copy), r=[opb], w=[opb])
                        k.dve(lambda e: e.tensor_tensor(out=AL32[:], in0=AL32[:], in1=PCb, op=ALU.mult), r=[opb, eb], w=[opb])
                        yield
                        for c in range(2):
                            k.pe(lambda e: e.transpose(out=PS[a_][:, c * 128:(c + 1) * 128], in_=KT32[:, c, :], identity=IDF[:]), r=[opb, cb], w=[PSb[a_]])
                            k.pe(lambda e: e.transpose(out=PS[a_][:, 256 + c * 128:256 + (c + 1) * 128], in_=AL32[:, c, :], identity=IDF[:]), r=[opb, cb], w=[PSb[a_]])
                            k.pe(lambda e: e.transpose(out=PS[b_][:, c * 128:(c + 1) * 128], in_=XM[:, 4 + c, :], identity=IDF[:]), r=[xmb, cb], w=[PSb[b_]])
                        k.act(lambda e: e.activation(out=KPT[:], in_=PS[a_][:, 0:256], func=AF.Copy), r=[PSb[a_]], w=[tmb])
                        k.act(lambda e: e.activation(out=APT[:], in_=PS[a_][:, 256:512], func=AF.Copy), r=[PSb[a_]], w=[tmb])
                        k.act(lambda e: e.activation(out=VT[:], in_=PS[b_][:, 0:256], func=AF.Copy), r=[PSb[b_]], w=[tmb])
                        k.dve(lambda e: e.tensor_copy(out=V32[:], in_=PS[b_][:, 0:256]), r=[PSb[b_]], w=[tmb])
                        yield
                        k.dve(lambda e: e.tensor_tensor(out=SQ[:], in0=XM[:, 0:2, :], in1=K2[:], op=ALU.mult), r=[xmb, kb], w=[kb])
                        k.dve(lambda e: e.tensor_tensor(out=SQ[:], in0=SQ[:], in1=bc2(RKp), op=ALU.mult), r=[kb, pb], w=[kb])
                        for c in range(2):
                            mm(PS[b_][:, 256 + 2 * c:256 + 2 * c + 2], SQ[:, c, :], HSEL[:], True, True, [kb, cb], [PSb[b_]])
                        k.act(lambda e: e.activation(out=BON[:], in_=PS[b_][:, 256:260], func=AF.Copy), r=[PSb[b_]], w=[tmb])
                        yield
                        k.act(lambda e: e.activation(out=SZ[:], in_=XM[:, 7, :], func=AF.Sigmoid), r=[xmb], w=[tmb])
                        mm(PS[c_][:, 0:256], SZ[:], G2[:], True, True, [tmb, pb], [PSb[c_]])
                        k.act(lambda e: e.activation(out=GTM_[:], in_=PS[c_][:, 0:256], func=AF.Copy), r=[PSb[c_]], w=[tmb])
                        yield
                        for h in range(4):
                            c = h // 2
                            bk = a_ if h < 2 else b_
                            mm(PS[bk][:, (h % 2) * 256:(h % 2 + 1) * 256], KTt[hsl(h), c, :], BRt[hsl(h), c].rearrange("p t n -> p (t n)"), True, True, [opb], [PSb[bk]], sw=True)
                        for i_, bk in enumerate((a_, b_)):
                            k.dve(lambda e: e.tensor_tensor(out=A1m[:, 2 * i_:2 * i_ + 2, :], in0=v4(PS[bk][:], 2), in1=MLTLE[:].unsqueeze(1).to_broadcast([128, 2, 256]), op=ALU.mult), r=[PSb[bk], pb], w=[a1b])
                        yield
                        for h in range(4):
                            c = h // 2
                            bk = a_ if h < 2 else b_
                            mm(PS[bk][:, (h % 2) * 256:(h % 2 + 1) * 256], ALt[hsl(h), c, :], BRt[hsl(h), c].rearrange("p t n -> p (t n)"), True, True, [opb], [PSb[bk]], sw=True)
                        for i_, bk in enumerate((a_, b_)):
                            k.dve(lambda e: e.tensor_tensor(out=A2m[:, 2 * i_:2 * i_ + 2, :], in0=v4(PS[bk][:], 2), in1=MLTLE[:].unsqueeze(1).to_broadcast([128, 2, 256]), op=ALU.mult), r=[PSb[bk], pb], w=[a2b])
                        yield
                        for h in range(4):
                            c = h // 2
                            mm(PS[b_][:, h * 128:(h + 1) * 128], BRt[hsl(h), c, 0, :], ALt[hsl(h), c, :], True, True, [opb], [PSb[b_]], sw=True)
                        k.dve(lambda e: e.tensor_tensor(out=Z[0][:], in0=v4(PS[b_][:], 4), in1=MGT[:].unsqueeze(1).to_broadcast([128, 4, 128]), op=ALU.mult), r=[PSb[b_], cb], w=[zb[0]])
                        k.dve(lambda e: e.tensor_tensor(out=TT[0][:], in0=A2m[:, :, 0:128], in1=IDF[:].unsqueeze(1).to_broadcast([128, 4, 128]), op=ALU.add), r=[a2b, cb], w=[ttb[0]])
                        yield
                        for i_ in range(1, 7):
                            o_, n_ = (i_ - 1) % 2, i_ % 2
                            if i_ == 1:
                                zt_old, zt_oldb = (lambda h: A2m[:, h, 0:128]), a2b
                            else:
                                zt_old, zt_oldb = (lambda h, o_=o_: ZT[o_][:, h, :]), ztb[o_]
                            for h in range(4):
                                mm(PS[a_][:, h * 128:(h + 1) * 128], zt_old(h), Z[o_][:, h, :], True, True, [zt_oldb, zb[o_]], [PSb[a_]])
                            k.act(lambda e: e.activation(out=Z[n_][:], in_=v4(PS[a_][:], 4), func=AF.Copy), r=[PSb[a_]], w=[zb[n_]])
                            if i_ < 6:
                                for h in range(4):
                                    mm(PS[b_][:, h * 128:(h + 1) * 128], Z[o_][:, h, :], zt_old(h), True, True, [zt_oldb, zb[o_]], [PSb[b_]])
                                k.dve(lambda e: e.tensor_copy(out=ZT[n_][:], in_=v4(PS[b_][:], 4)), r=[PSb[b_]], w=[ztb[n_]])
                            yield
                            for h in range(4):
                                mm(PS[c_][:, h * 128:(h + 1) * 128], Z[n_][:, h, :], TT[o_][:, h, :], True, True, [zb[n_], ttb[o_]], [PSb[c_]])
                            k.dve(lambda e: e.tensor_tensor(out=TT[n_][:], in0=v4(PS[c_][:], 4), in1=TT[o_][:], op=ALU.add), r=[PSb[c_], ttb[o_]], w=[ttb[n_]])
                            yield
                        prep_done[tt] = True
                        yield

                def chain_gen():
                    for tt in range(NT):
                        while not prep_done[tt]:
                            yield
                        st = sets[tt % 2]
                        tsl = slice(tt * 128, (tt + 1) * 128)
                        BRt, A1m, A2m, KPT, APT, VT, V32, BON, GTM_, PCs = st.BRt, st.A1m, st.A2m, st.KPT, st.APT, st.VT, st.V32, st.BON, st.GTM_, st.PCs
                        opb, tmb, a1b, a2b = st.opb, st.tmb, st.a1b, st.a2b
                        TTf, ttfb = st.TT[0], st.ttb[0]
                        for h in range(4):
                            c = h // 2
                            mm(PS[4][:, hc(h)], BRt[hsl(h), c, 0, :], MBF[hsl(h), c, :], True, False, [opb, mbfb], [PSb[4]], sw=True)
                            mm(PS[4][:, hc(h)], A1m[:, h, 0:128], VT[:, hc(h)], False, True, [a1b, tmb], [PSb[4]])
                        k.act(lambda e: e.activation(out=X0[:], in_=v4(PS[4][:, 0:256], 4), func=AF.Copy), r=[PSb[4]], w=[x0b])
                        yield
                        for h in range(4):
                            mm(PS[4][:, 256 + h * 64:256 + (h + 1) * 64], TTf[:, h, :], X0[:, h, :], True, True, [ttfb, x0b], [PSb[4]])
                        k.act(lambda e: e.activation(out=UH[:], in_=v4(PS[4][:, 256:512], 4), func=AF.Copy), r=[PSb[4]], w=[uhb])
                        yield
                        for h in range(4):
                            c = h // 2
                            mm(PS[5][hsl(h), 256 + c * 64:256 + (c + 1) * 64], KPT[:, hc(h)], VT[:, hc(h)], True, False, [tmb], [PSb[5]])
                            mm(PS[5][hsl(h), 256 + c * 64:256 + (c + 1) * 64], APT[:, hc(h)], UH[:, h, :], False, True, [tmb, uhb], [PSb[5]])
                        for h in range(4):
                            c = h // 2
                            mm(PS[5][:, hc(h)], BRt[hsl(h), c, 1, :], MBF[hsl(h), c, :], True, False, [opb, mbfb], [PSb[5]], sw=True)
                            mm(PS[5][:, hc(h)], A1m[:, h, 128:256], VT[:, hc(h)], False, False, [a1b, tmb], [PSb[5]])
                            mm(PS[5][:, hc(h)], A2m[:, h, 128:256], UH[:, h, :], False, True, [a2b, uhb], [PSb[5]])
                        yield
                        k.dve(lambda e: e.tensor_tensor(out=M32[:], in0=M32[:], in1=PCs[:].unsqueeze(2).to_broadcast([128, 2, 64]), op=ALU.mult), r=[mb, tmb], w=[mb])
                        k.dve(lambda e: e.tensor_tensor(out=M32[:], in0=M32[:], in1=v4(PS[5][:, 256:384], 2), op=ALU.add), r=[mb, PSb[5]], w=[mb])
                        k.act(lambda e: e.activation(out=MBF[:], in_=M32[:], func=AF.Copy), r=[mb], w=[mbfb])
                        k.act(lambda e: e.activation(out=Y[:], in_=v4(PS[5][:, 0:256], 4), func=AF.Copy), r=[PSb[5]], w=[yb])
                        yield
                        k.dve(lambda e: e.tensor_reduce(out=S1[:], in_=Y[:], axis=AX.X, op=ALU.add), r=[yb], w=[yb])
                        k.dve(lambda e: e.tensor_tensor(out=SQY[:], in0=Y[:], in1=Y[:], op=ALU.mult), r=[yb], w=[yb])
                        k.dve(lambda e: e.tensor_reduce(out=S2[:], in_=SQY[:], axis=AX.X, op=ALU.add), r=[yb], w=[yb])
                        k.dve(lambda e: e.tensor_scalar(out=S1[:], in0=S1[:], scalar1=1.0 / 64, scalar2=None, op0=ALU.mult), r=[yb], w=[yb])
                        yield
                        k.dve(lambda e: e.tensor_tensor(out=MS[:], in0=S1[:], in1=S1[:], op=ALU.mult), r=[yb], w=[yb])
                        k.dve(lambda e: e.scalar_tensor_tensor(out=S2[:], in0=S2[:], scalar=1.0 / 64, in1=MS[:], op0=ALU.mult, op1=ALU.subtract), r=[yb], w=[yb])
                        k.act(lambda e: e.activation(out=S2[:], in_=S2[:], func=AF.Sqrt, bias=RWKV_GN_EPS), r=[yb], w=[yb])
                        k.dve(lambda e: e.reciprocal(out=S2[:], in_=S2[:]), r=[yb], w=[yb])
                        yield
                        k.dve(lambda e: e.tensor_tensor(out=Y[:], in0=Y[:], in1=S1[:].unsqueeze(2).to_broadcast([128, 4, 64]), op=ALU.subtract), r=[yb], w=[yb])
                        k.dve(lambda e: e.tensor_tensor(out=Y[:], in0=Y[:], in1=S2[:].unsqueeze(2).to_broadcast([128, 4, 64]), op=ALU.mult), r=[yb], w=[yb])
                        Yf = Y[:].rearrange("p h d -> p (h d)")
                        k.dve(lambda e: e.tensor_tensor(out=Yf, in0=Yf, in1=LNG[:], op=ALU.mult), r=[yb, pb], w=[yb])
                        k.dve(lambda e: e.tensor_tensor(out=Yf, in0=Yf, in1=LNB[:], op=ALU.add), r=[yb, pb], w=[yb])
                        yield
                        k.dve(lambda e: e.tensor_tensor(out=SQY[:], in0=v4(V32[:], 4), in1=BON[:].unsqueeze(2).to_broadcast([128, 4, 64]), op=ALU.mult), r=[tmb, yb], w=[yb])
                        k.dve(lambda e: e.tensor_tensor(out=Y[:], in0=Y[:], in1=SQY[:], op=ALU.add), r=[yb], w=[yb])
                        k.dve(lambda e: e.tensor_tensor(out=Yf, in0=Yf, in1=GTM_[:], op=ALU.mult), r=[yb, tmb], w=[yb])
                        for cc in range(2):
                            k.pe(lambda e: e.transpose(out=PS[4][:, cc * 128:(cc + 1) * 128], in_=Yf[:, cc * 128:(cc + 1) * 128], identity=IDF[:]), r=[yb, cb], w=[PSb[4]])
                        k.act(lambda e: e.activation(out=BR[:, 6:8, tsl], in_=v4(PS[4][:, 0:256], 2), func=AF.Copy), r=[PSb[4]], w=[BRb])
                        chain_done[tt] = True
                        yield

                run_gens([prep_gen(0), prep_gen(1), chain_gen()] + list(extra_gens), weights=[int(x) for x in os.environ.get('K_W', '1,1,1,1').split(',')][:3 + len(extra_gens)])
                k.barrier()

        def residual_stats(tt, ysrc_fn, yrb, JK, jb, SSY, RSY, ssb_):
            ssb = ssb_[tt % len(ssb_)]
            for half in range(2):
                k.act(lambda e: e.activation(out=JK[:, 0:512], in_=ysrc_fn(half), func=AF.Square, accum_out=SSY[:, 2 * tt + half:2 * tt + half + 1]), r=[yrb[half]], w=[jb, ssb])
            k.dve(lambda e: e.tensor_tensor(out=RSY[:, tt:tt + 1], in0=SSY[:, 2 * tt:2 * tt + 1], in1=SSY[:, 2 * tt + 1:2 * tt + 2], op=ALU.add), r=[ssb], w=[ssb])
            k.act(lambda e: e.activation(out=RSY[:, tt:tt + 1], in_=RSY[:, tt:tt + 1], func=AF.Sqrt, scale=1.0 / D, bias=RMS_EPS), r=[ssb], w=[ssb])
            k.dve(lambda e: e.reciprocal(out=RSY[:, tt:tt + 1], in_=RSY[:, tt:tt + 1]), r=[ssb], w=[ssb])

        def residual_apply(tt, ysrc_fn, yrb, GT_, modb, xdst, xdstb, XR, xrb, YT, ytb, RSY, ssb_):
            ssb = ssb_[tt % len(ssb_)]
            s = tt % 2
            x_ = tt % len(XR)
            for half in range(2):
                hs = slice(half * 512, (half + 1) * 512)
                k.dve(lambda e: e.scalar_tensor_tensor(out=YT[s][:, hs], in0=ysrc_fn(half), scalar=RSY[:, tt:tt + 1], in1=GT_[:, hs], op0=ALU.mult, op1=ALU.mult), r=[yrb[half], ssb, modb], w=[ytb[s]])
            k.pool(lambda e: e.tensor_tensor(out=XR[x_][:], in0=XR[x_][:], in1=YT[s][:], op=ALU.add), r=[xrb[x_], ytb[s]], w=[xrb[x_]])
            k.dma("sp", xdst[tt * 128:(tt + 1) * 128, :], XR[x_][:], reads=[xrb[x_]], writes=[xdstb[tt]])

        def phase_merge(l, BR, BRb, GTM, modb, xsrc, xsrcb, xdst, xdstb):
            with ExitStack() as ph:
                MT = sb(ph, "mt", [128, 8, T], BF16)
                mtb = [Buf() for _ in range(NB)]
                WO = sb(ph, "wo", [128, 8, D], BF16)
                GW = [sb(ph, "gwt%d" % i_, [128, 4, 8, 128], BF16) for i_ in range(2)]
                WB = [sb(ph, "wbt%d" % i_, [128, 4, 2, 128], BF16) for i_ in range(2)]
                SGm = [sb(ph, "msg%d" % i_, [128, 512]) for i_ in range(2)]
                TMP = [sb(ph, "mtmp%d" % i_, [128, 512]) for i_ in range(2)]
                ACC = [sb(ph, "macc%d" % i_, [128, 512]) for i_ in range(2)]
                XR = [sb(ph, "mxr%d" % i_, [128, D]) for i_ in range(4)]
                YT = [sb(ph, "myt%d" % i_, [128, D]) for i_ in range(2)]
                JK = sb(ph, "mjk", [128, 512], BF16)
                SSY = sb(ph, "mssy", [128, 2 * NT])
                RSY = sb(ph, "mrsy", [128, NT])
                wbb, sgb, tmpb, accb, ytb = ([Buf(), Buf()] for _ in range(5))
                xrb = [Buf() for _ in range(4)]
                gwb = [[Buf() for _ in range(4)] for _ in range(2)]
                wob, jb = Buf(), Buf()
                ssb = [Buf() for _ in range(4)]
                k.pool(lambda e: e.memset(SSY[:], 0.0), w=ssb)
                wbv = dr["w_branch"][l].rearrange("g (kc p) n -> p g kc n", p=128)

                def load(j_):
                    s_ = j_ % 2
                    if j_ == 0:
                        for g_ in range(4):
                            k.dma("pool", GW[s_][:, g_], dr["w_gate"][l, j_][:, g_], writes=[gwb[s_][g_]])
                    else:
                        k.dma("pool", GW[s_][:], dr["w_gate"][l, j_], writes=gwb[s_])
                    k.dma("pool", WB[s_][:], wbv[:, :, :, j_ * 128:(j_ + 1) * 128], writes=[wbb[s_]])

                load(0)
                cnt = 0
                for j_ in range(8):
                    s = j_ % 2
                    if j_ + 1 < 8:
                        load(j_ + 1)
                    else:
                        k.dma("pool", WO[:], dr["w_out"][l].rearrange("(kc p) n -> p kc n", p=128), writes=[wob])
                    for tb in range(NB):
                        sl = slice(tb * 512, (tb + 1) * 512)
                        a = tb % 2
                        for g in range(4):
                            i2 = cnt % 2
                            cnt += 1
                            pg, pgb = PS[i2], PSb[i2]
                            pbr, pbrb = PS[2 + i2], PSb[2 + i2]
                            for kc in range(8):
                                mm(pg[:], GW[s][:, g, kc, :], HT[:, kc, sl], kc == 0, kc == 7, [gwb[s][g], HTb], [pgb])
                            for kc2 in range(2):
                                mm(pbr[:], WB[s][:, g, kc2, :], BR[:, 2 * g + kc2, sl], kc2 == 0, kc2 == 1, [wbb[s], BRb], [pbrb])
                            k.act(lambda e: e.activation(out=SGm[i2][:], in_=pg[:], func=AF.Sigmoid), r=[pgb], w=[sgb[i2]])
                            if g == 0:
                                k.dve(lambda e: e.tensor_tensor(out=ACC[a][:], in0=pbr[:], in1=SGm[i2][:], op=ALU.mult), r=[pbrb, sgb[i2]], w=[accb[a]])
                            else:
                                k.dve(lambda e: e.tensor_tensor(out=TMP[i2][:], in0=pbr[:], in1=SGm[i2][:], op=ALU.mult), r=[pbrb, sgb[i2]], w=[tmpb[i2]])
                                if g < 3:
                                    k.pool(lambda e: e.tensor_tensor(out=ACC[a][:], in0=ACC[a][:], in1=TMP[i2][:], op=ALU.add), r=[accb[a], tmpb[i2]], w=[accb[a]])
                                else:
                                    k.pool(lambda e: e.tensor_tensor(out=MT[:, j_, sl], in0=ACC[a][:], in1=TMP[i2][:], op=ALU.add), r=[accb[a], tmpb[i2]], w=[mtb[tb]])
                tap("mt%d" % l, MT[:], mtb[3], [128, 8, T])
                def wo_mm(tt):
                    s = tt % 2
                    for half in range(2):
                        for j_ in range(8):
                            mm(PS[4 + 2 * s + half][:], MT[:, j_, tt * 128:(tt + 1) * 128], WO[:, j_, half * 512:(half + 1) * 512], j_ == 0, j_ == 7, [mtb[tt // 4], wob], [PSb[4 + 2 * s + half]])

                def ysrc(tt):
                    s = tt % 2
                    return (lambda half: PS[4 + 2 * s + half][:]), [PSb[4 + 2 * s + h_] for h_ in range(2)]

                def x_load(tt):
                    k.dma("sp", XR[tt % 4][:], xsrc[tt * 128:(tt + 1) * 128, :], reads=[xsrcb[tt]], writes=[xrb[tt % 4]])

                x_load(0)
                x_load(1)
                for step in range(NT + 2):
                    if step >= 2:
                        fn_, bf_ = ysrc(step - 2)
                        residual_apply(step - 2, fn_, bf_, GTM, modb, xdst, xdstb, XR, xrb, YT, ytb, RSY, ssb)
                    if step + 2 < NT:
                        x_load(step + 2)
                    if step < NT:
                        wo_mm(step)
                    if 1 <= step <= NT:
                        fn_, bf_ = ysrc(step - 1)
                        residual_stats(step - 1, fn_, bf_, JK, jb, SSY, RSY, ssb)
                k.barrier()

        def phase_ffn(l, moe, MODT, GTF, modb, xsrc, xsrcb, xdst, xdstb):
            with ExitStack() as ph:
                YA = sb(ph, "ya", [128, NT, D])
                yab = [Buf() for _ in range(NT)]
                COMB = sb(ph, "comb", [128, NT, 8])
                combb = Buf()
                if moe:
                    RW = sb(ph, "rwt", [128, 8, 8])
                    RB = sb(ph, "rbt", [128, 8])
                    RT = sb(ph, "rtmp", [128, 8, 8])
                    rb_, rtb = Buf(), Buf()
                    k.dma("sp", RW[:], dr["router_wt"][0], writes=[rb_])
                    k.dma("sp", RB[:], dr["router_b"][0:1, :].partition_broadcast(128), writes=[rb_])

                    def router(tt, H32, h32b):
                        LGt, EQ1, L2, EQ2 = RT[:, 0, :], RT[:, 1, :], RT[:, 2, :], RT[:, 3, :]
                        M1, M2, DD, P1, P2 = RT[:, 4, 0:1], RT[:, 4, 1:2], RT[:, 4, 2:3], RT[:, 4, 3:4], RT[:, 4, 4:5]
                        for kc in range(8):
                            mm(PS[7][:, 0:8], H32[:, kc, :], RW[:, kc, :], kc == 0, kc == 7, [h32b, rb_], [PSb[7]])
                        k.dve(lambda e: e.tensor_tensor(out=LGt, in0=PS[7][:, 0:8], in1=RB[:], op=ALU.add), r=[PSb[7], rb_], w=[rtb])
                        k.dve(lambda e: e.tensor_reduce(out=M1, in_=LGt, axis=AX.X, op=ALU.max), r=[rtb], w=[rtb])
                        k.dve(lambda e: e.tensor_scalar(out=EQ1, in0=LGt, scalar1=M1, scalar2=None, op0=ALU.is_equal), r=[rtb], w=[rtb])
                        k.dve(lambda e: e.scalar_tensor_tensor(out=L2, in0=EQ1, scalar=-1e30, in1=LGt, op0=ALU.mult, op1=ALU.add), r=[rtb], w=[rtb])
                        k.dve(lambda e: e.tensor_reduce(out=M2, in_=L2, axis=AX.X, op=ALU.max), r=[rtb], w=[rtb])
                        k.dve(lambda e: e.tensor_scalar(out=EQ2, in0=L2, scalar1=M2, scalar2=None, op0=ALU.is_equal), r=[rtb], w=[rtb])
                        k.dve(lambda e: e.tensor_tensor(out=DD, in0=M2, in1=M1, op=ALU.subtract), r=[rtb], w=[rtb])
                        k.act(lambda e: e.activation(out=DD, in_=DD, func=AF.Exp), r=[rtb], w=[rtb])
                        k.dve(lambda e: e.tensor_scalar(out=P1, in0=DD, scalar1=1.0, scalar2=None, op0=ALU.add), r=[rtb], w=[rtb])
                        k.dve(lambda e: e.reciprocal(out=P1, in_=P1), r=[rtb], w=[rtb])
                        k.dve(lambda e: e.tensor_scalar(out=P2, in0=P1, scalar1=-1.0, scalar2=1.0, op0=ALU.mult, op1=ALU.add), r=[rtb], w=[rtb])
                        k.dve(lambda e: e.tensor_scalar(out=COMB[:, tt, :], in0=EQ1, scalar1=P1, scalar2=None, op0=ALU.mult), r=[rtb], w=[combb])
                        k.dve(lambda e: e.scalar_tensor_tensor(out=COMB[:, tt, :], in0=EQ2, scalar=P2, in1=COMB[:, tt, :], op0=ALU.mult, op1=ALU.add), r=[rtb, combb], w=[combb])
                else:
                    router = None
                phase_prenorm(xsrc, xsrcb, dr["nfp_t"][l], 32, 24, MODT, modb, router=router)
                tap("htf%d" % l, HT[:], HTb, [128, 8, T])
                if moe:
                    tap("comb%d" % l, COMB[:], combb, [128, NT, 8])
                with ExitStack() as p1:
                    WGt = [sb(p1, "fwg%d" % i_, [128, 4, 8, 128], BF16) for i_ in range(2)]
                    WUt = [sb(p1, "fwu%d" % i_, [128, 4, 8, 128], BF16) for i_ in range(2)]
                    WDt = [sb(p1, "fwd%d" % i_, [128, 4, D], BF16) for i_ in range(2)]
                    AT = sb(p1, "fat", [128, 4, T], BF16)
                    SGf = [sb(p1, "fsg%d" % i_, [128, 512]) for i_ in range(2)]
                    sgb = [Buf(), Buf()]
                    wgb, wub, wdb = ([[Buf() for _ in range(4)] for _ in range(2)] for _ in range(3))
                    atb = [Buf() for _ in range(4)]
                    groups = []
                    if moe:
                        for e_ in range(N_EXP):
                            for fc0 in range(0, 28, 4):
                                groups.append((e_, fc0, 4))
                    else:
                        for fc0 in range(0, 22, 4):
                            groups.append((None, fc0, min(4, 22 - fc0)))

                    def load(gi):
                        e_, fc0, F = groups[gi]
                        s_ = gi % 2
                        if moe:
                            wg_ap = dr["moe_wg"][0, e_, fc0:fc0 + F].rearrange("fc p kc f -> p fc kc f")
                            wu_ap = dr["moe_wu"][0, e_, fc0:fc0 + F].rearrange("fc p kc f -> p fc kc f")
                            wd_ap = dr["moe_wd"][0, e_, fc0 * 128:(fc0 + F) * 128, :].rearrange("(fc p) d -> p fc d", p=128)
                        else:
                            wg_ap = dr["ffn_wg"][0, fc0:fc0 + F].rearrange("fc p kc f -> p fc kc f")
                            wu_ap = dr["ffn_wu"][0, fc0:fc0 + F].rearrange("fc p kc f -> p fc kc f")
                            wd_ap = dr["ffn_wd"][0, fc0 * 128:(fc0 + F) * 128, :].rearrange("(fc p) d -> p fc d", p=128)
                        if gi == 0:
                            for fi_ in range(F):
                                k.dma("pool", WGt[s_][:, fi_], wg_ap[:, fi_], writes=[wgb[s_][fi_]])
                                k.dma("pool", WUt[s_][:, fi_], wu_ap[:, fi_], writes=[wub[s_][fi_]])
                            for fi_ in range(F):
                                k.dma("pool", WDt[s_][:, fi_], wd_ap[:, fi_], writes=[wdb[s_][fi_]])
                        else:
                            k.dma("pool", WGt[s_][:, 0:F], wg_ap, writes=wgb[s_][0:F])
                            k.dma("pool", WUt[s_][:, 0:F], wu_ap, writes=wub[s_][0:F])
                            k.dma("pool", WDt[s_][:, 0:F], wd_ap, writes=wdb[s_][0:F])

                    load(0)
                    cnt = 0
                    for gi, (e_, fc0, F) in enumerate(groups):
                        s = gi % 2
                        if gi + 1 < len(groups):
                            load(gi + 1)
                        for fi in range(F):
                            for tb in range(NB):
                                sl = slice(tb * 512, (tb + 1) * 512)
                                i2 = cnt % 2
                                cnt += 1
                                pg, pgb = PS[i2], PSb[i2]
                                pu, pub = PS[2 + i2], PSb[2 + i2]
                                for kc in range(8):
                                    mm(pg[:], WGt[s][:, fi, kc, :], HT[:, kc, sl], kc == 0, kc == 7, [wgb[s][fi], HTb], [pgb])
                                for kc in range(8):
                                    mm(pu[:], WUt[s][:, fi, kc, :], HT[:, kc, sl], kc == 0, kc == 7, [wub[s][fi], HTb], [pub])
                                k.act(lambda e: e.activation(out=SGf[i2][:], in_=pg[:], func=AF.Silu), r=[pgb], w=[sgb[i2]])
                                k.dve(lambda e: e.tensor_tensor(out=AT[:, fi, sl], in0=pu[:], in1=SGf[i2][:], op=ALU.mult), r=[pub, sgb[i2]], w=[atb[fi]])
                        for tt in range(NT):
                            for half in range(2):
                                hs = slice(half * 512, (half + 1) * 512)
                                i4 = (2 * tt + half) % 4
                                py, pyb = PS[4 + i4], PSb[4 + i4]
                                for fi in range(F):
                                    mm(py[:], AT[:, fi, tt * 128:(tt + 1) * 128], WDt[s][:, fi, hs], fi == 0, fi == F - 1, [atb[fi], wdb[s][fi]], [pyb])
                                if gi == 0:
                                    if moe:
                                        k.dve(lambda e: e.tensor_scalar(out=YA[:, tt, hs], in0=py[:], scalar1=COMB[:, tt, e_:e_ + 1], scalar2=None, op0=ALU.mult), r=[pyb, combb], w=[yab[tt]])
                                    else:
                                        k.act(lambda e: e.activation(out=YA[:, tt, hs], in_=py[:], func=AF.Copy), r=[pyb], w=[yab[tt]])
                                elif moe:
                                    k.dve(lambda e: e.scalar_tensor_tensor(out=YA[:, tt, hs], in0=py[:], scalar=COMB[:, tt, e_:e_ + 1], in1=YA[:, tt, hs], op0=ALU.mult, op1=ALU.add), r=[pyb, combb, yab[tt]], w=[yab[tt]])
                                else:
                                    k.dve(lambda e: e.tensor_tensor(out=YA[:, tt, hs], in0=py[:], in1=YA[:, tt, hs], op=ALU.add), r=[pyb, yab[tt]], w=[yab[tt]])
                    k.barrier()
                tap("ya%d" % l, YA[:], yab[NT - 1], [128, NT, D])
                with ExitStack() as p2:
                    XR = [sb(p2, "fxr%d" % i_, [128, D]) for i_ in range(4)]
                    YT = [sb(p2, "fyt%d" % i_, [128, D]) for i_ in range(2)]
                    JK = sb(p2, "fjk", [128, 512], BF16)
                    SSY = sb(p2, "fssy", [128, 2 * NT])
                    RSY = sb(p2, "frsy", [128, NT])
                    xrb, ytb = [Buf() for _ in range(4)], [Buf(), Buf()]
                    jb = Buf()
                    ssb = [Buf() for _ in range(4)]
                    k.pool(lambda e: e.memset(SSY[:], 0.0), w=ssb)
                    def x_load(tt):
                        k.dma("sp", XR[tt % 4][:], xsrc[tt * 128:(tt + 1) * 128, :], reads=[xsrcb[tt]], writes=[xrb[tt % 4]])

                    yfn = (lambda tt_: (lambda half: YA[:, tt_, half * 512:(half + 1) * 512]))
                    x_load(0)
                    x_load(1)
                    x_load(2)
                    for tt in range(NT + 1):
                        if tt < NT:
                            residual_stats(tt, yfn(tt), [yab[tt], yab[tt]], JK, jb, SSY, RSY, ssb)
                        if tt >= 1:
                            residual_apply(tt - 1, yfn(tt - 1), [yab[tt - 1], yab[tt - 1]], GTF, modb, xdst, xdstb, XR, xrb, YT, ytb, RSY, ssb)
                            if tt + 2 < NT:
                                x_load(tt + 2)
                    k.barrier()

        MODT = sb(es, "modt", [128, 48])
        GTM = sb(es, "gtm", [128, D])
        GTF = sb(es, "gtf", [128, D])
        modb = Buf("mod")
        xin_b = [Buf() for _ in range(NT)]
        xs_b = [[Buf() for _ in range(NT)] for _ in range(2)]
        yout_b = [Buf() for _ in range(NT)]
        chain = [(dr["x"], xin_b), (xs[0], xs_b[0]), (xs[1], xs_b[1]), (xs[0], xs_b[0]), (y_out, yout_b)]
        import os
        nlay = int(os.environ.get("K_NLAYERS", str(DEPTH)))
        skip_ffn = os.environ.get("K_SKIP_FFN", "0") == "1"
        for l in range(nlay):
            (xa, xab), (xm_, xmb_), (xf, xfb) = chain[2 * l], chain[2 * l + 1], chain[2 * l + 2]
            if skip_ffn or (nlay < DEPTH and l == nlay - 1 and False):
                pass
            phase_mod(l, None, MODT, GTM, GTF, modb)
            with ExitStack() as mixs:
                BR = sb(mixs, "br", [128, 8, T], BF16)
                BRb = Buf("br")
                phase_prenorm(xa, xab, dr["nmp_t"][l], 8, 0, MODT, modb)
                tap("ht%d" % l, HT[:], HTb, [128, 8, T])
                if os.environ.get("K_SEQCONV", "0") == "1":
                    phase_conv(l, BR, BRb)
                    phase_diff(l, BR, BRb)
                else:
                    with ExitStack() as cph:
                        cg = conv_setup(l, cph)(BR, BRb)

                        def pump(n, cg=cg):
                            for _ in range(n):
                                try:
                                    next(cg)
                                except StopIteration:
                                    return

                        phase_diff(l, BR, BRb, pump=pump)
                if os.environ.get("K_RWKV1", "0") == "1":
                    phase_gla(l, BR, BRb)
                    phase_rwkv(l, BR, BRb)
                else:
                    phase_rwkv2(l, BR, BRb, with_gla=True)
                tap("br%d" % l, BR[:], BRb, [128, 8, T])
                last_mix = skip_ffn and l == nlay - 1
                phase_merge(l, BR, BRb, GTM, modb, xa, xab, (y_out if last_mix else xm_), (yout_b if last_mix else xmb_))
            if last_mix:
                break
            last = (l == nlay - 1)
            phase_ffn(l, (l % 2 == 1), MODT, GTF, modb, xm_, xmb_, (y_out if last else xf), (yout_b if last else xfb))
        k.finish(yout_b)
        k.finish(P.tapbufs)
        k.barrier()
        print("instructions", k.nins, "waits", k.nwait)
    return nc, tap_out, list(dr.keys())


def prep_inputs(inputs, b):
    g = lambda n: np.asarray(inputs[n])
    m = {}
    m["x"] = np.ascontiguousarray(g("x")[b])
    m["c_t"] = _pcol(g("c")[b], 8)
    m["pos"] = np.ascontiguousarray(g("positions")[b][None, :]).astype(np.int32)
    m.update(_consts())
    return m


_SHARED = None


def prep_shared(inputs):
    g = lambda n: np.asarray(inputs[n])
    L = DEPTH
    m = {}
    m["ada_w"] = g("ada_w")
    m["ada_bt"] = np.stack([_pcol(g("ada_b")[l], 48) for l in range(L)])
    m["ada_b"] = g("ada_b")
    m["nmp_t"] = np.stack([_pcol(g("norm_mix_pre")[l], 8) for l in range(L)])
    m["nfp_t"] = np.stack([_pcol(g("norm_ffn_pre")[l], 8) for l in range(L)])
    m["norm_mix_post"] = g("norm_mix_post")
    m["norm_ffn_post"] = g("norm_ffn_post")
    w_in = g("w_in")
    m["w_in_a"] = np.ascontiguousarray(w_in[:, :, :N_IN_A])
    wg = w_in[:, :, N_IN_A:].reshape(L, 8, 128, 4, 8, 128)
    m["w_gate"] = np.ascontiguousarray(wg.transpose(0, 4, 2, 3, 1, 5))
    m["gla_gate_w2"] = g("gla_gate_w2")
    m["gla_gate_b"] = g("gla_gate_b")
    m["gla_norm"] = g("gla_norm")
    m["diff_lambda"] = g("diff_lambda").reshape(L, 128)
    m["diff_subln_t"] = np.stack([np.tile(g("diff_subln")[l], 2)[:, None] for l in range(L)])
    m["conv_wt"] = np.ascontiguousarray(g("conv_w").reshape(L, 31, 2, 128).transpose(0, 3, 2, 1))
    m["conv_bt"] = np.stack([_pcol(g("conv_b")[l], 2) for l in range(L)])
    m["conv_lg_t"] = np.stack([_pcol(g("conv_ln_g")[l], 2) for l in range(L)])
    m["conv_lb_t"] = np.stack([_pcol(g("conv_ln_b")[l], 2) for l in range(L)])
    m["rwkv_mu_t"] = np.stack([_pcol(g("rwkv_mu")[l], 8) for l in range(L)])
    m["rwkv_w0"] = g("rwkv_w0")
    m["rwkv_w2"] = g("rwkv_w2")
    m["rwkv_a0_t"] = np.stack([_pcol(g("rwkv_a0")[l], 2) for l in range(L)])
    m["rwkv_a2"] = g("rwkv_a2")
    m["rwkv_g2"] = g("rwkv_g2")
    m["rwkv_kk_t"] = np.stack([_pcol(g("rwkv_k_k")[l], 2) for l in range(L)])
    m["rwkv_ka_t"] = np.stack([_pcol(g("rwkv_k_a")[l], 2) for l in range(L)])
    m["rwkv_rk_t"] = np.stack([_pcol(g("rwkv_r_k")[l].reshape(256), 2) for l in range(L)])
    m["rwkv_ln_g"] = g("rwkv_ln_g")
    m["rwkv_ln_b"] = g("rwkv_ln_b")
    m["w_branch"] = g("w_branch")
    m["w_out"] = g("w_out")
    relay = lambda w, nfc: np.ascontiguousarray(w.reshape(w.shape[:-2] + (8, 128, nfc, 128)).transpose(tuple(range(w.ndim - 2)) + (w.ndim, w.ndim - 1, w.ndim - 2, w.ndim + 1)))
    m["ffn_wg"] = relay(g("ffn_w_gate"), 22)
    m["ffn_wu"] = relay(g("ffn_w_up"), 22)
    m["ffn_wd"] = g("ffn_w_down")
    m["router_wt"] = np.ascontiguousarray(g("router_w").reshape(1, 8, 128, 8).transpose(0, 2, 1, 3))
    m["router_b"] = g("router_b")
    m["moe_wg"] = relay(g("moe_w_gate"), 28)
    m["moe_wu"] = relay(g("moe_w_up"), 28)
    m["moe_wd"] = g("moe_w_down")
    return {k_: np.ascontiguousarray(v, dtype=np.float32) for k_, v in m.items()}


def kernel(**inputs):
    nc, _, used = build_program()
    shared = prep_shared(inputs)
    n = 8
    in_maps = []
    for b in range(n):
        pc = prep_inputs(inputs, b)
        in_maps.append({nm: (pc[nm] if nm in pc else shared[nm]) for nm in used})
    res = run_bass_kernel_spmd(nc, in_maps, core_ids=list(range(n)))
    return np.stack([np.asarray(r["y"], dtype=np.float32) for r in res.results], axis=0)
```

```python
import math
import numpy as np
from contextlib import ExitStack
import concourse.bass as bass
import concourse.mybir as mybir
from concourse.bass_utils import run_bass_kernel_spmd

F32 = mybir.dt.float32
BF16 = mybir.dt.bfloat16
I32 = mybir.dt.int32
AF = mybir.ActivationFunctionType
ALU = mybir.AluOpType
AX = mybir.AxisListType

T = 2048
D = 1024
NT = 16
NB = 4
DEPTH = 2
N_IN_A = 3088
D_FF = 2816
D_EXP = 3584
N_EXP = 8
RMS_EPS = 1e-6
LN_EPS = 1e-5
RWKV_GN_EPS = 64e-5
PI = math.pi


class Buf:
    __slots__ = ("w", "r", "excl", "name")

    def __init__(self, name="", excl=False):
        self.w = None
        self.r = {}
        self.excl = excl
        self.name = name


class Clock:
    def __init__(self, name, sem, eng=None):
        self.name = name
        self.sem = sem
        self.eng = eng
        self.count = 0
        self.seen = {}


class K:
    def __init__(self, nc, es, n_dma_sems=32):
        self.nc = nc
        self.E = {}
        for n, h in (("pe", nc.tensor), ("dve", nc.vector), ("act", nc.scalar), ("pool", nc.gpsimd), ("sp", nc.sync)):
            self.E[n] = Clock(n, es.enter_context(nc.semaphore("s_" + n)), h)
        self.dma_sems = [Clock("d%d" % i, es.enter_context(nc.semaphore("sd%d" % i))) for i in range(n_dma_sems)]
        self.dma_pool = {"sp": self.dma_sems[: n_dma_sems // 2], "pool": self.dma_sems[n_dma_sems // 2:]}
        self.dma_rr = {"sp": 0, "pool": 0}
        self.nins = 0
        self.nwait = 0

    def _deps(self, reads, writes):
        deps = {}

        def add(c, v):
            if deps.get(c, 0) < v:
                deps[c] = v

        for b in reads:
            if b.w is not None:
                add(*b.w)
            if b.excl:
                for c, v in b.r.items():
                    add(c, v)
        for b in writes:
            if b.w is not None:
                add(*b.w)
            for c, v in b.r.items():
                add(c, v)
        return deps

    def _wait(self, e, deps):
        for c, v in deps.items():
            if c is e and e.name == "pe":
                continue
            if e.seen.get(c, 0) >= v:
                continue
            e.eng.wait_ge(c.sem, v)
            e.seen[c] = v
            self.nwait += 1

    def _mark(self, c, v, reads, writes):
        for b in writes:
            b.w = (c, v)
            b.r = {}
        for b in reads:
            if b.excl:
                b.w = (c, v)
                b.r = {}
            else:
                b.r[c] = v

    def op(self, en, fn, reads=(), writes=()):
        e = self.E[en]
        self._wait(e, self._deps(reads, writes))
        ins = fn(e.eng)
        e.count += 1
        ins.then_inc(e.sem, 1)
        self.nins += 1
        self._mark(e, e.count, reads, writes)
        return ins

    def pe(self, fn, r=(), w=()):
        return self.op("pe", fn, r, w)

    def dve(self, fn, r=(), w=()):
        return self.op("dve", fn, r, w)

    def act(self, fn, r=(), w=()):
        return self.op("act", fn, r, w)

    def pool(self, fn, r=(), w=()):
        return self.op("pool", fn, r, w)

    def dma(self, qn, out, in_, reads=(), writes=(), **kw):
        q = self.E[qn]
        pool_ = self.dma_pool[qn]
        d = pool_[self.dma_rr[qn]]
        self.dma_rr[qn] = (self.dma_rr[qn] + 1) % len(pool_)
        deps = self._deps(reads, writes)
        if d.count > 0 and deps.get(d, 0) < d.count:
            deps[d] = d.count
        self._wait(q, deps)
        ins = q.eng.dma_start(out=out, in_=in_, **kw)
        d.count += 16
        ins.then_inc(d.sem, 16)
        self.nins += 1
        self._mark(d, d.count, reads, writes)
        return ins

    def barrier(self, engines=None):
        clocks = list(self.E.values()) + [d for d in self.dma_sems if d.count > 0]
        targets = [(c, c.count) for c in clocks if c.count > 0]
        for e in self.E.values():
            for c, v in targets:
                if e.seen.get(c, 0) < v:
                    e.eng.wait_ge(c.sem, v)
                    e.seen[c] = v
                    self.nwait += 1

    def finish(self, bufs):
        q = self.E["sp"]
        deps = {}
        for b in bufs:
            if b.w is not None:
                c, v = b.w
                if deps.get(c, 0) < v:
                    deps[c] = v
        self._wait(q, deps)


def _pcol(v, nchunk):
    return np.ascontiguousarray(np.asarray(v).reshape(nchunk, 128).T)


def _consts():
    p = np.arange(128)[:, None]
    f = np.arange(128)[None, :]
    c = {}
    c["ident"] = (p == f).astype(np.float32)
    c["m_le"] = (p <= f).astype(np.float32)
    c["m_lt"] = (p < f).astype(np.float32)
    c["m_gt"] = (p > f).astype(np.float32)
    c["blk64"] = ((p // 64) == (f // 64)).astype(np.float32)
    c["hsel"] = ((p // 64) == np.arange(2)[None, :]).astype(np.float32)
    j = (np.arange(128) % 32) % 16
    c["invf"] = (1.0 / (10000.0 ** (2.0 * j / 32.0))).astype(np.float32)[:, None]
    return c


class Prog:
    pass


def build_program(taps=(), upto=None):
    nc = bass.Bass("TRN2", target_bir_lowering=False)
    P = Prog()
    P.nc = nc
    shapes = {
        "x": ([T, D], F32),
        "c_t": ([128, 8], F32),
        "pos": ([1, T], I32),
        "ada_w": ([DEPTH, D, 6 * D], F32),
        "ada_bt": ([DEPTH, 128, 48], F32),
        "ada_b": ([DEPTH, 6 * D], F32),
        "nmp_t": ([DEPTH, 128, 8], F32),
        "nfp_t": ([DEPTH, 128, 8], F32),
        "norm_mix_post": ([DEPTH, D], F32),
        "norm_ffn_post": ([DEPTH, D], F32),
        "w_in_a": ([DEPTH, D, N_IN_A], F32),
        "w_gate": ([DEPTH, 8, 128, 4, 8, 128], F32),
        "gla_gate_w2": ([DEPTH, 16, 128], F32),
        "gla_gate_b": ([DEPTH, 128], F32),
        "gla_norm": ([DEPTH, 64], F32),
        "diff_lambda": ([DEPTH, 128], F32),
        "diff_subln_t": ([DEPTH, 128, 1], F32),
        "conv_wt": ([DEPTH, 128, 2, 31], F32),
        "conv_bt": ([DEPTH, 128, 2], F32),
        "conv_lg_t": ([DEPTH, 128, 2], F32),
        "conv_lb_t": ([DEPTH, 128, 2], F32),
        "rwkv_mu_t": ([DEPTH, 128, 8], F32),
        "rwkv_w0": ([DEPTH, 256], F32),
        "rwkv_w2": ([DEPTH, 64, 256], F32),
        "rwkv_a0_t": ([DEPTH, 128, 2], F32),
        "rwkv_a2": ([DEPTH, 64, 256], F32),
        "rwkv_g2": ([DEPTH, 128, 256], F32),
        "rwkv_kk_t": ([DEPTH, 128, 2], F32),
        "rwkv_ka_t": ([DEPTH, 128, 2], F32),
        "rwkv_rk_t": ([DEPTH, 128, 2], F32),
        "rwkv_ln_g": ([DEPTH, 256], F32),
        "rwkv_ln_b": ([DEPTH, 256], F32),
        "w_branch": ([DEPTH, 4, 256, D], F32),
        "w_out": ([DEPTH, D, D], F32),
        "ffn_wg": ([1, 22, 128, 8, 128], F32),
        "ffn_wu": ([1, 22, 128, 8, 128], F32),
        "ffn_wd": ([1, D_FF, D], F32),
        "router_wt": ([1, 128, 8, 8], F32),
        "router_b": ([1, 8], F32),
        "moe_wg": ([1, N_EXP, 28, 128, 8, 128], F32),
        "moe_wu": ([1, N_EXP, 28, 128, 8, 128], F32),
        "moe_wd": ([1, N_EXP, D_EXP, D], F32),
    }
    for n_, a_ in _consts().items():
        shapes[n_] = (list(a_.shape), F32)

    class LazyDr(dict):
        def __missing__(self, name):
            shp, dt = shapes[name]
            ap = nc.dram_tensor(name, list(shp), dt, kind="ExternalInput").ap()
            self[name] = ap
            return ap

    dr = LazyDr()
    y_out = nc.dram_tensor("y", [T, D], F32, kind="ExternalOutput").ap()
    xs = [nc.dram_tensor("xs%d" % i, [T, D], F32, kind="Internal").ap() for i in range(2)]
    rope_dram = nc.dram_tensor("rope_tab", [2, 128, T], F32, kind="Internal").ap()
    ropeb = Buf("rope")
    tap_out = {}
    P.dr = dr

    with ExitStack() as es:
        k = K(nc, es)
        P.k = k

        sbn = [0]

        def sb(stack, name, shape, dt=F32):
            sbn[0] += 1
            return stack.enter_context(nc.sbuf_tensor("sb%d_%s" % (sbn[0], name), list(shape), dt))

        PS = [es.enter_context(nc.psum_tensor("ps%d" % i, [128, 512], F32)) for i in range(8)]
        PSb = [Buf("ps%d" % i, excl=True) for i in range(8)]

        def tap(name, ap, buf, shape):
            if name not in taps:
                return
            t = nc.dram_tensor("tap_" + name, list(shape), ap.dtype, kind="ExternalOutput").ap()
            tap_out[name] = t
            b = Buf("tap")
            k.dma("sp", t, ap, reads=[buf], writes=[b])
            P.tapbufs.append(b)

        P.tapbufs = []

        IDF = sb(es, "idf", [128, 128])
        MLE = sb(es, "mle", [128, 128])
        MLT = sb(es, "mlt", [128, 128])
        MGT = sb(es, "mgt", [128, 128])
        BLK = sb(es, "blk", [128, 128])
        HSEL = sb(es, "hsel", [128, 2])
        INVF = sb(es, "invf", [128, 1])
        ONES = sb(es, "ones", [128, 128])
        ONESB = sb(es, "onesb", [128, 128], BF16)
        MLEB = sb(es, "mleb", [128, 128], BF16)
        CT = sb(es, "ct", [128, 8])
        CA = sb(es, "ca", [128, 8], BF16)
        CAR = sb(es, "car", [128, 8, 128], BF16)
        HT = sb(es, "ht", [128, 8, T], BF16)
        cb = Buf("consts")
        HTb = Buf("ht")
        for nm, t_ in (("ident", IDF), ("m_le", MLE), ("m_lt", MLT), ("m_gt", MGT), ("blk64", BLK), ("hsel", HSEL), ("invf", INVF), ("c_t", CT)):
            k.dma("sp", t_[:], dr[nm][:], writes=[cb])
        k.pool(lambda e: e.memset(ONES[:], 1.0), w=[cb])
        k.pool(lambda e: e.memset(ONESB[:], 1.0), w=[cb])
        k.dve(lambda e: e.tensor_copy(out=MLEB[:], in_=MLE[:]), r=[cb], w=[cb])
        k.act(lambda e: e.activation(out=CA[:], in_=CT[:], func=AF.Silu), r=[cb], w=[cb])
        k.dve(lambda e: e.tensor_copy(out=CAR[:], in_=CA[:].unsqueeze(2).to_broadcast([128, 8, 128])), r=[cb], w=[cb])
        k.barrier()

        def win(l, c0, n):
            return dr["w_in_a"][l].rearrange("(kc p) n -> p kc n", p=128)[:, :, c0:c0 + n]

        def mm(ps_ap, lhsT, rhs, start, stop, r, w, tp=None, sw=False):
            if sw:
                pe_ = k.E["pe"]
                if pe_.count > 0 and pe_.seen.get(pe_, 0) < pe_.count:
                    pe_.eng.wait_ge(pe_.sem, pe_.count)
                    pe_.seen[pe_] = pe_.count
            if tp is None:
                return k.pe(lambda e: e.matmul(ps_ap, lhsT, rhs, start=start, stop=stop), r=r, w=w)
            return k.pe(lambda e: e.matmul(ps_ap, lhsT, rhs, start=start, stop=stop, tile_position=tp), r=r, w=w)

        def phase_mod(l, lay, MODT, GTM, GTF, modb):
            with ExitStack() as ph:
                WA = [sb(ph, "wa%d" % i, [128, 8, 512], BF16) for i in range(2)]
                WAb = [Buf(), Buf()]
                ABT = sb(ph, "abt", [128, 48])
                ABB = sb(ph, "abb", [128, 512])
                GPB = sb(ph, "gpb", [128, 512])
                tb_ = Buf()
                k.dma("sp", ABT[:], dr["ada_bt"][l], writes=[tb_])
                awv = dr["ada_w"][l].rearrange("(kc p) n -> p kc n", p=128)
                for grp in range(12):
                    w_ = WA[grp % 2]
                    wb_ = WAb[grp % 2]
                    k.dma("pool", w_[:], awv[:, :, grp * 512:(grp + 1) * 512], writes=[wb_])
                    if grp in (4, 5, 10, 11):
                        ps = PS[1 + grp % 2]
                        psb = PSb[1 + grp % 2]
                        for kc in range(8):
                            mm(ps[:], CAR[:, kc, :], w_[:, kc, :], kc == 0, kc == 7, [wb_, cb], [psb])
                        dst = GTM if grp < 6 else GTF
                        half = grp % 2
                        gsrc = dr["norm_mix_post"] if grp < 6 else dr["norm_ffn_post"]
                        k.dma("sp", ABB[:], dr["ada_b"][l:l + 1, grp * 512:(grp + 1) * 512].partition_broadcast(128), writes=[tb_])
                        k.dma("sp", GPB[:], gsrc[l:l + 1, half * 512:(half + 1) * 512].partition_broadcast(128), writes=[tb_])
                        k.dve(lambda e: e.tensor_tensor(out=ABB[:], in0=ps[:], in1=ABB[:], op=ALU.add), r=[psb, tb_], w=[tb_])
                        k.dve(lambda e: e.tensor_tensor(out=dst[:, half * 512:(half + 1) * 512], in0=ABB[:], in1=GPB[:], op=ALU.mult), r=[tb_], w=[modb])
                    else:
                        for jj in range(4):
                            col = grp * 4 + jj
                            for kc in range(8):
                                mm(PS[0][:, col:col + 1], w_[:, kc, jj * 128:(jj + 1) * 128], CA[:, kc:kc + 1], kc == 0, kc == 7, [wb_, cb], [PSb[0]])
                k.pool(lambda e: e.memset(MODT[:], 0.0), w=[modb])
                for c0 in (0, 24):
                    k.dve(lambda e: e.tensor_tensor(out=MODT[:, c0:c0 + 16], in0=PS[0][:, c0:c0 + 16], in1=ABT[:, c0:c0 + 16], op=ALU.add), r=[PSb[0], tb_], w=[modb])
                k.barrier()

        def phase_prenorm(xsrc, xsrcb, gT_ap, sc0, sh0, MODT, modb, router=None):
            with ExitStack() as ph:
                GT_ = sb(ph, "gT", [128, 8])
                A_ = sb(ph, "A_", [128, 8])
                XT = [sb(ph, "xt%d" % i, [128, D]) for i in range(2)]
                XTb = [Buf(), Buf()]
                XN = [sb(ph, "xn%d" % i, [128, D]) for i in range(2)]
                XNb = [Buf(), Buf()]
                JK = sb(ph, "jk", [128, D], BF16)
                SS = sb(ph, "ss", [128, NT])
                RS = sb(ph, "rs", [128, NT])
                H32 = [sb(ph, "h32%d" % i, [128, 8, 128]) for i in range(2)] if router is not None else None
                H32b = [Buf(), Buf()]
                pb = Buf()
                jb = Buf()
                ssm = Buf()
                k.pool(lambda e: e.memset(SS[:], 0.0), w=[ssm])
                k.dma("sp", GT_[:], gT_ap, writes=[pb])
                k.dve(lambda e: e.scalar_tensor_tensor(out=A_[:], in0=MODT[:, sc0:sc0 + 8], scalar=1.0, in1=GT_[:], op0=ALU.add, op1=ALU.mult), r=[modb, pb], w=[pb])
                stb = [Buf(), Buf()]
                htw = [Buf() for _ in range(8)]
                h32w = [[Buf() for _ in range(8)] for _ in range(2)]
                def stage_a(tt):
                        s = tt % 2
                        xt = XT[s]
                        k.dma("sp", xt[:], xsrc[tt * 128:(tt + 1) * 128, :], reads=[xsrcb[tt]], writes=[XTb[s]])
                        k.act(lambda e: e.activation(out=JK[:], in_=xt[:], func=AF.Square, accum_out=SS[:, tt:tt + 1]), r=[XTb[s], ssm], w=[jb, stb[s]])
                        k.act(lambda e: e.activation(out=RS[:, tt:tt + 1], in_=SS[:, tt:tt + 1], func=AF.Sqrt, scale=1.0 / D, bias=RMS_EPS), r=[stb[s]], w=[stb[s]])
                        k.dve(lambda e: e.reciprocal(out=RS[:, tt:tt + 1], in_=RS[:, tt:tt + 1]), r=[stb[s]], w=[stb[s]])
                        k.dve(lambda e: e.tensor_scalar(out=XN[s][:], in0=xt[:], scalar1=RS[:, tt:tt + 1], scalar2=None, op0=ALU.mult), r=[XTb[s], stb[s]], w=[XNb[s]])

                def stage_b(tt):
                        s = tt % 2
                        for half in range(2):
                            ps = PS[2 + half + 2 * s]
                            psb = PSb[2 + half + 2 * s]
                            for q in range(4):
                                kc = half * 4 + q
                                k.pe(lambda e: e.transpose(out=ps[:, q * 128:(q + 1) * 128], in_=XN[s][:, kc * 128:(kc + 1) * 128], identity=IDF[:]), r=[XNb[s], cb], w=[psb])
                            for q in range(4):
                                kc = half * 4 + q
                                if router is None:
                                    dst = HT[:, kc, tt * 128:(tt + 1) * 128]
                                    wr = [htw[kc]]
                                else:
                                    dst = H32[s][:, kc, :]
                                    wr = [h32w[s][kc]]
                                if q % 2 == 0:
                                    k.act(lambda e: e.activation(out=dst, in_=ps[:, q * 128:(q + 1) * 128], func=AF.Identity, scale=A_[:, kc:kc + 1], bias=MODT[:, sh0 + kc:sh0 + kc + 1]), r=[psb, pb, modb], w=wr)
                                else:
                                    k.dve(lambda e: e.tensor_scalar(out=dst, in0=ps[:, q * 128:(q + 1) * 128], scalar1=A_[:, kc:kc + 1], scalar2=MODT[:, sh0 + kc:sh0 + kc + 1], op0=ALU.mult, op1=ALU.add), r=[psb, pb, modb], w=wr)
                        if router is not None:
                            k.dve(lambda e: e.tensor_copy(out=HT[:, :, tt * 128:(tt + 1) * 128], in_=H32[s][:]), r=h32w[s], w=[htw[0], H32b[s]])
                            router(tt, H32[s], H32b[s])

                for step in range(NT + 1):
                    if step < NT:
                        stage_a(step)
                    if step >= 1:
                        stage_b(step - 1)
                k.barrier()

        def conv_setup(l, ph):
            WC = sb(ph, "wc", [128, 8, 512], BF16)
            U = sb(ph, "cu", [128, 2, T + 32])
            Y = sb(ph, "cy", [128, 2, T])
            CW = sb(ph, "cw", [128, 2, 31])
            CBt = sb(ph, "cbt", [128, 2])
            LG = sb(ph, "clg", [128, 2])
            LB = sb(ph, "clb", [128, 2])
            OND = sb(ph, "ond", [128, 128])
            SG = [sb(ph, "csg%d" % i, [128, 512]) for i in range(2)]
            SQ = sb(ph, "csq", [128, 2, 512])
            MEAN = sb(ph, "cmean", [128, 512])
            VAR = sb(ph, "cvar", [128, 512])
            TT_ = sb(ph, "ctt", [128, 512])
            wb_, ub, yb, pb, sgb, tb2 = Buf(), Buf(), Buf(), Buf(), [Buf(), Buf()], Buf()
            k.dma("pool", WC[:], win(l, 1552, 512), writes=[wb_])
            k.dma("sp", CW[:], dr["conv_wt"][l], writes=[pb])
            k.dma("sp", CBt[:], dr["conv_bt"][l], writes=[pb])
            k.dma("sp", LG[:], dr["conv_lg_t"][l], writes=[pb])
            k.dma("sp", LB[:], dr["conv_lb_t"][l], writes=[pb])
            k.pool(lambda e: e.memset(U[:, :, 0:32], 0.0), w=[ub])
            k.pool(lambda e: e.memset(OND[:], 1.0 / 256.0), w=[pb])
            pa, pab, pg, pgb = PS[6], PSb[6], PS[7], PSb[7]

            def gen(BR, BRb):
                i = 0
                for c in range(2):
                    for tb in range(NB):
                        s = i % 2
                        i += 1
                        for kc in range(8):
                            mm(pa[:], WC[:, kc, c * 128:(c + 1) * 128], HT[:, kc, tb * 512:(tb + 1) * 512], kc == 0, kc == 7, [wb_, HTb], [pab])
                        for kc in range(8):
                            mm(pg[:], WC[:, kc, 256 + c * 128:256 + (c + 1) * 128], HT[:, kc, tb * 512:(tb + 1) * 512], kc == 0, kc == 7, [wb_, HTb], [pgb])
                        k.act(lambda e: e.activation(out=SG[s][:], in_=pg[:], func=AF.Sigmoid), r=[pgb], w=[sgb[s]])
                        k.dve(lambda e: e.tensor_tensor(out=U[:, c, 32 + tb * 512:32 + (tb + 1) * 512], in0=pa[:], in1=SG[s][:], op=ALU.mult), r=[pab, sgb[s]], w=[ub])
                        yield
                for c in range(2):
                    k.dve(lambda e: e.tensor_scalar(out=Y[:, c, :], in0=U[:, c, 2:2 + T], scalar1=CW[:, c, 0:1], scalar2=CBt[:, c:c + 1], op0=ALU.mult, op1=ALU.add), r=[ub, pb], w=[yb])
                    yield
                    for j in range(1, 31):
                        k.dve(lambda e: e.scalar_tensor_tensor(out=Y[:, c, :], in0=U[:, c, 2 + j:2 + j + T], scalar=CW[:, c, j:j + 1], in1=Y[:, c, :], op0=ALU.mult, op1=ALU.add), r=[ub, pb, yb], w=[yb])
                        yield
                for tb in range(NB):
                    sl = slice(tb * 512, (tb + 1) * 512)
                    k.act(lambda e: e.activation(out=SQ[:], in_=Y[:, :, sl], func=AF.Square), r=[yb], w=[tb2])
                    for c in range(2):
                        mm(pa[:], OND[:], Y[:, c, sl], c == 0, c == 1, [pb, yb], [pab])
                    for c in range(2):
                        mm(pg[:], OND[:], SQ[:, c, :], c == 0, c == 1, [pb, tb2], [pgb])
                    k.act(lambda e: e.activation(out=MEAN[:], in_=pa[:], func=AF.Copy), r=[pab], w=[tb2])
                    yield
                    k.dve(lambda e: e.tensor_tensor(out=VAR[:], in0=MEAN[:], in1=MEAN[:], op=ALU.mult), r=[tb2], w=[tb2])
                    k.dve(lambda e: e.tensor_tensor(out=VAR[:], in0=pg[:], in1=VAR[:], op=ALU.subtract), r=[pgb, tb2], w=[tb2])
                    k.act(lambda e: e.activation(out=VAR[:], in_=VAR[:], func=AF.Sqrt, bias=LN_EPS), r=[tb2], w=[tb2])
                    k.dve(lambda e: e.reciprocal(out=VAR[:], in_=VAR[:]), r=[tb2], w=[tb2])
                    yield
                    for c in range(2):
                        k.dve(lambda e: e.tensor_tensor(out=TT_[:], in0=Y[:, c, sl], in1=MEAN[:], op=ALU.subtract), r=[yb, tb2], w=[tb2])
                        k.dve(lambda e: e.tensor_tensor(out=TT_[:], in0=TT_[:], in1=VAR[:], op=ALU.mult), r=[tb2], w=[tb2])
                        k.act(lambda e: e.activation(out=BR[:, 4 + c, sl], in_=TT_[:], func=AF.Silu, scale=LG[:, c:c + 1], bias=LB[:, c:c + 1]), r=[tb2, pb], w=[BRb])
                        yield

            return gen

        def phase_conv(l, BR, BRb):
            with ExitStack() as ph:
                g_ = conv_setup(l, ph)(BR, BRb)
                for _ in g_:
                    pass
                k.barrier()

        def phase_diff(l, BR, BRb, pump=lambda n: None):
            lam_init = 0.8 - 0.6 * math.exp(-0.3 * l)
            PI_S = 3.1415925
            with ExitStack() as ph:
                QT = sb(ph, "dqt", [128, 2, T], BF16)
                KT = sb(ph, "dkt", [128, 2, T], BF16)
                VA = sb(ph, "dva", [128, NT, 4, 65], BF16)
                LP = sb(ph, "dlp", [128, 128])
                PR = sb(ph, "dpr", [128, 2, 32])
                SR = sb(ph, "dsr", [128, 2])
                COEF = sb(ph, "dcoef", [128, 8])
                SUB = sb(ph, "dsub", [128, 1])
                qtb, ktb, vab, pb = Buf(), Buf(), Buf(), Buf()
                k.dma("sp", LP[:], dr["diff_lambda"][l:l + 1, :].partition_broadcast(128), writes=[pb])
                k.dma("sp", SUB[:], dr["diff_subln_t"][l], writes=[pb])
                LPv = LP[:].rearrange("p (a b d) -> p a b d", a=2, b=2)
                k.dve(lambda e: e.tensor_tensor(out=PR[:], in0=LPv[:, :, 0, :], in1=LPv[:, :, 1, :], op=ALU.mult), r=[pb], w=[pb])
                k.dve(lambda e: e.tensor_reduce(out=SR[:], in_=PR[:], axis=AX.X, op=ALU.add), r=[pb], w=[pb])
                k.act(lambda e: e.activation(out=SR[:], in_=SR[:], func=AF.Exp), r=[pb], w=[pb])
                k.pool(lambda e: e.memset(COEF[:], 1.0), w=[pb])
                k.dve(lambda e: e.tensor_tensor(out=SR[:, 0:1], in0=SR[:, 1:2], in1=SR[:, 0:1], op=ALU.subtract), r=[pb], w=[pb])
                COEFv = COEF[:].rearrange("p (h c) -> p h c", c=2)
                k.dve(lambda e: e.tensor_scalar(out=COEFv[:, :, 1], in0=SR[:, 0:1].to_broadcast([128, 4]), scalar1=-lam_init, scalar2=None, op0=ALU.add), r=[pb], w=[pb])
                with ExitStack() as p1:
                    COS = sb(p1, "dcos", [128, T])
                    SIN = sb(p1, "dsin", [128, T])
                    W2 = sb(p1, "dw2", [128, 8, 512], BF16)
                    T1 = [sb(p1, "dt1%d" % i_, [128, 512]) for i_ in range(2)]
                    T2 = [sb(p1, "dt2%d" % i_, [128, 512]) for i_ in range(2)]
                    tabb, w2b, t1b, t2b = Buf(), Buf(), [Buf(), Buf()], [Buf(), Buf()]
                    if l == 0:
                        with ExitStack() as p0:
                            POSI = sb(p0, "dposi", [128, 1024], I32)
                            ANG = sb(p0, "dang", [128, 1024])
                            TQ = sb(p0, "dtq", [128, 1024])
                            TI = sb(p0, "dti", [128, 1024], I32)
                            ab = Buf()
                            for tb in range(2):
                                sl = slice(tb * 1024, (tb + 1) * 1024)
                                k.dma("sp", POSI[:], dr["pos"][0:1, sl].partition_broadcast(128), writes=[ab])
                                k.dve(lambda e: e.tensor_copy(out=ANG[:], in_=POSI[:]), r=[ab], w=[ab])
                                k.dve(lambda e: e.tensor_scalar(out=ANG[:], in0=ANG[:], scalar1=INVF[:, 0:1], scalar2=None, op0=ALU.mult), r=[ab, cb], w=[ab])
                                for dst, shift in ((SIN, 0.0), (COS, PI / 2)):
                                    k.dve(lambda e: e.tensor_scalar(out=TQ[:], in0=ANG[:], scalar1=1.0 / (2 * PI), scalar2=shift / (2 * PI), op0=ALU.mult, op1=ALU.add), r=[ab], w=[ab])
                                    k.dve(lambda e: e.tensor_copy(out=TI[:], in_=TQ[:]), r=[ab], w=[ab])
                                    k.dve(lambda e: e.tensor_copy(out=TQ[:], in_=TI[:]), r=[ab], w=[ab])
                                    k.dve(lambda e: e.scalar_tensor_tensor(out=TQ[:], in0=TQ[:], scalar=-2 * PI, in1=ANG[:], op0=ALU.mult, op1=ALU.add), r=[ab], w=[ab])
                                    k.dve(lambda e: e.tensor_scalar(out=TQ[:], in0=TQ[:], scalar1=shift, scalar2=-PI_S, op0=ALU.add, op1=ALU.max), r=[ab], w=[ab])
                                    k.dve(lambda e: e.tensor_scalar(out=TQ[:], in0=TQ[:], scalar1=PI_S, scalar2=None, op0=ALU.min), r=[ab], w=[ab])
                                    k.act(lambda e: e.activation(out=dst[:, sl], in_=TQ[:], func=AF.Sin), r=[ab], w=[tabb])
                                pump(4)
                            k.dma("sp", rope_dram[0], COS[:], reads=[tabb], writes=[ropeb])
                            k.dma("sp", rope_dram[1], SIN[:], reads=[tabb], writes=[ropeb])
                            k.barrier()
                    else:
                        k.dma("sp", COS[:], rope_dram[0], reads=[ropeb], writes=[tabb])
                        k.dma("sp", SIN[:], rope_dram[1], reads=[ropeb], writes=[tabb])
                        pump(8)
                        k.barrier()
                    i_ = 0
                    for off, dst, dstb in ((784, QT, qtb), (1040, KT, ktb)):
                        k.dma("pool", W2[:, :, 0:256], win(l, off, 256), writes=[w2b])
                        Wv = W2[:, :, 0:256].rearrange("p k (g t j) -> p k g t j", g=8, t=2)
                        Rv = W2[:, :, 256:512].rearrange("p k (g t j) -> p k g t j", g=8, t=2)
                        k.dve(lambda e: e.tensor_scalar(out=Rv[:, :, :, 0, :], in0=Wv[:, :, :, 1, :], scalar1=-1.0, scalar2=None, op0=ALU.mult), r=[w2b], w=[w2b])
                        k.dve(lambda e: e.tensor_copy(out=Rv[:, :, :, 1, :], in_=Wv[:, :, :, 0, :]), r=[w2b], w=[w2b])
                        for c in range(2):
                            for tb in range(NB):
                                s = i_ % 2
                                i_ += 1
                                sl = slice(tb * 512, (tb + 1) * 512)
                                pa, pab = PS[2 * s], PSb[2 * s]
                                pr_, prb = PS[2 * s + 1], PSb[2 * s + 1]
                                for kc in range(8):
                                    mm(pa[:], W2[:, kc, c * 128:(c + 1) * 128], HT[:, kc, sl], kc == 0, kc == 7, [w2b, HTb], [pab])
                                for kc in range(8):
                                    mm(pr_[:], W2[:, kc, 256 + c * 128:256 + (c + 1) * 128], HT[:, kc, sl], kc == 0, kc == 7, [w2b, HTb], [prb])
                                k.dve(lambda e: e.tensor_tensor(out=T1[s][:], in0=pa[:], in1=COS[:, sl], op=ALU.mult), r=[pab, tabb], w=[t1b[s]])
                                k.dve(lambda e: e.tensor_tensor(out=T2[s][:], in0=pr_[:], in1=SIN[:, sl], op=ALU.mult), r=[prb, tabb], w=[t2b[s]])
                                k.pool(lambda e: e.tensor_tensor(out=dst[:, c, sl], in0=T1[s][:], in1=T2[s][:], op=ALU.add), r=[t1b[s], t2b[s]], w=[dstb])
                    k.dma("pool", W2[:, :, 0:256], win(l, 1296, 256), writes=[w2b])
                    k.pool(lambda e: e.memset(VA[:, :, :, 64:65], 1.0), w=[vab])
                    for tt in range(NT):
                        s = tt % 2
                        ps, psb = PS[4 + s], PSb[4 + s]
                        for kc in range(8):
                            mm(ps[:, 0:256], HT[:, kc, tt * 128:(tt + 1) * 128], W2[:, kc, 0:256], kc == 0, kc == 7, [w2b, HTb], [psb])
                        k.act(lambda e: e.activation(out=VA[:, tt, :, 0:64], in_=ps[:, 0:256].rearrange("p (h d) -> p h d", h=4), func=AF.Copy), r=[psb], w=[vab])
                    k.barrier()
                tap("dqt%d" % l, QT[:], qtb, [128, 2, T])
                tap("dkt%d" % l, KT[:], ktb, [128, 2, T])
                with ExitStack() as p2:
                    PT = sb(p2, "dpt", [128, 16, 512], BF16)
                    ptb = [Buf() for _ in range(16)]
                    OACC2 = [sb(p2, "doacc%d" % i_, [128, 4, 8, 65]) for i_ in range(2)]
                    ob2 = [Buf(), Buf()]
                    RSs = sb(p2, "drs", [128, 4, 8])
                    ON = sb(p2, "don", [128, 4, 8, 64])
                    OD = sb(p2, "dod", [128, 4, 4, 64])
                    SQd = sb(p2, "dsq", [128, 4, 4, 64])
                    SSd = sb(p2, "dss", [128, 4, 4])
                    fb = Buf()
                    sc = 32.0 ** -0.5
                    rot = 0
                    for qb in range(NB):
                        OACC, ob = OACC2[qb % 2], ob2[qb % 2]
                        for g in range(8):
                            c, gl, h = g // 4, g % 4, g // 2
                            nk = 4 * qb + 4
                            for kt in range(nk):
                                r_ = kt - 4 * qb
                                n0 = max(0, r_) * 128
                                ps, psb = PS[rot % 3], PSb[rot % 3]
                                rot += 1
                                mm(ps[:, n0:512], KT[32 * gl:32 * gl + 32, c, kt * 128:(kt + 1) * 128], QT[32 * gl:32 * gl + 32, c, qb * 512 + n0:(qb + 1) * 512], True, True, [ktb, qtb], [psb], tp=(32 * gl, 0))
                                k.act(lambda e: e.activation(out=PT[:, kt, n0:512], in_=ps[:, n0:512], func=AF.Exp, scale=sc), r=[psb], w=[ptb[kt]])
                                if r_ >= 0:
                                    k.pool(lambda e: e.tensor_tensor(out=PT[:, kt, n0:n0 + 128], in0=PT[:, kt, n0:n0 + 128], in1=MLEB[:], op=ALU.mult), r=[ptb[kt], cb], w=[ptb[kt]])
                            po, pob = PS[3 + (g % 2)], PSb[3 + (g % 2)]
                            for qi in range(4):
                                last = 4 * qb + qi
                                for kt in range(last + 1):
                                    mm(po[:, qi * 65:(qi + 1) * 65], PT[:, kt, qi * 128:(qi + 1) * 128], VA[:, kt, h, :], kt == 0, kt == last, [ptb[kt], vab], [pob])
                            k.act(lambda e: e.activation(out=OACC[:, :, g, :], in_=po[:, 0:260].rearrange("p (q e) -> p q e", q=4), func=AF.Copy), r=[pob], w=[ob])
                            pump(3)
                        k.dve(lambda e: e.reciprocal(out=RSs[:], in_=OACC[:, :, :, 64]), r=[ob], w=[fb])
                        k.dve(lambda e: e.tensor_tensor(out=RSs[:], in0=RSs[:], in1=COEF[:].unsqueeze(1).to_broadcast([128, 4, 8]), op=ALU.mult), r=[fb, pb], w=[fb])
                        k.dve(lambda e: e.tensor_tensor(out=ON[:], in0=OACC[:, :, :, 0:64], in1=RSs[:].unsqueeze(3).to_broadcast([128, 4, 8, 64]), op=ALU.mult), r=[ob, fb], w=[fb])
                        ONv = ON[:].rearrange("p q (h c) d -> p q h c d", c=2)
                        k.dve(lambda e: e.tensor_tensor(out=OD[:], in0=ONv[:, :, :, 0, :], in1=ONv[:, :, :, 1, :], op=ALU.add), r=[fb], w=[fb])
                        k.dve(lambda e: e.tensor_tensor(out=SQd[:], in0=OD[:], in1=OD[:], op=ALU.mult), r=[fb], w=[fb])
                        k.dve(lambda e: e.tensor_reduce(out=SSd[:], in_=SQd[:], axis=AX.X, op=ALU.add), r=[fb], w=[fb])
                        k.act(lambda e: e.activation(out=SSd[:], in_=SSd[:], func=AF.Sqrt, scale=1.0 / 64, bias=RMS_EPS), r=[fb], w=[fb])
                        k.dve(lambda e: e.reciprocal(out=SSd[:], in_=SSd[:]), r=[fb], w=[fb])
                        k.dve(lambda e: e.tensor_tensor(out=OD[:], in0=OD[:], in1=SSd[:].unsqueeze(3).to_broadcast([128, 4, 4, 64]), op=ALU.mult), r=[fb], w=[fb])
                        for qi in range(4):
                            tt = 4 * qb + qi
                            ps, psb = PS[5], PSb[5]
                            ODf = OD[:, qi].rearrange("p h d -> p (h d)")
                            for cc in range(2):
                                k.pe(lambda e: e.transpose(out=ps[:, cc * 128:(cc + 1) * 128], in_=ODf[:, cc * 128:(cc + 1) * 128], identity=IDF[:]), r=[fb, cb], w=[psb])
                            k.dve(lambda e: e.tensor_scalar(out=BR[:, 2:4, tt * 128:(tt + 1) * 128], in0=ps[:, 0:256].rearrange("p (c n) -> p c n", c=2), scalar1=SUB[:, 0:1], scalar2=(1.0 - lam_init), op0=ALU.mult, op1=ALU.mult), r=[psb, pb], w=[BRb])
                    pump(100000)
                    k.barrier()

        def phase_gla(l, BR, BRb):
            with ExitStack() as ph:
                WG = sb(ph, "gw", [128, 8, 784], BF16)
                W2F = sb(ph, "gw2f", [17, 128])
                GN = sb(ph, "ggn", [128, 64])
                S32 = sb(ph, "gs32", [128, 64])
                SBF = sb(ph, "gsbf", [128, 64], BF16)
                GZT = sb(ph, "ggzt", [17, 128])
                E1 = sb(ph, "ge1", [128, 128])
                LL = sb(ph, "gll", [128, 128])
                EP = sb(ph, "gep", [128, 128])
                EM = sb(ph, "gem", [128, 128])
                E3 = sb(ph, "ge3", [128, 128])
                QS = sb(ph, "gqs", [128, 128], BF16)
                KS = sb(ph, "gks", [128, 128], BF16)
                KH = sb(ph, "gkh", [128, 128], BF16)
                V = sb(ph, "gv", [128, 256], BF16)
                SOG = sb(ph, "gsog", [128, 256])
                AM = sb(ph, "gam", [128, 4, 128], BF16)
                O = sb(ph, "go", [128, 4, 64])
                SQ = sb(ph, "gsq", [128, 4, 64])
                SS = sb(ph, "gss", [128, 4])
                wb_, pb, sb_, zb, eb, qb_, vb_, ob = Buf(), Buf(), Buf(), Buf(), Buf(), Buf(), Buf(), Buf()
                k.dma("pool", WG[:], win(l, 0, 784), writes=[wb_])
                k.dma("sp", W2F[0:16, :], dr["gla_gate_w2"][l], writes=[pb])
                k.dma("sp", W2F[16:17, :], dr["gla_gate_b"][l:l + 1, :], writes=[pb])
                k.dma("sp", GN[:], dr["gla_norm"][l:l + 1, :].partition_broadcast(128), writes=[pb])
                k.pool(lambda e: e.memset(S32[:], 0.0), w=[sb_])
                k.pool(lambda e: e.memset(SBF[:], 0.0), w=[sb_])
                k.pool(lambda e: e.memset(GZT[:], 1.0), w=[zb])
                import os
                gstage = float(os.environ.get("GLA_STAGE", "99"))
                for tt in range(NT if gstage >= 99 else 1):
                    tsl = slice(tt * 128, (tt + 1) * 128)
                    for kc in range(8):
                        mm(PS[0][:, 0:384], HT[:, kc, tsl], WG[:, kc, 128:512], kc == 0, kc == 7, [wb_, HTb], [PSb[0]])
                    for kc in range(8):
                        mm(PS[1][:, 0:256], HT[:, kc, tsl], WG[:, kc, 512:768], kc == 0, kc == 7, [wb_, HTb], [PSb[1]])
                    for kc in range(8):
                        mm(PS[2][:, 0:128], WG[:, kc, 0:128], HT[:, kc, tsl], kc == 0, kc == 7, [wb_, HTb], [PSb[2]])
                    for kc in range(8):
                        mm(PS[2][:, 128:256], WG[:, kc, 128:256], HT[:, kc, tsl], kc == 0, kc == 7, [wb_, HTb], [PSb[2]])
                    for kc in range(8):
                        mm(PS[3][0:16, 0:128], WG[:, kc, 768:784], HT[:, kc, tsl], kc == 0, kc == 7, [wb_, HTb], [PSb[3]])
                    if gstage < 1:
                        break
                    k.act(lambda e: e.activation(out=GZT[0:16, :], in_=PS[3][0:16, 0:128], func=AF.Copy), r=[PSb[3]], w=[zb])
                    mm(PS[3][:, 128:256], GZT[0:17, :], W2F[0:17, :], True, True, [zb, pb], [PSb[3]])
                    k.act(lambda e: e.activation(out=E1[:], in_=PS[3][:, 128:256], func=AF.Exp, scale=-1.0), r=[PSb[3]], w=[eb])
                    k.act(lambda e: e.activation(out=LL[:], in_=E1[:], func=AF.Ln, bias=1.0), r=[eb], w=[eb])
                    if gstage < 2:
                        break
                    mm(PS[4][:, 0:128], LL[:], MLE[:], True, True, [eb, cb], [PSb[4]])
                    mm(PS[4][:, 128:256], MGT[:], LL[:], True, True, [eb, cb], [PSb[4]])
                    if gstage < 2.1:
                        break
                    k.act(lambda e: e.activation(out=EP[:], in_=PS[4][:, 0:128], func=AF.Exp, scale=-1.0 / 16), r=[PSb[4]], w=[eb])
                    if gstage < 2.2:
                        break
                    k.act(lambda e: e.activation(out=EM[:], in_=PS[4][:, 0:128], func=AF.Exp, scale=1.0 / 16), r=[PSb[4]], w=[eb])
                    if gstage < 2.3:
                        break
                    k.act(lambda e: e.activation(out=E3[:], in_=PS[4][:, 128:256], func=AF.Exp, scale=-1.0 / 16), r=[PSb[4]], w=[eb])
                    if gstage < 2.4:
                        break
                    k.dve(lambda e: e.scalar_tensor_tensor(out=QS[:], in0=PS[2][:, 0:128], scalar=32.0 ** -0.5, in1=EP[:], op0=ALU.mult, op1=ALU.mult), r=[PSb[2], eb], w=[qb_])
                    if gstage < 2.5:
                        break
                    k.dve(lambda e: e.tensor_tensor(out=KS[:], in0=PS[2][:, 128:256], in1=EM[:], op=ALU.mult), r=[PSb[2], eb], w=[qb_])
                    if gstage < 2.6:
                        break
                    k.dve(lambda e: e.tensor_tensor(out=KH[:], in0=PS[0][:, 0:128], in1=E3[:], op=ALU.mult), r=[PSb[0], eb], w=[qb_])
                    if gstage < 2.7:
                        break
                    k.act(lambda e: e.activation(out=V[:], in_=PS[0][:, 128:384], func=AF.Copy), r=[PSb[0]], w=[vb_])
                    if gstage < 2.8:
                        break
                    k.act(lambda e: e.activation(out=SOG[:], in_=PS[1][:, 0:256], func=AF.Silu), r=[PSb[1]], w=[vb_])
                    if gstage < 3:
                        break
                    for h in range(4):
                        mm(PS[5][:, h * 128:(h + 1) * 128], KS[32 * h:32 * h + 32, :], QS[32 * h:32 * h + 32, :], True, True, [qb_], [PSb[5]], tp=(32 * h, 0), sw=True)
                    k.dve(lambda e: e.tensor_tensor(out=AM[:], in0=PS[5][:].rearrange("p (h n) -> p h n", h=4), in1=MLE[:].unsqueeze(1).to_broadcast([128, 4, 128]), op=ALU.mult), r=[PSb[5], cb], w=[qb_])
                    if gstage < 4:
                        break
                    for h in range(4):
                        mm(PS[6][:, h * 64:(h + 1) * 64], AM[:, h, :], V[:, h * 64:(h + 1) * 64], True, False, [qb_, vb_], [PSb[6]])
                        mm(PS[6][:, h * 64:(h + 1) * 64], QS[32 * h:32 * h + 32, :], SBF[32 * h:32 * h + 32, :], False, True, [qb_, sb_], [PSb[6]], tp=(32 * h, 0))
                    if gstage < 5:
                        break
                    mm(PS[7][:, 0:256], KH[:], V[:], True, True, [qb_, vb_], [PSb[7]])
                    for h in range(4):
                        hs = slice(32 * h, 32 * h + 32)
                        k.dve(lambda e: e.scalar_tensor_tensor(out=S32[hs, :], in0=S32[hs, :], scalar=EP[hs, 127:128], in1=PS[7][hs, h * 64:(h + 1) * 64], op0=ALU.mult, op1=ALU.add), r=[sb_, eb, PSb[7]], w=[sb_])
                    k.dve(lambda e: e.tensor_copy(out=SBF[:], in_=S32[:]), r=[sb_], w=[sb_])
                    if gstage < 6:
                        break
                    k.act(lambda e: e.activation(out=O[:], in_=PS[6][:, 0:256].rearrange("p (h d) -> p h d", h=4), func=AF.Copy), r=[PSb[6]], w=[ob])
                    k.dve(lambda e: e.tensor_tensor(out=SQ[:], in0=O[:], in1=O[:], op=ALU.mult), r=[ob], w=[ob])
                    k.dve(lambda e: e.tensor_reduce(out=SS[:], in_=SQ[:], axis=AX.X, op=ALU.add), r=[ob], w=[ob])
                    k.act(lambda e: e.activation(out=SS[:], in_=SS[:], func=AF.Sqrt, scale=1.0 / 64, bias=RMS_EPS), r=[ob], w=[ob])
                    k.dve(lambda e: e.reciprocal(out=SS[:], in_=SS[:]), r=[ob], w=[ob])
                    k.dve(lambda e: e.tensor_tensor(out=O[:], in0=O[:], in1=SS[:].unsqueeze(2).to_broadcast([128, 4, 64]), op=ALU.mult), r=[ob], w=[ob])
                    k.dve(lambda e: e.tensor_tensor(out=O[:], in0=O[:], in1=GN[:].unsqueeze(1).to_broadcast([128, 4, 64]), op=ALU.mult), r=[ob, pb], w=[ob])
                    k.dve(lambda e: e.tensor_tensor(out=O[:], in0=O[:], in1=SOG[:].rearrange("p (h d) -> p h d", h=4), op=ALU.mult), r=[ob, vb_], w=[ob])
                    Of = O[:].rearrange("p h d -> p (h d)")
                    for cc in range(2):
                        k.pe(lambda e: e.transpose(out=PS[1][:, cc * 128:(cc + 1) * 128], in_=Of[:, cc * 128:(cc + 1) * 128], identity=IDF[:]), r=[ob, cb], w=[PSb[1]])
                    k.act(lambda e: e.activation(out=BR[:, 0:2, tsl], in_=PS[1][:, 0:256].rearrange("p (c n) -> p c n", c=2), func=AF.Copy), r=[PSb[1]], w=[BRb])
                k.barrier()


        def phase_rwkv(l, BR, BRb):
            SDEC = -math.exp(-0.5)
            with ExitStack() as ph:
                f32 = lambda n, shp: sb(ph, n, shp)
                b16 = lambda n, shp: sb(ph, n, shp, BF16)
                WR = b16("rw", [128, 8, 1024])
                MU = f32("rmu", [128, 8])
                W2E = f32("rw2e", [65, 256])
                A2 = f32("ra2", [128, 256])
                G2 = b16("rg2", [128, 256])
                A0 = f32("ra0", [128, 2])
                KKp = f32("rkk", [128, 2])
                KA = f32("rka", [128, 2])
                OMK = f32("romk", [128, 2])
                RKp = f32("rrk", [128, 2])
                LNG = f32("rlng", [128, 256])
                LNB = f32("rlnb", [128, 256])
                MLELT = f32("rmlelt", [128, 256])
                MLTLE = f32("rmltle", [128, 256])
                RAW = f32("rraw", [128, 8, 129])
                XM = f32("rxm", [128, 8, 128])
                DX = f32("rdx", [128, 8, 128])
                M32 = f32("rm32", [128, 2, 64])
                MBF = b16("rmbf", [128, 2, 64])
                TZW = f32("rtzw", [65, 128])
                SG = f32("rsg", [128, 256])
                EP = f32("rep", [128, 2, 128])
                EM = f32("rem", [128, 2, 128])
                EX = f32("rex", [128, 2, 128])
                AT = f32("rat", [128, 2, 128])
                KK0 = f32("rkk0", [128, 2, 128])
                SQ = f32("rsq", [128, 2, 128])
                RN = f32("rrn", [128, 2, 128])
                T1 = f32("rt1", [128, 2, 128])
                K2 = f32("rk2", [128, 2, 128])
                NB_ = f32("rnb", [128, 2, 128])
                BRt = b16("rbrt", [128, 2, 2, 128])
                KTt = b16("rktt", [128, 2, 128])
                ALt = b16("ralt", [128, 2, 128])
                KT32 = f32("rkt32", [128, 2, 128])
                AL32 = f32("ral32", [128, 2, 128])
                KP32 = f32("rkp32", [128, 2, 128])
                AP32 = f32("rap32", [128, 2, 128])
                KPT = b16("rkpt", [128, 256])
                APT = b16("rapt", [128, 256])
                VT = b16("rvt", [128, 256])
                V32 = f32("rv32", [128, 256])
                T2 = f32("rt2", [128, 2, 128])
                BON = f32("rbon", [128, 4])
                SZ = b16("rsz", [128, 128])
                GTM_ = f32("rgtm", [128, 256])
                A1m = b16("ra1m", [128, 4, 256])
                A2m = b16("ra2m", [128, 4, 256])
                Z = [b16("rz%d" % i_, [128, 4, 128]) for i_ in range(2)]
                ZT = [b16("rzt%d" % i_, [128, 4, 128]) for i_ in range(2)]
                TT = [b16("rtt%d" % i_, [128, 4, 128]) for i_ in range(2)]
                X0 = b16("rx0", [128, 4, 64])
                UH = b16("ruh", [128, 4, 64])
                Y = f32("ry", [128, 4, 64])
                SQY = f32("rsqy", [128, 4, 64])
                S1 = f32("rs1", [128, 4])
                S2 = f32("rs2", [128, 4])
                MS = f32("rms", [128, 4])
                wb_, pb, rawb, xb, xmb, tzb, sgb, eb, ab, kb, opb, tmb = [Buf() for _ in range(12)]
                a1b, a2b, x0b, uhb, mb, mbfb, yb = [Buf() for _ in range(7)]
                zb, ztb, ttb = [Buf(), Buf()], [Buf(), Buf()], [Buf(), Buf()]
                k.dma("pool", WR[:], win(l, 2064, 1024), writes=[wb_])
                k.dma("sp", MU[:], dr["rwkv_mu_t"][l], writes=[pb])
                k.dma("sp", W2E[0:64, :], dr["rwkv_w2"][l], writes=[pb])
                k.dma("sp", W2E[64:65, :], dr["rwkv_w0"][l:l + 1, :], writes=[pb])
                k.dma("sp", A2[64:128, :], dr["rwkv_a2"][l], writes=[pb])
                k.dma("pool", G2[:], dr["rwkv_g2"][l], writes=[pb])
                k.dma("sp", A0[:], dr["rwkv_a0_t"][l], writes=[pb])
                k.dma("sp", KKp[:], dr["rwkv_kk_t"][l], writes=[pb])
                k.dma("sp", KA[:], dr["rwkv_ka_t"][l], writes=[pb])
                k.dma("sp", RKp[:], dr["rwkv_rk_t"][l], writes=[pb])
                k.dma("sp", LNG[:], dr["rwkv_ln_g"][l:l + 1, :].partition_broadcast(128), writes=[pb])
                k.dma("sp", LNB[:], dr["rwkv_ln_b"][l:l + 1, :].partition_broadcast(128), writes=[pb])
                k.dve(lambda e: e.tensor_scalar(out=OMK[:], in0=KA[:], scalar1=-1.0, scalar2=1.0, op0=ALU.mult, op1=ALU.add), r=[pb], w=[pb])
                k.dve(lambda e: e.tensor_copy(out=MLELT[:, 0:128], in_=MLE[:]), r=[cb], w=[pb])
                k.dve(lambda e: e.tensor_copy(out=MLELT[:, 128:256], in_=MLT[:]), r=[cb], w=[pb])
                k.dve(lambda e: e.tensor_copy(out=MLTLE[:, 0:128], in_=MLT[:]), r=[cb], w=[pb])
                k.dve(lambda e: e.tensor_copy(out=MLTLE[:, 128:256], in_=MLE[:]), r=[cb], w=[pb])
                k.pool(lambda e: e.memset(RAW[:, :, 0:1], 0.0), w=[rawb])
                k.pool(lambda e: e.memset(M32[:], 0.0), w=[mb])
                k.pool(lambda e: e.memset(MBF[:], 0.0), w=[mbfb])
                k.pool(lambda e: e.memset(TZW[:], 1.0), w=[tzb])
                bc2 = lambda t_, n=128: t_[:].unsqueeze(2).to_broadcast([128, 2, n])
                v4 = lambda ap, h: ap.rearrange("p (h n) -> p h n", h=h)
                import os
                rstage = float(os.environ.get("RWKV_STAGE", "99"))
                for tt in range(NT if rstage >= 99 else 1):
                    tsl = slice(tt * 128, (tt + 1) * 128)
                    for j in range(8):
                        ps, psb = (PS[0], PSb[0]) if j < 4 else (PS[1], PSb[1])
                        for kc in range(8):
                            mm(ps[:, (j % 4) * 128:(j % 4 + 1) * 128], WR[:, kc, j * 128:(j + 1) * 128], HT[:, kc, tsl], kc == 0, kc == 7, [wb_, HTb], [psb])
                    k.act(lambda e: e.activation(out=RAW[:, 0:4, 1:129], in_=v4(PS[0][:], 4), func=AF.Copy), r=[PSb[0]], w=[rawb])
                    k.act(lambda e: e.activation(out=RAW[:, 4:8, 1:129], in_=v4(PS[1][:], 4), func=AF.Copy), r=[PSb[1]], w=[rawb])
                    k.dve(lambda e: e.tensor_tensor(out=DX[:], in0=RAW[:, :, 0:128], in1=RAW[:, :, 1:129], op=ALU.subtract), r=[rawb], w=[xb])
                    k.dve(lambda e: e.tensor_tensor(out=DX[:], in0=DX[:], in1=MU[:].unsqueeze(2).to_broadcast([128, 8, 128]), op=ALU.mult), r=[xb, pb], w=[xb])
                    k.dve(lambda e: e.tensor_tensor(out=XM[:], in0=DX[:], in1=RAW[:, :, 1:129], op=ALU.add), r=[xb, rawb], w=[xmb])
                    k.act(lambda e: e.activation(out=RAW[:, :, 0:1], in_=RAW[:, :, 128:129], func=AF.Copy), r=[rawb], w=[rawb])
                    if rstage < 1:
                        tap("rxm", XM[:], xmb, [128, 8, 128])
                        break
                    k.act(lambda e: e.activation(out=TZW[0:64, :], in_=XM[0:64, 6, :], func=AF.Tanh), r=[xmb], w=[tzb])
                    mm(PS[2][:, 0:256], TZW[0:65, :], W2E[0:65, :], True, True, [tzb, pb], [PSb[2]], sw=True)
                    k.act(lambda e: e.activation(out=SG[:], in_=PS[2][:, 0:256], func=AF.Sigmoid), r=[PSb[2]], w=[sgb])
                    for c in range(2):
                        mm(PS[3][:, c * 256:(c + 1) * 256], SG[:, c * 128:(c + 1) * 128], MLELT[:], True, True, [sgb, pb], [PSb[3]])
                    P3 = PS[3][:].rearrange("p (c t n) -> p c t n", c=2, t=2)
                    k.act(lambda e: e.activation(out=EP[:], in_=P3[:, :, 0, :], func=AF.Exp, scale=SDEC), r=[PSb[3]], w=[eb])
                    k.act(lambda e: e.activation(out=EM[:], in_=P3[:, :, 0, :], func=AF.Exp, scale=-SDEC), r=[PSb[3]], w=[eb])
                    k.act(lambda e: e.activation(out=EX[:], in_=P3[:, :, 1, :], func=AF.Exp, scale=SDEC), r=[PSb[3]], w=[eb])
                    for c in range(2):
                        mm(PS[4][:, c * 128:(c + 1) * 128], A2[64:128, c * 128:(c + 1) * 128], XM[64:128, 6, :], True, True, [pb, xmb], [PSb[4]], sw=True)
                    for c in range(2):
                        k.act(lambda e: e.activation(out=AT[:, c, :], in_=PS[4][:, c * 128:(c + 1) * 128], func=AF.Sigmoid, bias=A0[:, c:c + 1]), r=[PSb[4], pb], w=[ab])
                    k.dve(lambda e: e.tensor_tensor(out=KK0[:], in0=XM[:, 2:4, :], in1=bc2(KKp), op=ALU.mult), r=[xmb, pb], w=[kb])
                    k.dve(lambda e: e.tensor_tensor(out=SQ[:], in0=KK0[:], in1=KK0[:], op=ALU.mult), r=[kb], w=[kb])
                    for c in range(2):
                        mm(PS[4][:, 256 + c * 128:256 + (c + 1) * 128], BLK[:], SQ[:, c, :], True, True, [cb, kb], [PSb[4]])
                    k.act(lambda e: e.activation(out=RN[:], in_=v4(PS[4][:, 256:512], 2), func=AF.Sqrt), r=[PSb[4]], w=[kb])
                    k.dve(lambda e: e.tensor_scalar(out=RN[:], in0=RN[:], scalar1=1e-12, scalar2=None, op0=ALU.max), r=[kb], w=[kb])
                    k.dve(lambda e: e.reciprocal(out=RN[:], in_=RN[:]), r=[kb], w=[kb])
                    k.dve(lambda e: e.tensor_tensor(out=KK0[:], in0=KK0[:], in1=RN[:], op=ALU.mult), r=[kb], w=[kb])
                    k.dve(lambda e: e.tensor_tensor(out=T1[:], in0=AT[:], in1=bc2(KA), op=ALU.mult), r=[ab, pb], w=[kb])
                    k.dve(lambda e: e.tensor_tensor(out=T1[:], in0=T1[:], in1=bc2(OMK), op=ALU.add), r=[kb, pb], w=[kb])
                    k.dve(lambda e: e.tensor_tensor(out=K2[:], in0=XM[:, 2:4, :], in1=T1[:], op=ALU.mult), r=[xmb, kb], w=[kb])
                    PCb = EP[:, :, 127:128].to_broadcast([128, 2, 128])
                    k.dve(lambda e: e.tensor_tensor(out=BRt[:, :, 1, :], in0=XM[:, 0:2, :], in1=EP[:], op=ALU.mult), r=[xmb, eb], w=[opb])
                    k.dve(lambda e: e.tensor_tensor(out=BRt[:, :, 0, :], in0=KK0[:], in1=EX[:], op=ALU.mult), r=[kb, eb], w=[opb])
                    k.dve(lambda e: e.tensor_tensor(out=KT32[:], in0=K2[:], in1=EM[:], op=ALU.mult), r=[kb, eb], w=[opb])
                    k.act(lambda e: e.activation(out=KTt[:], in_=KT32[:], func=AF.Copy), r=[opb], w=[opb])
                    k.dve(lambda e: e.tensor_tensor(out=KP32[:], in0=KT32[:], in1=PCb, op=ALU.mult), r=[opb, eb], w=[opb])
                    k.dve(lambda e: e.scalar_tensor_tensor(out=NB_[:], in0=KK0[:], scalar=-1.0, in1=AT[:], op0=ALU.mult, op1=ALU.mult), r=[kb, ab], w=[opb])
                    k.dve(lambda e: e.tensor_tensor(out=AL32[:], in0=NB_[:], in1=EM[:], op=ALU.mult), r=[opb, eb], w=[opb])
                    k.act(lambda e: e.activation(out=ALt[:], in_=AL32[:], func=AF.Copy), r=[opb], w=[opb])
                    k.dve(lambda e: e.tensor_tensor(out=AP32[:], in0=AL32[:], in1=PCb, op=ALU.mult), r=[opb, eb], w=[opb])
                    for c in range(2):
                        k.pe(lambda e: e.transpose(out=PS[5][:, c * 128:(c + 1) * 128], in_=KP32[:, c, :], identity=IDF[:]), r=[opb, cb], w=[PSb[5]])
                        k.pe(lambda e: e.transpose(out=PS[5][:, 256 + c * 128:256 + (c + 1) * 128], in_=AP32[:, c, :], identity=IDF[:]), r=[opb, cb], w=[PSb[5]])
                        k.pe(lambda e: e.transpose(out=PS[6][:, c * 128:(c + 1) * 128], in_=XM[:, 4 + c, :], identity=IDF[:]), r=[xmb, cb], w=[PSb[6]])
                    k.act(lambda e: e.activation(out=KPT[:], in_=PS[5][:, 0:256], func=AF.Copy), r=[PSb[5]], w=[tmb])
                    k.act(lambda e: e.activation(out=APT[:], in_=PS[5][:, 256:512], func=AF.Copy), r=[PSb[5]], w=[tmb])
                    k.act(lambda e: e.activation(out=VT[:], in_=PS[6][:, 0:256], func=AF.Copy), r=[PSb[6]], w=[tmb])
                    k.dve(lambda e: e.tensor_copy(out=V32[:], in_=PS[6][:, 0:256]), r=[PSb[6]], w=[tmb])
                    k.dve(lambda e: e.tensor_tensor(out=T2[:], in0=XM[:, 0:2, :], in1=K2[:], op=ALU.mult), r=[xmb, kb], w=[kb])
                    k.dve(lambda e: e.tensor_tensor(out=T2[:], in0=T2[:], in1=bc2(RKp), op=ALU.mult), r=[kb, pb], w=[kb])
                    for c in range(2):
                        mm(PS[6][:, 256 + 2 * c:256 + 2 * c + 2], T2[:, c, :], HSEL[:], True, True, [kb, cb], [PSb[6]])
                    k.act(lambda e: e.activation(out=BON[:], in_=PS[6][:, 256:260], func=AF.Copy), r=[PSb[6]], w=[tmb])
                    k.act(lambda e: e.activation(out=SZ[:], in_=XM[:, 7, :], func=AF.Sigmoid), r=[xmb], w=[tmb])
                    mm(PS[2][:, 256:512], SZ[:], G2[:], True, True, [tmb, pb], [PSb[2]])
                    k.act(lambda e: e.activation(out=GTM_[:], in_=PS[2][:, 256:512], func=AF.Copy), r=[PSb[2]], w=[tmb])
                    if rstage < 2:
                        tap("rkpt", KPT[:], tmb, [128, 256])
                        tap("rbrt", BRt[:], opb, [128, 2, 2, 128])
                        break
                    hsl = lambda h: slice(64 * (h % 2), 64 * (h % 2) + 64)
                    for h in range(4):
                        c = h // 2
                        mm(PS[h // 2][:, (h % 2) * 256:(h % 2 + 1) * 256], KTt[hsl(h), c, :], BRt[hsl(h), c].rearrange("p t n -> p (t n)"), True, True, [opb], [PSb[h // 2]], sw=True)
                    for h in range(4):
                        c = h // 2
                        mm(PS[2 + h // 2][:, (h % 2) * 256:(h % 2 + 1) * 256], ALt[hsl(h), c, :], BRt[hsl(h), c].rearrange("p t n -> p (t n)"), True, True, [opb], [PSb[2 + h // 2]], sw=True)
                    for h in range(4):
                        c = h // 2
                        mm(PS[4][:, h * 128:(h + 1) * 128], BRt[hsl(h), c, 0, :], ALt[hsl(h), c, :], True, True, [opb], [PSb[4]], sw=True)
                    for i_ in range(2):
                        k.dve(lambda e: e.tensor_tensor(out=A1m[:, 2 * i_:2 * i_ + 2, :], in0=v4(PS[i_][:], 2), in1=MLTLE[:].unsqueeze(1).to_broadcast([128, 2, 256]), op=ALU.mult), r=[PSb[i_], pb], w=[a1b])
                    for i_ in range(2):
                        k.dve(lambda e: e.tensor_tensor(out=A2m[:, 2 * i_:2 * i_ + 2, :], in0=v4(PS[2 + i_][:], 2), in1=MLTLE[:].unsqueeze(1).to_broadcast([128, 2, 256]), op=ALU.mult), r=[PSb[2 + i_], pb], w=[a2b])
                    k.dve(lambda e: e.tensor_tensor(out=Z[0][:], in0=v4(PS[4][:], 4), in1=MGT[:].unsqueeze(1).to_broadcast([128, 4, 128]), op=ALU.mult), r=[PSb[4], cb], w=[zb[0]])
                    k.dve(lambda e: e.tensor_tensor(out=TT[0][:], in0=A2m[:, :, 0:128], in1=IDF[:].unsqueeze(1).to_broadcast([128, 4, 128]), op=ALU.add), r=[a2b, cb], w=[ttb[0]])
                    for i_ in range(1, 7):
                        o_, n_ = (i_ - 1) % 2, i_ % 2
                        if i_ == 1:
                            zt_old, zt_oldb = (lambda h: A2m[:, h, 0:128]), a2b
                        else:
                            zt_old, zt_oldb = (lambda h, o_=o_: ZT[o_][:, h, :]), ztb[o_]
                        for h in range(4):
                            mm(PS[5][:, h * 128:(h + 1) * 128], zt_old(h), Z[o_][:, h, :], True, True, [zt_oldb, zb[o_]], [PSb[5]])
                        k.act(lambda e: e.activation(out=Z[n_][:], in_=v4(PS[5][:], 4), func=AF.Copy), r=[PSb[5]], w=[zb[n_]])
                        if i_ < 6:
                            for h in range(4):
                                mm(PS[6][:, h * 128:(h + 1) * 128], Z[o_][:, h, :], zt_old(h), True, True, [zt_oldb, zb[o_]], [PSb[6]])
                            k.dve(lambda e: e.tensor_copy(out=ZT[n_][:], in_=v4(PS[6][:], 4)), r=[PSb[6]], w=[ztb[n_]])
                        for h in range(4):
                            mm(PS[7][:, h * 128:(h + 1) * 128], Z[n_][:, h, :], TT[o_][:, h, :], True, True, [zb[n_], ttb[o_]], [PSb[7]])
                        k.dve(lambda e: e.tensor_tensor(out=TT[n_][:], in0=v4(PS[7][:], 4), in1=TT[o_][:], op=ALU.add), r=[PSb[7], ttb[o_]], w=[ttb[n_]])
                    TTf, ttfb = TT[0], ttb[0]
                    hc = lambda h: slice(h * 64, (h + 1) * 64)
                    for h in range(4):
                        c = h // 2
                        mm(PS[0][:, hc(h)], BRt[hsl(h), c, 0, :], MBF[hsl(h), c, :], True, False, [opb, mbfb], [PSb[0]], sw=True)
                        mm(PS[0][:, hc(h)], A1m[:, h, 0:128], VT[:, hc(h)], False, True, [a1b, tmb], [PSb[0]])
                    k.act(lambda e: e.activation(out=X0[:], in_=v4(PS[0][:, 0:256], 4), func=AF.Copy), r=[PSb[0]], w=[x0b])
                    for h in range(4):
                        mm(PS[1][:, hc(h)], TTf[:, h, :], X0[:, h, :], True, True, [ttfb, x0b], [PSb[1]])
                    k.act(lambda e: e.activation(out=UH[:], in_=v4(PS[1][:, 0:256], 4), func=AF.Copy), r=[PSb[1]], w=[uhb])
                    for h in range(4):
                        c = h // 2
                        mm(PS[2][:, hc(h)], BRt[hsl(h), c, 1, :], MBF[hsl(h), c, :], True, False, [opb, mbfb], [PSb[2]], sw=True)
                        mm(PS[2][:, hc(h)], A1m[:, h, 128:256], VT[:, hc(h)], False, False, [a1b, tmb], [PSb[2]])
                        mm(PS[2][:, hc(h)], A2m[:, h, 128:256], UH[:, h, :], False, True, [a2b, uhb], [PSb[2]])
                    for h in range(4):
                        c = h // 2
                        mm(PS[3][hsl(h), c * 64:(c + 1) * 64], KPT[:, hc(h)], VT[:, hc(h)], True, False, [tmb], [PSb[3]])
                        mm(PS[3][hsl(h), c * 64:(c + 1) * 64], APT[:, hc(h)], UH[:, h, :], False, True, [tmb, uhb], [PSb[3]])
                    k.dve(lambda e: e.tensor_tensor(out=M32[:], in0=M32[:], in1=EP[:, :, 127:128].to_broadcast([128, 2, 64]), op=ALU.mult), r=[mb, eb], w=[mb])
                    k.dve(lambda e: e.tensor_tensor(out=M32[:], in0=M32[:], in1=v4(PS[3][:, 0:128], 2), op=ALU.add), r=[mb, PSb[3]], w=[mb])
                    k.act(lambda e: e.activation(out=MBF[:], in_=M32[:], func=AF.Copy), r=[mb], w=[mbfb])
                    k.act(lambda e: e.activation(out=Y[:], in_=v4(PS[2][:, 0:256], 4), func=AF.Copy), r=[PSb[2]], w=[yb])
                    if rstage < 3:
                        tap("ry", Y[:], yb, [128, 4, 64])
                        break
                    k.dve(lambda e: e.tensor_reduce(out=S1[:], in_=Y[:], axis=AX.X, op=ALU.add), r=[yb], w=[yb])
                    k.dve(lambda e: e.tensor_tensor(out=SQY[:], in0=Y[:], in1=Y[:], op=ALU.mult), r=[yb], w=[yb])
                    k.dve(lambda e: e.tensor_reduce(out=S2[:], in_=SQY[:], axis=AX.X, op=ALU.add), r=[yb], w=[yb])
                    k.dve(lambda e: e.tensor_scalar(out=S1[:], in0=S1[:], scalar1=1.0 / 64, scalar2=None, op0=ALU.mult), r=[yb], w=[yb])
                    k.dve(lambda e: e.tensor_tensor(out=MS[:], in0=S1[:], in1=S1[:], op=ALU.mult), r=[yb], w=[yb])
                    k.dve(lambda e: e.scalar_tensor_tensor(out=S2[:], in0=S2[:], scalar=1.0 / 64, in1=MS[:], op0=ALU.mult, op1=ALU.subtract), r=[yb], w=[yb])
                    k.act(lambda e: e.activation(out=S2[:], in_=S2[:], func=AF.Sqrt, bias=RWKV_GN_EPS), r=[yb], w=[yb])
                    k.dve(lambda e: e.reciprocal(out=S2[:], in_=S2[:]), r=[yb], w=[yb])
                    k.dve(lambda e: e.tensor_tensor(out=Y[:], in0=Y[:], in1=S1[:].unsqueeze(2).to_broadcast([128, 4, 64]), op=ALU.subtract), r=[yb], w=[yb])
                    k.dve(lambda e: e.tensor_tensor(out=Y[:], in0=Y[:], in1=S2[:].unsqueeze(2).to_broadcast([128, 4, 64]), op=ALU.mult), r=[yb], w=[yb])
                    Yf = Y[:].rearrange("p h d -> p (h d)")
                    k.dve(lambda e: e.tensor_tensor(out=Yf, in0=Yf, in1=LNG[:], op=ALU.mult), r=[yb, pb], w=[yb])
                    k.dve(lambda e: e.tensor_tensor(out=Yf, in0=Yf, in1=LNB[:], op=ALU.add), r=[yb, pb], w=[yb])
                    k.dve(lambda e: e.tensor_tensor(out=SQY[:], in0=v4(V32[:], 4), in1=BON[:].unsqueeze(2).to_broadcast([128, 4, 64]), op=ALU.mult), r=[tmb, yb], w=[yb])
                    k.dve(lambda e: e.tensor_tensor(out=Y[:], in0=Y[:], in1=SQY[:], op=ALU.add), r=[yb], w=[yb])
                    k.dve(lambda e: e.tensor_tensor(out=Yf, in0=Yf, in1=GTM_[:], op=ALU.mult), r=[yb, tmb], w=[yb])
                    for cc in range(2):
                        k.pe(lambda e: e.transpose(out=PS[5][:, cc * 128:(cc + 1) * 128], in_=Yf[:, cc * 128:(cc + 1) * 128], identity=IDF[:]), r=[yb, cb], w=[PSb[5]])
                    k.act(lambda e: e.activation(out=BR[:, 6:8, tsl], in_=v4(PS[5][:, 0:256], 2), func=AF.Copy), r=[PSb[5]], w=[BRb])
                k.barrier()


        def gla_setup(l, ph):
            G = Prog()
            WG = sb(ph, "gw", [128, 8, 784], BF16)
            W2F = sb(ph, "gw2f", [17, 128])
            GN = sb(ph, "ggn", [128, 64])
            S32 = sb(ph, "gs32", [128, 64])
            SBF = sb(ph, "gsbf", [128, 64], BF16)
            GZT = sb(ph, "ggzt", [17, 128])
            E1 = sb(ph, "ge1", [128, 128])
            LL = sb(ph, "gll", [128, 128])
            EP = sb(ph, "gep", [128, 128])
            EM = sb(ph, "gem", [128, 128])
            E3 = sb(ph, "ge3", [128, 128])
            KTM = sb(ph, "gktm", [128, 128])
            QKr = sb(ph, "gqkr", [128, 256])
            QS = sb(ph, "gqs", [128, 128], BF16)
            KS = sb(ph, "gks", [128, 128], BF16)
            KH = sb(ph, "gkh", [128, 128], BF16)
            V = sb(ph, "gv", [128, 256], BF16)
            SOG = sb(ph, "gsog", [128, 256])
            AM = sb(ph, "gam", [128, 4, 128], BF16)
            O = sb(ph, "go", [128, 4, 64])
            SQ = sb(ph, "gsq", [128, 4, 64])
            SS = sb(ph, "gss", [128, 4])
            wb_, pb, sb_, zb, eb, qb_, vb_, ob, rb_, rq_ = [Buf() for _ in range(10)]
            k.dma("pool", WG[:, :, 128:512], win(l, 128, 384), writes=[wb_])
            k.dma("pool", WG[:, :, 512:784], win(l, 512, 272), writes=[wb_])
            k.dma("pool", WG[:, :, 0:128], win(l, 0, 128), writes=[wb_])
            k.dma("sp", W2F[0:16, :], dr["gla_gate_w2"][l], writes=[pb])
            k.dma("sp", W2F[16:17, :], dr["gla_gate_b"][l:l + 1, :], writes=[pb])
            k.dma("sp", GN[:], dr["gla_norm"][l:l + 1, :].partition_broadcast(128), writes=[pb])
            k.pool(lambda e: e.memset(S32[:], 0.0), w=[sb_])
            k.pool(lambda e: e.memset(SBF[:], 0.0), w=[sb_])
            k.pool(lambda e: e.memset(GZT[:], 1.0), w=[zb])
            X_, Xb, Y_, Yb = PS[6], PSb[6], PS[7], PSb[7]

            def gen(BR, BRb):
                for tt in range(NT):
                    tsl = slice(tt * 128, (tt + 1) * 128)
                    for kc in range(8):
                        mm(X_[:, 0:384], HT[:, kc, tsl], WG[:, kc, 128:512], kc == 0, kc == 7, [wb_, HTb], [Xb])
                    k.act(lambda e: e.activation(out=KTM[:], in_=X_[:, 0:128], func=AF.Copy), r=[Xb], w=[rb_])
                    k.act(lambda e: e.activation(out=V[:], in_=X_[:, 128:384], func=AF.Copy), r=[Xb], w=[vb_])
                    yield
                    for kc in range(8):
                        mm(Y_[:, 0:256], HT[:, kc, tsl], WG[:, kc, 512:768], kc == 0, kc == 7, [wb_, HTb], [Yb])
                    k.act(lambda e: e.activation(out=SOG[:], in_=Y_[:, 0:256], func=AF.Silu), r=[Yb], w=[vb_])
                    yield
                    for kc in range(8):
                        mm(X_[:, 0:128], WG[:, kc, 0:128], HT[:, kc, tsl], kc == 0, kc == 7, [wb_, HTb], [Xb])
                    for kc in range(8):
                        mm(X_[:, 128:256], WG[:, kc, 128:256], HT[:, kc, tsl], kc == 0, kc == 7, [wb_, HTb], [Xb])
                    k.dve(lambda e: e.tensor_copy(out=QKr[:], in_=X_[:, 0:256]), r=[Xb], w=[rq_])
                    yield
                    for kc in range(8):
                        mm(Y_[0:16, 0:128], WG[:, kc, 768:784], HT[:, kc, tsl], kc == 0, kc == 7, [wb_, HTb], [Yb])
                    k.act(lambda e: e.activation(out=GZT[0:16, :], in_=Y_[0:16, 0:128], func=AF.Copy), r=[Yb], w=[zb])
                    mm(Y_[:, 128:256], GZT[0:17, :], W2F[0:17, :], True, True, [zb, pb], [Yb])
                    k.act(lambda e: e.activation(out=E1[:], in_=Y_[:, 128:256], func=AF.Exp, scale=-1.0), r=[Yb], w=[eb])
                    k.act(lambda e: e.activation(out=LL[:], in_=E1[:], func=AF.Ln, bias=1.0), r=[eb], w=[eb])
                    yield
                    mm(X_[:, 0:128], LL[:], MLE[:], True, True, [eb, cb], [Xb])
                    mm(X_[:, 128:256], MGT[:], LL[:], True, True, [eb, cb], [Xb])
                    k.act(lambda e: e.activation(out=EP[:], in_=X_[:, 0:128], func=AF.Exp, scale=-1.0 / 16), r=[Xb], w=[eb])
                    k.act(lambda e: e.activation(out=EM[:], in_=X_[:, 0:128], func=AF.Exp, scale=1.0 / 16), r=[Xb], w=[eb])
                    k.act(lambda e: e.activation(out=E3[:], in_=X_[:, 128:256], func=AF.Exp, scale=-1.0 / 16), r=[Xb], w=[eb])
                    yield
                    k.dve(lambda e: e.scalar_tensor_tensor(out=QS[:], in0=QKr[:, 0:128], scalar=32.0 ** -0.5, in1=EP[:], op0=ALU.mult, op1=ALU.mult), r=[rq_, eb], w=[qb_])
                    k.dve(lambda e: e.tensor_tensor(out=KS[:], in0=QKr[:, 128:256], in1=EM[:], op=ALU.mult), r=[rq_, eb], w=[qb_])
                    k.dve(lambda e: e.tensor_tensor(out=KH[:], in0=KTM[:], in1=E3[:], op=ALU.mult), r=[rb_, eb], w=[qb_])
                    yield
                    for h in range(4):
                        mm(Y_[:, h * 128:(h + 1) * 128], KS[32 * h:32 * h + 32, :], QS[32 * h:32 * h + 32, :], True, True, [qb_], [Yb], tp=(32 * h, 0), sw=True)
                    k.dve(lambda e: e.tensor_tensor(out=AM[:], in0=Y_[:].rearrange("p (h n) -> p h n", h=4), in1=MLE[:].unsqueeze(1).to_broadcast([128, 4, 128]), op=ALU.mult), r=[Yb, cb], w=[qb_])
                    yield
                    for h in range(4):
                        mm(X_[:, h * 64:(h + 1) * 64], AM[:, h, :], V[:, h * 64:(h + 1) * 64], True, False, [qb_, vb_], [Xb])
                        mm(X_[:, h * 64:(h + 1) * 64], QS[32 * h:32 * h + 32, :], SBF[32 * h:32 * h + 32, :], False, True, [qb_, sb_], [Xb], tp=(32 * h, 0))
                    mm(Y_[:, 0:256], KH[:], V[:], True, True, [qb_, vb_], [Yb])
                    for h in range(4):
                        hs = slice(32 * h, 32 * h + 32)
                        k.dve(lambda e: e.scalar_tensor_tensor(out=S32[hs, :], in0=S32[hs, :], scalar=EP[hs, 127:128], in1=Y_[hs, h * 64:(h + 1) * 64], op0=ALU.mult, op1=ALU.add), r=[sb_, eb, Yb], w=[sb_])
                    k.dve(lambda e: e.tensor_copy(out=SBF[:], in_=S32[:]), r=[sb_], w=[sb_])
                    k.act(lambda e: e.activation(out=O[:], in_=X_[:, 0:256].rearrange("p (h d) -> p h d", h=4), func=AF.Copy), r=[Xb], w=[ob])
                    yield
                    k.dve(lambda e: e.tensor_tensor(out=SQ[:], in0=O[:], in1=O[:], op=ALU.mult), r=[ob], w=[ob])
                    k.dve(lambda e: e.tensor_reduce(out=SS[:], in_=SQ[:], axis=AX.X, op=ALU.add), r=[ob], w=[ob])
                    k.act(lambda e: e.activation(out=SS[:], in_=SS[:], func=AF.Sqrt, scale=1.0 / 64, bias=RMS_EPS), r=[ob], w=[ob])
                    k.dve(lambda e: e.reciprocal(out=SS[:], in_=SS[:]), r=[ob], w=[ob])
                    yield
                    k.dve(lambda e: e.tensor_tensor(out=O[:], in0=O[:], in1=SS[:].unsqueeze(2).to_broadcast([128, 4, 64]), op=ALU.mult), r=[ob], w=[ob])
                    k.dve(lambda e: e.tensor_tensor(out=O[:], in0=O[:], in1=GN[:].unsqueeze(1).to_broadcast([128, 4, 64]), op=ALU.mult), r=[ob, pb], w=[ob])
                    k.dve(lambda e: e.tensor_tensor(out=O[:], in0=O[:], in1=SOG[:].rearrange("p (h d) -> p h d", h=4), op=ALU.mult), r=[ob, vb_], w=[ob])
                    Of = O[:].rearrange("p h d -> p (h d)")
                    for cc in range(2):
                        k.pe(lambda e: e.transpose(out=X_[:, cc * 128:(cc + 1) * 128], in_=Of[:, cc * 128:(cc + 1) * 128], identity=IDF[:]), r=[ob, cb], w=[Xb])
                    k.act(lambda e: e.activation(out=BR[:, 0:2, tsl], in_=X_[:, 0:256].rearrange("p (c n) -> p c n", c=2), func=AF.Copy), r=[Xb], w=[BRb])
                    yield

            return gen

        def run_gens(gens, weights=None):
            gens = list(gens)
            weights = list(weights) if weights is not None else [1] * len(gens)
            live = list(range(len(gens)))
            while live:
                for gi in list(live):
                    for _ in range(weights[gi]):
                        try:
                            next(gens[gi])
                        except StopIteration:
                            live.remove(gi)
                            break

        def phase_rwkv2(l, BR, BRb, with_gla=False):
            SDEC = -math.exp(-0.5)
            with ExitStack() as ph:
                extra_gens = []
                if with_gla:
                    extra_gens.append(gla_setup(l, ph)(BR, BRb))
                f32 = lambda n, shp: sb(ph, n, shp)
                b16 = lambda n, shp: sb(ph, n, shp, BF16)
                WR = b16("rw", [128, 8, 1024])
                MU = f32("rmu", [128, 8])
                W2E = f32("rw2e", [65, 256])
                A2 = f32("ra2", [128, 256])
                G2 = b16("rg2", [128, 256])
                A0 = f32("ra0", [128, 2])
                KKp = f32("rkk", [128, 2])
                KA = f32("rka", [128, 2])
                OMK = f32("romk", [128, 2])
                RKp = f32("rrk", [128, 2])
                LNG = f32("rlng", [128, 256])
                LNB = f32("rlnb", [128, 256])
                MLELT = f32("rmlelt", [128, 256])
                MLTLE = f32("rmltle", [128, 256])
                M32 = f32("rm32", [128, 2, 64])
                MBF = b16("rmbf", [128, 2, 64])
                X0 = b16("rx0", [128, 4, 64])
                UH = b16("ruh", [128, 4, 64])
                Y = f32("ry", [128, 4, 64])
                SQY = f32("rsqy", [128, 4, 64])
                S1 = f32("rs1", [128, 4])
                S2 = f32("rs2", [128, 4])
                MS = f32("rms", [128, 4])
                wb_, pb, x0b, uhb, mb, mbfb, yb = [Buf() for _ in range(7)]

                class SetT:
                    pass

                sets = []
                for p in range(2):
                    S_ = SetT()
                    n_ = lambda nm: "r%d%s" % (p, nm)
                    S_.RAW = f32(n_("raw"), [128, 8, 129])
                    S_.XM = f32(n_("xm"), [128, 8, 128])
                    S_.TZW = f32(n_("tzw"), [65, 128])
                    S_.SG = f32(n_("sg"), [128, 256])
                    S_.EP = f32(n_("ep"), [128, 2, 128])
                    S_.EM = f32(n_("em"), [128, 2, 128])
                    S_.EX = f32(n_("ex"), [128, 2, 128])
                    S_.AT = f32(n_("at"), [128, 2, 128])
                    S_.KK0 = f32(n_("kk0"), [128, 2, 128])
                    S_.SQ = f32(n_("sq"), [128, 2, 128])
                    S_.T1 = f32(n_("t1"), [128, 2, 128])
                    S_.K2 = f32(n_("k2"), [128, 2, 128])
                    S_.KT32 = f32(n_("kt32"), [128, 2, 128])
                    S_.AL32 = f32(n_("al32"), [128, 2, 128])
                    S_.KTt = b16(n_("ktt"), [128, 2, 128])
                    S_.ALt = b16(n_("alt"), [128, 2, 128])
                    S_.SZ = b16(n_("sz"), [128, 128])
                    S_.Z = [b16(n_("z%d" % i_), [128, 4, 128]) for i_ in range(2)]
                    S_.ZT = [b16(n_("zt%d" % i_), [128, 4, 128]) for i_ in range(2)]
                    S_.TT = [b16(n_("tt%d" % i_), [128, 4, 128]) for i_ in range(2)]
                    S_.BRt = b16(n_("brt"), [128, 2, 2, 128])
                    S_.A1m = b16(n_("a1m"), [128, 4, 256])
                    S_.A2m = b16(n_("a2m"), [128, 4, 256])
                    S_.KPT = b16(n_("kpt"), [128, 256])
                    S_.APT = b16(n_("apt"), [128, 256])
                    S_.VT = b16(n_("vt"), [128, 256])
                    S_.V32 = f32(n_("v32"), [128, 256])
                    S_.BON = f32(n_("bon"), [128, 4])
                    S_.GTM_ = f32(n_("gtm"), [128, 256])
                    S_.PCs = f32(n_("pcs"), [128, 2])
                    (S_.rawb, S_.xmb, S_.tzb, S_.sgb, S_.eb, S_.ab, S_.kb, S_.opb, S_.tmb, S_.a1b, S_.a2b) = [Buf() for _ in range(11)]
                    (S_.kpb, S_.vtb, S_.v32b, S_.bonb, S_.szb, S_.gtb, S_.pcb) = [Buf() for _ in range(7)]
                    S_.zb, S_.ztb, S_.ttb = [Buf(), Buf()], [Buf(), Buf()], [Buf(), Buf()]
                    S_.B = [2 * p, 2 * p + 1, 2 * p]
                    k.pool(lambda e: e.memset(S_.TZW[:], 1.0), w=[S_.tzb])
                    sets.append(S_)
                k.pool(lambda e: e.memset(sets[0].RAW[:, :, 0:1], 0.0), w=[sets[0].rawb])
                wbq = [Buf() for _ in range(4)]
                for q_ in range(4):
                    k.dma("pool", WR[:, :, q_ * 256:(q_ + 1) * 256], win(l, 2064 + q_ * 256, 256), writes=[wbq[q_]])
                k.dma("sp", MU[:], dr["rwkv_mu_t"][l], writes=[pb])
                k.dma("sp", W2E[0:64, :], dr["rwkv_w2"][l], writes=[pb])
                k.dma("sp", W2E[64:65, :], dr["rwkv_w0"][l:l + 1, :], writes=[pb])
                k.dma("sp", A2[64:128, :], dr["rwkv_a2"][l], writes=[pb])
                k.dma("pool", G2[:], dr["rwkv_g2"][l], writes=[pb])
                k.dma("sp", A0[:], dr["rwkv_a0_t"][l], writes=[pb])
                k.dma("sp", KKp[:], dr["rwkv_kk_t"][l], writes=[pb])
                k.dma("sp", KA[:], dr["rwkv_ka_t"][l], writes=[pb])
                k.dma("sp", RKp[:], dr["rwkv_rk_t"][l], writes=[pb])
                k.dma("sp", LNG[:], dr["rwkv_ln_g"][l:l + 1, :].partition_broadcast(128), writes=[pb])
                k.dma("sp", LNB[:], dr["rwkv_ln_b"][l:l + 1, :].partition_broadcast(128), writes=[pb])
                k.dve(lambda e: e.tensor_scalar(out=OMK[:], in0=KA[:], scalar1=-1.0, scalar2=1.0, op0=ALU.mult, op1=ALU.add), r=[pb], w=[pb])
                k.dve(lambda e: e.tensor_copy(out=MLELT[:, 0:128], in_=MLE[:]), r=[cb], w=[pb])
                k.dve(lambda e: e.tensor_copy(out=MLELT[:, 128:256], in_=MLT[:]), r=[cb], w=[pb])
                k.dve(lambda e: e.tensor_copy(out=MLTLE[:, 0:128], in_=MLT[:]), r=[cb], w=[pb])
                k.dve(lambda e: e.tensor_copy(out=MLTLE[:, 128:256], in_=MLE[:]), r=[cb], w=[pb])
                k.pool(lambda e: e.memset(M32[:], 0.0), w=[mb])
                k.pool(lambda e: e.memset(MBF[:], 0.0), w=[mbfb])
                bc2 = lambda t_, n=128: t_[:].unsqueeze(2).to_broadcast([128, 2, n])
                v4 = lambda ap, h: ap.rearrange("p (h n) -> p h n", h=h)
                hsl = lambda h: slice(64 * (h % 2), 64 * (h % 2) + 64)
                hc = lambda h: slice(h * 64, (h + 1) * 64)
                prep_done = [False] * NT
                chain_done = [False] * NT

                def prep_gen(p):
                    st = sets[p]
                    a_, b_, c_ = st.B
                    for _ in range(p * int(os.environ.get("K_STAG", "0"))):
                        yield
                    RAW, XM, TZW, SG, EP, EM, EX, AT, KK0, SQ, T1, K2 = st.RAW, st.XM, st.TZW, st.SG, st.EP, st.EM, st.EX, st.AT, st.KK0, st.SQ, st.T1, st.K2
                    KT32, AL32, KTt, ALt, SZ, Z, ZT, TT = st.KT32, st.AL32, st.KTt, st.ALt, st.SZ, st.Z, st.ZT, st.TT
                    BRt, A1m, A2m, KPT, APT, VT, V32, BON, GTM_, PCs = st.BRt, st.A1m, st.A2m, st.KPT, st.APT, st.VT, st.V32, st.BON, st.GTM_, st.PCs
                    rawb, xmb, tzb, sgb, eb, ab, kb, opb, tmb, a1b, a2b, zb, ztb, ttb = st.rawb, st.xmb, st.tzb, st.sgb, st.eb, st.ab, st.kb, st.opb, st.tmb, st.a1b, st.a2b, st.zb, st.ztb, st.ttb
                    kpb, vtb, v32b, bonb, szb, gtb, pcb = st.kpb, st.vtb, st.v32b, st.bonb, st.szb, st.gtb, st.pcb
                    for tt in range(p, NT, 2):
                        t0 = tt * 128
                        nco = 128 if tt == 0 else 129
                        src0 = t0 if tt == 0 else t0 - 1
                        for groups in (((a_, (0, 1, 2)), (b_, (3, 4, 5))), ((a_, (6, 7)),)):
                            for bk, chs in groups:
                                for ji, j in enumerate(chs):
                                    for kc in range(8):
                                        mm(PS[bk][:, ji * 129:ji * 129 + nco], WR[:, kc, j * 128:(j + 1) * 128], HT[:, kc, src0:t0 + 128], kc == 0, kc == 7, [wbq[j // 2], HTb], [PSb[bk]])
                                yield
                            for bk, chs in groups:
                                n3 = len(chs)
                                src = PS[bk][:, 0:n3 * 129].rearrange("p (j n) -> p j n", j=n3)[:, :, 0:nco]
                                k.act(lambda e: e.activation(out=RAW[:, chs[0]:chs[0] + n3, 129 - nco:129], in_=src, func=AF.Copy), r=[PSb[bk]], w=[rawb])
                            yield
                        sh_eng = k.pool if os.environ.get("K_POOLSHIFT", "0") == "1" else k.dve
                        sh_eng(lambda e: e.tensor_tensor(out=XM[:], in0=RAW[:, :, 0:128], in1=RAW[:, :, 1:129], op=ALU.subtract), r=[rawb], w=[xmb])
                        sh_eng(lambda e: e.tensor_tensor(out=XM[:], in0=XM[:], in1=MU[:].unsqueeze(2).to_broadcast([128, 8, 128]), op=ALU.mult), r=[xmb, pb], w=[xmb])
                        sh_eng(lambda e: e.tensor_tensor(out=XM[:], in0=XM[:], in1=RAW[:, :, 1:129], op=ALU.add), r=[xmb, rawb], w=[xmb])
                        yield
                        k.act(lambda e: e.activation(out=TZW[0:64, :], in_=XM[0:64, 6, :], func=AF.Tanh), r=[xmb], w=[tzb])
                        mm(PS[a_][:, 0:256], TZW[0:65, :], W2E[0:65, :], True, True, [tzb, pb], [PSb[a_]], sw=True)
                        k.act(lambda e: e.activation(out=SG[:], in_=PS[a_][:, 0:256], func=AF.Sigmoid), r=[PSb[a_]], w=[sgb])
                        yield
                        for c in range(2):
                            mm(PS[b_][:, c * 256:(c + 1) * 256], SG[:, c * 128:(c + 1) * 128], MLELT[:], True, True, [sgb, pb], [PSb[b_]])
                        P3 = PS[b_][:].rearrange("p (c t n) -> p c t n", c=2, t=2)
                        k.act(lambda e: e.activation(out=EP[:], in_=P3[:, :, 0, :], func=AF.Exp, scale=SDEC), r=[PSb[b_]], w=[eb])
                        k.act(lambda e: e.activation(out=EM[:], in_=P3[:, :, 0, :], func=AF.Exp, scale=-SDEC), r=[PSb[b_]], w=[eb])
                        k.act(lambda e: e.activation(out=EX[:], in_=P3[:, :, 1, :], func=AF.Exp, scale=SDEC), r=[PSb[b_]], w=[eb])
                        yield
                        for c in range(2):
                            mm(PS[c_][:, c * 128:(c + 1) * 128], A2[64:128, c * 128:(c + 1) * 128], XM[64:128, 6, :], True, True, [pb, xmb], [PSb[c_]], sw=True)
                        for c in range(2):
                            k.act(lambda e: e.activation(out=AT[:, c, :], in_=PS[c_][:, c * 128:(c + 1) * 128], func=AF.Sigmoid, bias=A0[:, c:c + 1]), r=[PSb[c_], pb], w=[ab])
                        yield
                        k.dve(lambda e: e.tensor_tensor(out=KK0[:], in0=XM[:, 2:4, :], in1=bc2(KKp), op=ALU.mult), r=[xmb, pb], w=[kb])
                        k.dve(lambda e: e.tensor_tensor(out=SQ[:], in0=KK0[:], in1=KK0[:], op=ALU.mult), r=[kb], w=[kb])
                        for c in range(2):
                            mm(PS[c_][:, 256 + c * 128:256 + (c + 1) * 128], BLK[:], SQ[:, c, :], True, True, [cb, kb], [PSb[c_]])
                        k.act(lambda e: e.activation(out=SQ[:], in_=v4(PS[c_][:, 256:512], 2), func=AF.Sqrt), r=[PSb[c_], kb], w=[kb])
                        yield
                        k.dve(lambda e: e.tensor_scalar(out=SQ[:], in0=SQ[:], scalar1=1e-12, scalar2=None, op0=ALU.max), r=[kb], w=[kb])
                        k.dve(lambda e: e.reciprocal(out=SQ[:], in_=SQ[:]), r=[kb], w=[kb])
                        k.dve(lambda e: e.tensor_tensor(out=KK0[:], in0=KK0[:], in1=SQ[:], op=ALU.mult), r=[kb], w=[kb])
                        yield
                        k.dve(lambda e: e.tensor_tensor(out=T1[:], in0=AT[:], in1=bc2(KA), op=ALU.mult), r=[ab, pb], w=[kb])
                        k.dve(lambda e: e.tensor_tensor(out=T1[:], in0=T1[:], in1=bc2(OMK), op=ALU.add), r=[kb, pb], w=[kb])
                        k.dve(lambda e: e.tensor_tensor(out=K2[:], in0=XM[:, 2:4, :], in1=T1[:], op=ALU.mult), r=[xmb, kb], w=[kb])
                        yield
                        if tt >= 2:
                            while not chain_done[tt - 2]:
                                yield
                        PCb = EP[:, :, 127:128].to_broadcast([128, 2, 128])
                        k.dve(lambda e: e.tensor_copy(out=PCs[:], in_=EP[:, :, 127]), r=[eb], w=[pcb])
                        k.dve(lambda e: e.tensor_tensor(out=BRt[:, :, 1, :], in0=XM[:, 0:2, :], in1=EP[:], op=ALU.mult), r=[xmb, eb], w=[opb])
                        k.dve(lambda e: e.tensor_tensor(out=BRt[:, :, 0, :], in0=KK0[:], in1=EX[:], op=ALU.mult), r=[kb, eb], w=[opb])
                        yield
                        k.dve(lambda e: e.tensor_tensor(out=KT32[:], in0=K2[:], in1=EM[:], op=ALU.mult), r=[kb, eb], w=[opb])
                        k.act(lambda e: e.activation(out=KTt[:], in_=KT32[:], func=AF.Copy), r=[opb], w=[opb])
                        k.dve(lambda e: e.tensor_tensor(out=KT32[:], in0=KT32[:], in1=PCb, op=ALU.mult), r=[opb, eb], w=[opb])
                        yield
                        k.dve(lambda e: e.scalar_tensor_tensor(out=T1[:], in0=KK0[:], scalar=-1.0, in1=AT[:], op0=ALU.mult, op1=ALU.mult), r=[kb, ab], w=[kb])
                        k.dve(lambda e: e.tensor_tensor(out=AL32[:], in0=T1[:], in1=EM[:], op=ALU.mult), r=[kb, eb], w=[opb])
                        k.act(lambda e: e.activation(out=ALt[:], in_=AL32[:], func=AF.Copy), r=[opb], w=[opb])
                        k.dve(lambda e: e.tensor_tensor(out=AL32[:], in0=AL32[:], in1=PCb, op=ALU.mult), r=[opb, eb], w=[opb])
                        yield
                        for c in range(2):
                            k.pe(lambda e: e.transpose(out=PS[a_][:, c * 128:(c + 1) * 128], in_=KT32[:, c, :], identity=IDF[:]), r=[opb, cb], w=[PSb[a_]])
                            k.pe(lambda e: e.transpose(out=PS[a_][:, 256 + c * 128:256 + (c + 1) * 128], in_=AL32[:, c, :], identity=IDF[:]), r=[opb, cb], w=[PSb[a_]])
                            k.pe(lambda e: e.transpose(out=PS[b_][:, c * 128:(c + 1) * 128], in_=XM[:, 4 + c, :], identity=IDF[:]), r=[xmb, cb], w=[PSb[b_]])
                        k.act(lambda e: e.activation(out=KPT[:], in_=PS[a_][:, 0:256], func=AF.Copy), r=[PSb[a_]], w=[kpb])
                        k.act(lambda e: e.activation(out=APT[:], in_=PS[a_][:, 256:512], func=AF.Copy), r=[PSb[a_]], w=[kpb])
                        k.act(lambda e: e.activation(out=VT[:], in_=PS[b_][:, 0:256], func=AF.Copy), r=[PSb[b_]], w=[vtb])
                        k.dve(lambda e: e.tensor_copy(out=V32[:], in_=PS[b_][:, 0:256]), r=[PSb[b_]], w=[v32b])
                        yield
                        k.dve(lambda e: e.tensor_tensor(out=SQ[:], in0=XM[:, 0:2, :], in1=K2[:], op=ALU.mult), r=[xmb, kb], w=[kb])
                        k.dve(lambda e: e.tensor_tensor(out=SQ[:], in0=SQ[:], in1=bc2(RKp), op=ALU.mult), r=[kb, pb], w=[kb])
                        for c in range(2):
                            mm(PS[b_][:, 256 + 2 * c:256 + 2 * c + 2], SQ[:, c, :], HSEL[:], True, True, [kb, cb], [PSb[b_]])
                        k.act(lambda e: e.activation(out=BON[:], in_=PS[b_][:, 256:260], func=AF.Copy), r=[PSb[b_]], w=[bonb])
                        yield
                        k.act(lambda e: e.activation(out=SZ[:], in_=XM[:, 7, :], func=AF.Sigmoid), r=[xmb], w=[szb])
                        mm(PS[c_][:, 0:256], SZ[:], G2[:], True, True, [szb, pb], [PSb[c_]])
                        k.act(lambda e: e.activation(out=GTM_[:], in_=PS[c_][:, 0:256], func=AF.Copy), r=[PSb[c_]], w=[gtb])
                        yield
                        for h in range(4):
                            c = h // 2
                            bk = a_ if h < 2 else b_
                            mm(PS[bk][:, (h % 2) * 256:(h % 2 + 1) * 256], KTt[hsl(h), c, :], BRt[hsl(h), c].rearrange("p t n -> p (t n)"), True, True, [opb], [PSb[bk]], sw=True)
                        for i_, bk in enumerate((a_, b_)):
                            k.dve(lambda e: e.tensor_tensor(out=A1m[:, 2 * i_:2 * i_ + 2, :], in0=v4(PS[bk][:], 2), in1=MLTLE[:].unsqueeze(1).to_broadcast([128, 2, 256]), op=ALU.mult), r=[PSb[bk], pb], w=[a1b])
                        yield
                        for h in range(4):
                            c = h // 2
                            bk = a_ if h < 2 else b_
                            mm(PS[bk][:, (h % 2) * 256:(h % 2 + 1) * 256], ALt[hsl(h), c, :], BRt[hsl(h), c].rearrange("p t n -> p (t n)"), True, True, [opb], [PSb[bk]], sw=True)
                        for i_, bk in enumerate((a_, b_)):
                            k.dve(lambda e: e.tensor_tensor(out=A2m[:, 2 * i_:2 * i_ + 2, :], in0=v4(PS[bk][:], 2), in1=MLTLE[:].unsqueeze(1).to_broadcast([128, 2, 256]), op=ALU.mult), r=[PSb[bk], pb], w=[a2b])
                        yield
                        for h in range(4):
                            c = h // 2
                            mm(PS[b_][:, h * 128:(h + 1) * 128], BRt[hsl(h), c, 0, :], ALt[hsl(h), c, :], True, True, [opb], [PSb[b_]], sw=True)
                        k.dve(lambda e: e.tensor_tensor(out=Z[0][:], in0=v4(PS[b_][:], 4), in1=MGT[:].unsqueeze(1).to_broadcast([128, 4, 128]), op=ALU.mult), r=[PSb[b_], cb], w=[zb[0]])
                        k.dve(lambda e: e.tensor_tensor(out=TT[0][:], in0=A2m[:, :, 0:128], in1=IDF[:].unsqueeze(1).to_broadcast([128, 4, 128]), op=ALU.add), r=[a2b, cb], w=[ttb[0]])
                        yield
                        for i_ in range(1, 7):
                            o_, n_ = (i_ - 1) % 2, i_ % 2
                            if i_ == 1:
                                zt_old, zt_oldb = (lambda h: A2m[:, h, 0:128]), a2b
                            else:
                                zt_old, zt_oldb = (lambda h, o_=o_: ZT[o_][:, h, :]), ztb[o_]
                            for h in range(4):
                                mm(PS[a_][:, h * 128:(h + 1) * 128], zt_old(h), Z[o_][:, h, :], True, True, [zt_oldb, zb[o_]], [PSb[a_]])
                            k.act(lambda e: e.activation(out=Z[n_][:], in_=v4(PS[a_][:], 4), func=AF.Copy), r=[PSb[a_]], w=[zb[n_]])
                            if i_ < 6:
                                for h in range(4):
                                    mm(PS[b_][:, h * 128:(h + 1) * 128], Z[o_][:, h, :], zt_old(h), True, True, [zt_oldb, zb[o_]], [PSb[b_]])
                                k.dve(lambda e: e.tensor_copy(out=ZT[n_][:], in_=v4(PS[b_][:], 4)), r=[PSb[b_]], w=[ztb[n_]])
                            yield
                            for h in range(4):
                                mm(PS[c_][:, h * 128:(h + 1) * 128], Z[n_][:, h, :], TT[o_][:, h, :], True, True, [zb[n_], ttb[o_]], [PSb[c_]])
                            k.dve(lambda e: e.tensor_tensor(out=TT[n_][:], in0=v4(PS[c_][:], 4), in1=TT[o_][:], op=ALU.add), r=[PSb[c_], ttb[o_]], w=[ttb[n_]])
                            yield
                        prep_done[tt] = True
                        yield

                def chain_gen():
                    for tt in range(NT):
                        while not prep_done[tt]:
                            yield
                        st = sets[tt % 2]
                        tsl = slice(tt * 128, (tt + 1) * 128)
                        BRt, A1m, A2m, KPT, APT, VT, V32, BON, GTM_, PCs = st.BRt, st.A1m, st.A2m, st.KPT, st.APT, st.VT, st.V32, st.BON, st.GTM_, st.PCs
                        opb, tmb, a1b, a2b = st.opb, st.tmb, st.a1b, st.a2b
                        kpb, vtb, v32b, bonb, gtb, pcb = st.kpb, st.vtb, st.v32b, st.bonb, st.gtb, st.pcb
                        TTf, ttfb = st.TT[0], st.ttb[0]
                        for h in range(4):
                            c = h // 2
                            mm(PS[4][:, hc(h)], BRt[hsl(h), c, 0, :], MBF[hsl(h), c, :], True, False, [opb, mbfb], [PSb[4]], sw=True)
                            mm(PS[4][:, hc(h)], A1m[:, h, 0:128], VT[:, hc(h)], False, True, [a1b, vtb], [PSb[4]])
                        k.act(lambda e: e.activation(out=X0[:], in_=v4(PS[4][:, 0:256], 4), func=AF.Copy), r=[PSb[4]], w=[x0b])
                        yield
                        for h in range(4):
                            mm(PS[4][:, 256 + h * 64:256 + (h + 1) * 64], TTf[:, h, :], X0[:, h, :], True, True, [ttfb, x0b], [PSb[4]])
                        k.act(lambda e: e.activation(out=UH[:], in_=v4(PS[4][:, 256:512], 4), func=AF.Copy), r=[PSb[4]], w=[uhb])
                        yield
                        for h in range(4):
                            c = h // 2
                            mm(PS[5][hsl(h), 256 + c * 64:256 + (c + 1) * 64], KPT[:, hc(h)], VT[:, hc(h)], True, False, [kpb, vtb], [PSb[5]])
                            mm(PS[5][hsl(h), 256 + c * 64:256 + (c + 1) * 64], APT[:, hc(h)], UH[:, h, :], False, True, [kpb, uhb], [PSb[5]])
                        for h in range(4):
                            c = h // 2
                            mm(PS[5][:, hc(h)], BRt[hsl(h), c, 1, :], MBF[hsl(h), c, :], True, False, [opb, mbfb], [PSb[5]], sw=True)
                            mm(PS[5][:, hc(h)], A1m[:, h, 128:256], VT[:, hc(h)], False, False, [a1b, vtb], [PSb[5]])
                            mm(PS[5][:, hc(h)], A2m[:, h, 128:256], UH[:, h, :], False, True, [a2b, uhb], [PSb[5]])
                        yield
                        k.dve(lambda e: e.tensor_tensor(out=M32[:], in0=M32[:], in1=PCs[:].unsqueeze(2).to_broadcast([128, 2, 64]), op=ALU.mult), r=[mb, pcb], w=[mb])
                        k.dve(lambda e: e.tensor_tensor(out=M32[:], in0=M32[:], in1=v4(PS[5][:, 256:384], 2), op=ALU.add), r=[mb, PSb[5]], w=[mb])
                        k.act(lambda e: e.activation(out=MBF[:], in_=M32[:], func=AF.Copy), r=[mb], w=[mbfb])
                        k.act(lambda e: e.activation(out=Y[:], in_=v4(PS[5][:, 0:256], 4), func=AF.Copy), r=[PSb[5]], w=[yb])
                        yield
                        k.dve(lambda e: e.tensor_reduce(out=S1[:], in_=Y[:], axis=AX.X, op=ALU.add), r=[yb], w=[yb])
                        k.dve(lambda e: e.tensor_tensor(out=SQY[:], in0=Y[:], in1=Y[:], op=ALU.mult), r=[yb], w=[yb])
                        k.dve(lambda e: e.tensor_reduce(out=S2[:], in_=SQY[:], axis=AX.X, op=ALU.add), r=[yb], w=[yb])
                        k.dve(lambda e: e.tensor_scalar(out=S1[:], in0=S1[:], scalar1=1.0 / 64, scalar2=None, op0=ALU.mult), r=[yb], w=[yb])
                        yield
                        k.dve(lambda e: e.tensor_tensor(out=MS[:], in0=S1[:], in1=S1[:], op=ALU.mult), r=[yb], w=[yb])
                        k.dve(lambda e: e.scalar_tensor_tensor(out=S2[:], in0=S2[:], scalar=1.0 / 64, in1=MS[:], op0=ALU.mult, op1=ALU.subtract), r=[yb], w=[yb])
                        k.act(lambda e: e.activation(out=S2[:], in_=S2[:], func=AF.Sqrt, bias=RWKV_GN_EPS), r=[yb], w=[yb])
                        k.dve(lambda e: e.reciprocal(out=S2[:], in_=S2[:]), r=[yb], w=[yb])
                        yield
                        k.dve(lambda e: e.tensor_tensor(out=Y[:], in0=Y[:], in1=S1[:].unsqueeze(2).to_broadcast([128, 4, 64]), op=ALU.subtract), r=[yb], w=[yb])
                        k.dve(lambda e: e.tensor_tensor(out=Y[:], in0=Y[:], in1=S2[:].unsqueeze(2).to_broadcast([128, 4, 64]), op=ALU.mult), r=[yb], w=[yb])
                        Yf = Y[:].rearrange("p h d -> p (h d)")
                        k.dve(lambda e: e.tensor_tensor(out=Yf, in0=Yf, in1=LNG[:], op=ALU.mult), r=[yb, pb], w=[yb])
                        k.dve(lambda e: e.tensor_tensor(out=Yf, in0=Yf, in1=LNB[:], op=ALU.add), r=[yb, pb], w=[yb])
                        yield
                        k.dve(lambda e: e.tensor_tensor(out=SQY[:], in0=v4(V32[:], 4), in1=BON[:].unsqueeze(2).to_broadcast([128, 4, 64]), op=ALU.mult), r=[v32b, bonb, yb], w=[yb])
                        k.dve(lambda e: e.tensor_tensor(out=Y[:], in0=Y[:], in1=SQY[:], op=ALU.add), r=[yb], w=[yb])
                        k.dve(lambda e: e.tensor_tensor(out=Yf, in0=Yf, in1=GTM_[:], op=ALU.mult), r=[yb, gtb], w=[yb])
                        for cc in range(2):
                            k.pe(lambda e: e.transpose(out=PS[4][:, cc * 128:(cc + 1) * 128], in_=Yf[:, cc * 128:(cc + 1) * 128], identity=IDF[:]), r=[yb, cb], w=[PSb[4]])
                        k.act(lambda e: e.activation(out=BR[:, 6:8, tsl], in_=v4(PS[4][:, 0:256], 2), func=AF.Copy), r=[PSb[4]], w=[BRb])
                        chain_done[tt] = True
                        yield

                run_gens([prep_gen(0), prep_gen(1), chain_gen()] + list(extra_gens), weights=[int(x) for x in os.environ.get('K_W', '1,1,1,1').split(',')][:3 + len(extra_gens)])
                k.barrier()

        def residual_stats(tt, ysrc_fn, yrb, JK, jb, SSY, RSY, ssb_):
            ssb = ssb_[tt % len(ssb_)]
            for half in range(2):
                k.act(lambda e: e.activation(out=JK[:, 0:512], in_=ysrc_fn(half), func=AF.Square, accum_out=SSY[:, 2 * tt + half:2 * tt + half + 1]), r=[yrb[half]], w=[jb, ssb])
            k.dve(lambda e: e.tensor_tensor(out=RSY[:, tt:tt + 1], in0=SSY[:, 2 * tt:2 * tt + 1], in1=SSY[:, 2 * tt + 1:2 * tt + 2], op=ALU.add), r=[ssb], w=[ssb])
            k.act(lambda e: e.activation(out=RSY[:, tt:tt + 1], in_=RSY[:, tt:tt + 1], func=AF.Sqrt, scale=1.0 / D, bias=RMS_EPS), r=[ssb], w=[ssb])
            k.dve(lambda e: e.reciprocal(out=RSY[:, tt:tt + 1], in_=RSY[:, tt:tt + 1]), r=[ssb], w=[ssb])

        def residual_apply(tt, ysrc_fn, yrb, GT_, modb, xdst, xdstb, XR, xrb, YT, ytb, RSY, ssb_):
            ssb = ssb_[tt % len(ssb_)]
            s = tt % 2
            x_ = tt % len(XR)
            for half in range(2):
                hs = slice(half * 512, (half + 1) * 512)
                k.dve(lambda e: e.scalar_tensor_tensor(out=YT[s][:, hs], in0=ysrc_fn(half), scalar=RSY[:, tt:tt + 1], in1=GT_[:, hs], op0=ALU.mult, op1=ALU.mult), r=[yrb[half], ssb, modb], w=[ytb[s]])
            k.pool(lambda e: e.tensor_tensor(out=XR[x_][:], in0=XR[x_][:], in1=YT[s][:], op=ALU.add), r=[xrb[x_], ytb[s]], w=[xrb[x_]])
            k.dma("sp", xdst[tt * 128:(tt + 1) * 128, :], XR[x_][:], reads=[xrb[x_]], writes=[xdstb[tt]])

        def phase_merge(l, BR, BRb, GTM, modb, xsrc, xsrcb, xdst, xdstb):
            with ExitStack() as ph:
                MT = sb(ph, "mt", [128, 8, T], BF16)
                mtb = [Buf() for _ in range(NB)]
                WO = sb(ph, "wo", [128, 8, D], BF16)
                GW = [sb(ph, "gwt%d" % i_, [128, 4, 8, 128], BF16) for i_ in range(2)]
                WB = [sb(ph, "wbt%d" % i_, [128, 4, 2, 128], BF16) for i_ in range(2)]
                SGm = [sb(ph, "msg%d" % i_, [128, 512]) for i_ in range(2)]
                TMP = [sb(ph, "mtmp%d" % i_, [128, 512]) for i_ in range(2)]
                ACC = [sb(ph, "macc%d" % i_, [128, 512]) for i_ in range(2)]
                XR = [sb(ph, "mxr%d" % i_, [128, D]) for i_ in range(4)]
                YT = [sb(ph, "myt%d" % i_, [128, D]) for i_ in range(2)]
                JK = sb(ph, "mjk", [128, 512], BF16)
                SSY = sb(ph, "mssy", [128, 2 * NT])
                RSY = sb(ph, "mrsy", [128, NT])
                wbb, sgb, tmpb, accb, ytb = ([Buf(), Buf()] for _ in range(5))
                xrb = [Buf() for _ in range(4)]
                gwb = [[Buf() for _ in range(4)] for _ in range(2)]
                wob, jb = Buf(), Buf()
                ssb = [Buf() for _ in range(4)]
                k.pool(lambda e: e.memset(SSY[:], 0.0), w=ssb)
                wbv = dr["w_branch"][l].rearrange("g (kc p) n -> p g kc n", p=128)

                def load(j_):
                    s_ = j_ % 2
                    if j_ == 0:
                        for g_ in range(4):
                            k.dma("pool", GW[s_][:, g_], dr["w_gate"][l, j_][:, g_], writes=[gwb[s_][g_]])
                    else:
                        k.dma("pool", GW[s_][:], dr["w_gate"][l, j_], writes=gwb[s_])
                    k.dma("pool", WB[s_][:], wbv[:, :, :, j_ * 128:(j_ + 1) * 128], writes=[wbb[s_]])

                load(0)
                cnt = 0
                for j_ in range(8):
                    s = j_ % 2
                    if j_ + 1 < 8:
                        load(j_ + 1)
                    else:
                        k.dma("pool", WO[:], dr["w_out"][l].rearrange("(kc p) n -> p kc n", p=128), writes=[wob])
                    for tb in range(NB):
                        sl = slice(tb * 512, (tb + 1) * 512)
                        a = tb % 2
                        for g in range(4):
                            i2 = cnt % 2
                            cnt += 1
                            pg, pgb = PS[i2], PSb[i2]
                            pbr, pbrb = PS[2 + i2], PSb[2 + i2]
                            for kc in range(8):
                                mm(pg[:], GW[s][:, g, kc, :], HT[:, kc, sl], kc == 0, kc == 7, [gwb[s][g], HTb], [pgb])
                            for kc2 in range(2):
                                mm(pbr[:], WB[s][:, g, kc2, :], BR[:, 2 * g + kc2, sl], kc2 == 0, kc2 == 1, [wbb[s], BRb], [pbrb])
                            k.act(lambda e: e.activation(out=SGm[i2][:], in_=pg[:], func=AF.Sigmoid), r=[pgb], w=[sgb[i2]])
                            if g == 0:
                                k.dve(lambda e: e.tensor_tensor(out=ACC[a][:], in0=pbr[:], in1=SGm[i2][:], op=ALU.mult), r=[pbrb, sgb[i2]], w=[accb[a]])
                            else:
                                k.dve(lambda e: e.tensor_tensor(out=TMP[i2][:], in0=pbr[:], in1=SGm[i2][:], op=ALU.mult), r=[pbrb, sgb[i2]], w=[tmpb[i2]])
                                if g < 3:
                                    k.pool(lambda e: e.tensor_tensor(out=ACC[a][:], in0=ACC[a][:], in1=TMP[i2][:], op=ALU.add), r=[accb[a], tmpb[i2]], w=[accb[a]])
                                else:
                                    k.pool(lambda e: e.tensor_tensor(out=MT[:, j_, sl], in0=ACC[a][:], in1=TMP[i2][:], op=ALU.add), r=[accb[a], tmpb[i2]], w=[mtb[tb]])
                tap("mt%d" % l, MT[:], mtb[3], [128, 8, T])
                def wo_mm(tt):
                    s = tt % 2
                    for half in range(2):
                        for j_ in range(8):
                            mm(PS[4 + 2 * s + half][:], MT[:, j_, tt * 128:(tt + 1) * 128], WO[:, j_, half * 512:(half + 1) * 512], j_ == 0, j_ == 7, [mtb[tt // 4], wob], [PSb[4 + 2 * s + half]])

                def ysrc(tt):
                    s = tt % 2
                    return (lambda half: PS[4 + 2 * s + half][:]), [PSb[4 + 2 * s + h_] for h_ in range(2)]

                def x_load(tt):
                    k.dma("sp", XR[tt % 4][:], xsrc[tt * 128:(tt + 1) * 128, :], reads=[xsrcb[tt]], writes=[xrb[tt % 4]])

                x_load(0)
                x_load(1)
                for step in range(NT + 2):
                    if step >= 2:
                        fn_, bf_ = ysrc(step - 2)
                        residual_apply(step - 2, fn_, bf_, GTM, modb, xdst, xdstb, XR, xrb, YT, ytb, RSY, ssb)
                    if step + 2 < NT:
                        x_load(step + 2)
                    if step < NT:
                        wo_mm(step)
                    if 1 <= step <= NT:
                        fn_, bf_ = ysrc(step - 1)
                        residual_stats(step - 1, fn_, bf_, JK, jb, SSY, RSY, ssb)
                k.barrier()

        def phase_ffn(l, moe, MODT, GTF, modb, xsrc, xsrcb, xdst, xdstb):
            with ExitStack() as ph:
                YA = sb(ph, "ya", [128, NT, D])
                yab = [Buf() for _ in range(NT)]
                COMB = sb(ph, "comb", [128, NT, 8])
                combb = Buf()
                if moe:
                    RW = sb(ph, "rwt", [128, 8, 8])
                    RB = sb(ph, "rbt", [128, 8])
                    RT = sb(ph, "rtmp", [128, 8, 8])
                    rb_, rtb = Buf(), Buf()
                    k.dma("sp", RW[:], dr["router_wt"][0], writes=[rb_])
                    k.dma("sp", RB[:], dr["router_b"][0:1, :].partition_broadcast(128), writes=[rb_])

                    def router(tt, H32, h32b):
                        LGt, EQ1, L2, EQ2 = RT[:, 0, :], RT[:, 1, :], RT[:, 2, :], RT[:, 3, :]
                        M1, M2, DD, P1, P2 = RT[:, 4, 0:1], RT[:, 4, 1:2], RT[:, 4, 2:3], RT[:, 4, 3:4], RT[:, 4, 4:5]
                        for kc in range(8):
                            mm(PS[7][:, 0:8], H32[:, kc, :], RW[:, kc, :], kc == 0, kc == 7, [h32b, rb_], [PSb[7]])
                        k.dve(lambda e: e.tensor_tensor(out=LGt, in0=PS[7][:, 0:8], in1=RB[:], op=ALU.add), r=[PSb[7], rb_], w=[rtb])
                        k.dve(lambda e: e.tensor_reduce(out=M1, in_=LGt, axis=AX.X, op=ALU.max), r=[rtb], w=[rtb])
                        k.dve(lambda e: e.tensor_scalar(out=EQ1, in0=LGt, scalar1=M1, scalar2=None, op0=ALU.is_equal), r=[rtb], w=[rtb])
                        k.dve(lambda e: e.scalar_tensor_tensor(out=L2, in0=EQ1, scalar=-1e30, in1=LGt, op0=ALU.mult, op1=ALU.add), r=[rtb], w=[rtb])
                        k.dve(lambda e: e.tensor_reduce(out=M2, in_=L2, axis=AX.X, op=ALU.max), r=[rtb], w=[rtb])
                        k.dve(lambda e: e.tensor_scalar(out=EQ2, in0=L2, scalar1=M2, scalar2=None, op0=ALU.is_equal), r=[rtb], w=[rtb])
                        k.dve(lambda e: e.tensor_tensor(out=DD, in0=M2, in1=M1, op=ALU.subtract), r=[rtb], w=[rtb])
                        k.act(lambda e: e.activation(out=DD, in_=DD, func=AF.Exp), r=[rtb], w=[rtb])
                        k.dve(lambda e: e.tensor_scalar(out=P1, in0=DD, scalar1=1.0, scalar2=None, op0=ALU.add), r=[rtb], w=[rtb])
                        k.dve(lambda e: e.reciprocal(out=P1, in_=P1), r=[rtb], w=[rtb])
                        k.dve(lambda e: e.tensor_scalar(out=P2, in0=P1, scalar1=-1.0, scalar2=1.0, op0=ALU.mult, op1=ALU.add), r=[rtb], w=[rtb])
                        k.dve(lambda e: e.tensor_scalar(out=COMB[:, tt, :], in0=EQ1, scalar1=P1, scalar2=None, op0=ALU.mult), r=[rtb], w=[combb])
                        k.dve(lambda e: e.scalar_tensor_tensor(out=COMB[:, tt, :], in0=EQ2, scalar=P2, in1=COMB[:, tt, :], op0=ALU.mult, op1=ALU.add), r=[rtb, combb], w=[combb])
                else:
                    router = None
                phase_prenorm(xsrc, xsrcb, dr["nfp_t"][l], 32, 24, MODT, modb, router=router)
                tap("htf%d" % l, HT[:], HTb, [128, 8, T])
                if moe:
                    tap("comb%d" % l, COMB[:], combb, [128, NT, 8])
                with ExitStack() as p1:
                    WGt = [sb(p1, "fwg%d" % i_, [128, 4, 8, 128], BF16) for i_ in range(2)]
                    WUt = [sb(p1, "fwu%d" % i_, [128, 4, 8, 128], BF16) for i_ in range(2)]
                    WDt = [sb(p1, "fwd%d" % i_, [128, 4, D], BF16) for i_ in range(2)]
                    AT = sb(p1, "fat", [128, 4, T], BF16)
                    SGf = [sb(p1, "fsg%d" % i_, [128, 512]) for i_ in range(2)]
                    sgb = [Buf(), Buf()]
                    wgb, wub, wdb = ([[Buf() for _ in range(4)] for _ in range(2)] for _ in range(3))
                    atb = [Buf() for _ in range(4)]
                    groups = []
                    if moe:
                        for e_ in range(N_EXP):
                            for fc0 in range(0, 28, 4):
                                groups.append((e_, fc0, 4))
                    else:
                        for fc0 in range(0, 22, 4):
                            groups.append((None, fc0, min(4, 22 - fc0)))

                    def load(gi):
                        e_, fc0, F = groups[gi]
                        s_ = gi % 2
                        if moe:
                            wg_ap = dr["moe_wg"][0, e_, fc0:fc0 + F].rearrange("fc p kc f -> p fc kc f")
                            wu_ap = dr["moe_wu"][0, e_, fc0:fc0 + F].rearrange("fc p kc f -> p fc kc f")
                            wd_ap = dr["moe_wd"][0, e_, fc0 * 128:(fc0 + F) * 128, :].rearrange("(fc p) d -> p fc d", p=128)
                        else:
                            wg_ap = dr["ffn_wg"][0, fc0:fc0 + F].rearrange("fc p kc f -> p fc kc f")
                            wu_ap = dr["ffn_wu"][0, fc0:fc0 + F].rearrange("fc p kc f -> p fc kc f")
                            wd_ap = dr["ffn_wd"][0, fc0 * 128:(fc0 + F) * 128, :].rearrange("(fc p) d -> p fc d", p=128)
                        if gi == 0:
                            for fi_ in range(F):
                                k.dma("pool", WGt[s_][:, fi_], wg_ap[:, fi_], writes=[wgb[s_][fi_]])
                                k.dma("pool", WUt[s_][:, fi_], wu_ap[:, fi_], writes=[wub[s_][fi_]])
                            for fi_ in range(F):
                                k.dma("pool", WDt[s_][:, fi_], wd_ap[:, fi_], writes=[wdb[s_][fi_]])
                        else:
                            k.dma("pool", WGt[s_][:, 0:F], wg_ap, writes=wgb[s_][0:F])
                            k.dma("pool", WUt[s_][:, 0:F], wu_ap, writes=wub[s_][0:F])
                            k.dma("pool", WDt[s_][:, 0:F], wd_ap, writes=wdb[s_][0:F])

                    load(0)
                    cnt = 0
                    for gi, (e_, fc0, F) in enumerate(groups):
                        s = gi % 2
                        if gi + 1 < len(groups):
                            load(gi + 1)
                        for fi in range(F):
                            for tb in range(NB):
                                sl = slice(tb * 512, (tb + 1) * 512)
                                i2 = cnt % 2
                                cnt += 1
                                pg, pgb = PS[i2], PSb[i2]
                                pu, pub = PS[2 + i2], PSb[2 + i2]
                                for kc in range(8):
                                    mm(pg[:], WGt[s][:, fi, kc, :], HT[:, kc, sl], kc == 0, kc == 7, [wgb[s][fi], HTb], [pgb])
                                for kc in range(8):
                                    mm(pu[:], WUt[s][:, fi, kc, :], HT[:, kc, sl], kc == 0, kc == 7, [wub[s][fi], HTb], [pub])
                                k.act(lambda e: e.activation(out=SGf[i2][:], in_=pg[:], func=AF.Silu), r=[pgb], w=[sgb[i2]])
                                k.dve(lambda e: e.tensor_tensor(out=AT[:, fi, sl], in0=pu[:], in1=SGf[i2][:], op=ALU.mult), r=[pub, sgb[i2]], w=[atb[fi]])
                        for tt in range(NT):
                            for half in range(2):
                                hs = slice(half * 512, (half + 1) * 512)
                                i4 = (2 * tt + half) % 4
                                py, pyb = PS[4 + i4], PSb[4 + i4]
                                for fi in range(F):
                                    mm(py[:], AT[:, fi, tt * 128:(tt + 1) * 128], WDt[s][:, fi, hs], fi == 0, fi == F - 1, [atb[fi], wdb[s][fi]], [pyb])
                                if gi == 0:
                                    if moe:
                                        k.dve(lambda e: e.tensor_scalar(out=YA[:, tt, hs], in0=py[:], scalar1=COMB[:, tt, e_:e_ + 1], scalar2=None, op0=ALU.mult), r=[pyb, combb], w=[yab[tt]])
                                    else:
                                        k.act(lambda e: e.activation(out=YA[:, tt, hs], in_=py[:], func=AF.Copy), r=[pyb], w=[yab[tt]])
                                elif moe:
                                    k.dve(lambda e: e.scalar_tensor_tensor(out=YA[:, tt, hs], in0=py[:], scalar=COMB[:, tt, e_:e_ + 1], in1=YA[:, tt, hs], op0=ALU.mult, op1=ALU.add), r=[pyb, combb, yab[tt]], w=[yab[tt]])
                                else:
                                    k.dve(lambda e: e.tensor_tensor(out=YA[:, tt, hs], in0=py[:], in1=YA[:, tt, hs], op=ALU.add), r=[pyb, yab[tt]], w=[yab[tt]])
                    k.barrier()
                tap("ya%d" % l, YA[:], yab[NT - 1], [128, NT, D])
                with ExitStack() as p2:
                    XR = [sb(p2, "fxr%d" % i_, [128, D]) for i_ in range(4)]
                    YT = [sb(p2, "fyt%d" % i_, [128, D]) for i_ in range(2)]
                    JK = sb(p2, "fjk", [128, 512], BF16)
                    SSY = sb(p2, "fssy", [128, 2 * NT])
                    RSY = sb(p2, "frsy", [128, NT])
                    xrb, ytb = [Buf() for _ in range(4)], [Buf(), Buf()]
                    jb = Buf()
                    ssb = [Buf() for _ in range(4)]
                    k.pool(lambda e: e.memset(SSY[:], 0.0), w=ssb)
                    def x_load(tt):
                        k.dma("sp", XR[tt % 4][:], xsrc[tt * 128:(tt + 1) * 128, :], reads=[xsrcb[tt]], writes=[xrb[tt % 4]])

                    yfn = (lambda tt_: (lambda half: YA[:, tt_, half * 512:(half + 1) * 512]))
                    x_load(0)
                    x_load(1)
                    x_load(2)
                    for tt in range(NT + 1):
                        if tt < NT:
                            residual_stats(tt, yfn(tt), [yab[tt], yab[tt]], JK, jb, SSY, RSY, ssb)
                        if tt >= 1:
                            residual_apply(tt - 1, yfn(tt - 1), [yab[tt - 1], yab[tt - 1]], GTF, modb, xdst, xdstb, XR, xrb, YT, ytb, RSY, ssb)
                            if tt + 2 < NT:
                                x_load(tt + 2)
                    k.barrier()

        MODT = sb(es, "modt", [128, 48])
        GTM = sb(es, "gtm", [128, D])
        GTF = sb(es, "gtf", [128, D])
        modb = Buf("mod")
        xin_b = [Buf() for _ in range(NT)]
        xs_b = [[Buf() for _ in range(NT)] for _ in range(2)]
        yout_b = [Buf() for _ in range(NT)]
        chain = [(dr["x"], xin_b), (xs[0], xs_b[0]), (xs[1], xs_b[1]), (xs[0], xs_b[0]), (y_out, yout_b)]
        import os
        nlay = int(os.environ.get("K_NLAYERS", str(DEPTH)))
        skip_ffn = os.environ.get("K_SKIP_FFN", "0") == "1"
        for l in range(nlay):
            (xa, xab), (xm_, xmb_), (xf, xfb) = chain[2 * l], chain[2 * l + 1], chain[2 * l + 2]
            if skip_ffn or (nlay < DEPTH and l == nlay - 1 and False):
                pass
            phase_mod(l, None, MODT, GTM, GTF, modb)
            with ExitStack() as mixs:
                BR = sb(mixs, "br", [128, 8, T], BF16)
                BRb = Buf("br")
                phase_prenorm(xa, xab, dr["nmp_t"][l], 8, 0, MODT, modb)
                tap("ht%d" % l, HT[:], HTb, [128, 8, T])
                if os.environ.get("K_SEQCONV", "0") == "1":
                    phase_conv(l, BR, BRb)
                    phase_diff(l, BR, BRb)
                else:
                    with ExitStack() as cph:
                        cg = conv_setup(l, cph)(BR, BRb)

                        def pump(n, cg=cg):
                            for _ in range(n):
                                try:
                                    next(cg)
                                except StopIteration:
                                    return

                        phase_diff(l, BR, BRb, pump=pump)
                if os.environ.get("K_RWKV1", "0") == "1":
                    phase_gla(l, BR, BRb)
                    phase_rwkv(l, BR, BRb)
                else:
                    phase_rwkv2(l, BR, BRb, with_gla=True)
                tap("br%d" % l, BR[:], BRb, [128, 8, T])
                last_mix = skip_ffn and l == nlay - 1
                phase_merge(l, BR, BRb, GTM, modb, xa, xab, (y_out if last_mix else xm_), (yout_b if last_mix else xmb_))
            if last_mix:
                break
            last = (l == nlay - 1)
            phase_ffn(l, (l % 2 == 1), MODT, GTF, modb, xm_, xmb_, (y_out if last else xf), (yout_b if last else xfb))
        k.finish(yout_b)
        k.finish(P.tapbufs)
        k.barrier()
        print("instructions", k.nins, "waits", k.nwait)
    return nc, tap_out, list(dr.keys())


def prep_inputs(inputs, b):
    g = lambda n: np.asarray(inputs[n])
    m = {}
    m["x"] = np.ascontiguousarray(g("x")[b])
    m["c_t"] = _pcol(g("c")[b], 8)
    m["pos"] = np.ascontiguousarray(g("positions")[b][None, :]).astype(np.int32)
    m.update(_consts())
    return m


_SHARED = None


def prep_shared(inputs):
    g = lambda n: np.asarray(inputs[n])
    L = DEPTH
    m = {}
    m["ada_w"] = g("ada_w")
    m["ada_bt"] = np.stack([_pcol(g("ada_b")[l], 48) for l in range(L)])
    m["ada_b"] = g("ada_b")
    m["nmp_t"] = np.stack([_pcol(g("norm_mix_pre")[l], 8) for l in range(L)])
    m["nfp_t"] = np.stack([_pcol(g("norm_ffn_pre")[l], 8) for l in range(L)])
    m["norm_mix_post"] = g("norm_mix_post")
    m["norm_ffn_post"] = g("norm_ffn_post")
    w_in = g("w_in")
    m["w_in_a"] = np.ascontiguousarray(w_in[:, :, :N_IN_A])
    wg = w_in[:, :, N_IN_A:].reshape(L, 8, 128, 4, 8, 128)
    m["w_gate"] = np.ascontiguousarray(wg.transpose(0, 4, 2, 3, 1, 5))
    m["gla_gate_w2"] = g("gla_gate_w2")
    m["gla_gate_b"] = g("gla_gate_b")
    m["gla_norm"] = g("gla_norm")
    m["diff_lambda"] = g("diff_lambda").reshape(L, 128)
    m["diff_subln_t"] = np.stack([np.tile(g("diff_subln")[l], 2)[:, None] for l in range(L)])
    m["conv_wt"] = np.ascontiguousarray(g("conv_w").reshape(L, 31, 2, 128).transpose(0, 3, 2, 1))
    m["conv_bt"] = np.stack([_pcol(g("conv_b")[l], 2) for l in range(L)])
    m["conv_lg_t"] = np.stack([_pcol(g("conv_ln_g")[l], 2) for l in range(L)])
    m["conv_lb_t"] = np.stack([_pcol(g("conv_ln_b")[l], 2) for l in range(L)])
    m["rwkv_mu_t"] = np.stack([_pcol(g("rwkv_mu")[l], 8) for l in range(L)])
    m["rwkv_w0"] = g("rwkv_w0")
    m["rwkv_w2"] = g("rwkv_w2")
    m["rwkv_a0_t"] = np.stack([_pcol(g("rwkv_a0")[l], 2) for l in range(L)])
    m["rwkv_a2"] = g("rwkv_a2")
    m["rwkv_g2"] = g("rwkv_g2")
    m["rwkv_kk_t"] = np.stack([_pcol(g("rwkv_k_k")[l], 2) for l in range(L)])
    m["rwkv_ka_t"] = np.stack([_pcol(g("rwkv_k_a")[l], 2) for l in range(L)])
    m["rwkv_rk_t"] = np.stack([_pcol(g("rwkv_r_k")[l].reshape(256), 2) for l in range(L)])
    m["rwkv_ln_g"] = g("rwkv_ln_g")
    m["rwkv_ln_b"] = g("rwkv_ln_b")
    m["w_branch"] = g("w_branch")
    m["w_out"] = g("w_out")
    relay = lambda w, nfc: np.ascontiguousarray(w.reshape(w.shape[:-2] + (8, 128, nfc, 128)).transpose(tuple(range(w.ndim - 2)) + (w.ndim, w.ndim - 1, w.ndim - 2, w.ndim + 1)))
    m["ffn_wg"] = relay(g("ffn_w_gate"), 22)
    m["ffn_wu"] = relay(g("ffn_w_up"), 22)
    m["ffn_wd"] = g("ffn_w_down")
    m["router_wt"] = np.ascontiguousarray(g("router_w").reshape(1, 8, 128, 8).transpose(0, 2, 1, 3))
    m["router_b"] = g("router_b")
    m["moe_wg"] = relay(g("moe_w_gate"), 28)
    m["moe_wu"] = relay(g("moe_w_up"), 28)
    m["moe_wd"] = g("moe_w_down")
    return {k_: np.ascontiguousarray(v, dtype=np.float32) for k_, v in m.items()}


def kernel(**inputs):
    nc, _, used = build_program()
    shared = prep_shared(inputs)
    n = 8
    in_maps = []
    for b in range(n):
        pc = prep_inputs(inputs, b)
        in_maps.append({nm: (pc[nm] if nm in pc else shared[nm]) for nm in used})
    res = run_bass_kernel_spmd(nc, in_maps, core_ids=list(range(n)))
    return np.stack([np.asarray(r["y"], dtype=np.float32) for r in res.results], axis=0)
```
